# Optimizing a Trainium2 kernel written in Bass

```python
import math
import jax
import jax.numpy as jnp
from jax import lax
import numpy as np

D_MODEL = 1024
BATCH = 16
SEQ = 4096
DEPTH = 1

N_HEADS_DIL = 6
N_HEADS_MOBA = 6
N_HEADS_MEM = 4
N_HEADS_MIX = N_HEADS_DIL + N_HEADS_MOBA + N_HEADS_MEM
HEAD_DIM = D_MODEL // N_HEADS_MIX
W_DIL = N_HEADS_DIL * HEAD_DIM
W_MOBA = N_HEADS_MOBA * HEAD_DIM
W_MEM = N_HEADS_MEM * HEAD_DIM
MIX_WIDTH = W_DIL + W_MOBA + W_MEM
IN_WIDTH = 3 * W_DIL + 3 * W_MOBA + W_MEM
DIL_CONFIGS = ((128, 1), (512, 4), (2048, 16))
BAND_BLOCK = 128
MOBA_BLOCK = 256
MOBA_TOPK = 3
MOBA_QBLOCK = 128
MEM_LEN = 256
PEER_HEADS = 8
PEER_NKEYS = 128
PEER_EXPERTS = PEER_NKEYS * PEER_NKEYS
PEER_TOPK = 16
PEER_DKEY = 256
PEER_CHUNK = 128
RMS_EPS = 1e-6
NEG_INF = -1e30

kernel_name = 'hymba_style_dilated_moba_mem_peer_layer'


def rms_norm(a, g):
    af = a.astype(jnp.float32)
    y = af * lax.rsqrt(jnp.mean(af * af, axis=-1, keepdims=True) + RMS_EPS)
    return (y * g.astype(jnp.float32)).astype(a.dtype)


def split_heads(a, n_heads):
    b, s, _ = a.shape
    return a.reshape(b, s, n_heads, HEAD_DIM).transpose(0, 2, 1, 3)


def merge_heads(a):
    b, h, s, dh = a.shape
    return a.transpose(0, 2, 1, 3).reshape(b, s, h * dh)


def alibi_slopes(n):
    return jnp.exp2(-8.0 * jnp.arange(1, n + 1, dtype=jnp.float32) / n)


def dilated_branch(q, k, v, slopes, window, dilation):
    B, H, S, Dh = q.shape
    L = S // dilation
    n_blk = -(-L // BAND_BLOCK)
    Lp = n_blk * BAND_BLOCK
    reach = window // dilation

    def split(a):
        a = a.reshape(B, H, L, dilation, Dh).transpose(0, 1, 3, 2, 4)
        a = jnp.pad(a, ((0, 0), (0, 0), (0, 0), (0, Lp - L), (0, 0)))
        return a.reshape(B, H, dilation, n_blk, BAND_BLOCK, Dh)

    def with_prev(a):
        prev = jnp.pad(a, ((0, 0), (0, 0), (0, 0), (1, 0), (0, 0), (0, 0)))[:, :, :, :-1]
        return jnp.concatenate([prev, a], axis=4)

    qb = split(q)
    kc = with_prev(split(k))
    vc = with_prev(split(v))
    s = jnp.einsum('bhrnqd,bhrnkd->bhrnqk', qb, kc).astype(jnp.float32) * (1.0 / math.sqrt(Dh))
    ql = jnp.arange(BAND_BLOCK)[:, None]
    kl = jnp.arange(2 * BAND_BLOCK)[None, :]
    delta = BAND_BLOCK + ql - kl
    blk = jnp.arange(n_blk)[:, None, None]
    valid = (delta >= 0) & (delta <= reach) & (blk * BAND_BLOCK - BAND_BLOCK + kl[None] >= 0)
    bias = -slopes[:, None, None, None, None] * (delta * dilation).astype(jnp.float32)
    s = jnp.where(valid, s + bias, NEG_INF)
    m = jnp.max(s, axis=-1, keepdims=True)
    p = jnp.exp(s - m)
    den = jnp.sum(p, axis=-1, keepdims=True)
    lse = m[..., 0] + jnp.log(den[..., 0])
    o = jnp.einsum('bhrnqk,bhrnkd->bhrnqd', p.astype(v.dtype), vc) / den.astype(v.dtype)

    def merge(a):
        tail = a.shape[5:]
        a = a.reshape((B, H, dilation, Lp) + tail)[:, :, :, :L]
        a = jnp.moveaxis(a, 2, 3)
        return a.reshape((B, H, S) + tail)

    return merge(o), merge(lse)


def dilated_mixture(q, k, v, slopes):
    outs, lses = [], []
    for window, dilation in DIL_CONFIGS:
        o, l = dilated_branch(q, k, v, slopes, window, dilation)
        outs.append(o)
        lses.append(l)
    wts = jax.nn.softmax(jnp.stack(lses, axis=0), axis=0).astype(q.dtype)
    return jnp.einsum('cbhs,cbhsd->bhsd', wts, jnp.stack(outs, axis=0))


def moba_attention(q, k, v, slopes):
    B, H, S, Dh = q.shape
    n_blk = -(-S // MOBA_BLOCK)
    Sp = n_blk * MOBA_BLOCK
    pad = ((0, 0), (0, 0), (0, Sp - S), (0, 0))
    kb = jnp.pad(k, pad).reshape(B, H, n_blk, MOBA_BLOCK, Dh)
    vb = jnp.pad(v, pad).reshape(B, H, n_blk, MOBA_BLOCK, Dh)
    k_mean = jnp.mean(kb.astype(jnp.float32), axis=3).astype(k.dtype)
    gate = jnp.einsum('bhsd,bhnd->bhsn', q, k_mean).astype(jnp.float32)
    n_past = jnp.arange(S) // MOBA_BLOCK
    past = jnp.arange(n_blk)[None, :] < n_past[:, None]
    gate = jnp.where(past, gate, NEG_INF)
    k_sel = min(MOBA_TOPK, n_blk)
    _, sel = lax.top_k(gate, k_sel)

    n_qb = S // MOBA_QBLOCK
    qq = q.reshape(B, H, n_qb, MOBA_QBLOCK, Dh).transpose(0, 2, 1, 3, 4).reshape(B * n_qb, H, MOBA_QBLOCK, Dh)
    ss = sel.reshape(B, H, n_qb, MOBA_QBLOCK, k_sel).transpose(0, 2, 1, 3, 4).reshape(B * n_qb, H, MOBA_QBLOCK, k_sel)
    b_ids = jnp.repeat(jnp.arange(B, dtype=jnp.int32), n_qb)
    n_ids = jnp.tile(jnp.arange(n_qb, dtype=jnp.int32), B)
    head = jnp.arange(H)[:, None, None]
    scale = 1.0 / math.sqrt(Dh)
    n_sel_keys = k_sel * MOBA_BLOCK

    def step(args):
        q_blk, sel_blk, b, n = args
        kb_b = lax.dynamic_index_in_dim(kb, b, axis=0, keepdims=False)
        vb_b = lax.dynamic_index_in_dim(vb, b, axis=0, keepdims=False)
        t_q = n * MOBA_QBLOCK + jnp.arange(MOBA_QBLOCK)
        k_g = kb_b[head, sel_blk]
        v_g = vb_b[head, sel_blk]
        s_sel = jnp.einsum('hqd,hqjkd->hqjk', q_blk, k_g).astype(jnp.float32) * scale
        s_pos = sel_blk[..., None] * MOBA_BLOCK + jnp.arange(MOBA_BLOCK)
        ok = jnp.arange(k_sel)[None, :] < (t_q // MOBA_BLOCK)[:, None]
        dist_sel = (t_q[None, :, None, None] - s_pos).astype(jnp.float32)
        s_sel = jnp.where(ok[None, :, :, None], s_sel - slopes[:, None, None, None] * dist_sel, NEG_INF)
        own = (n * MOBA_QBLOCK) // MOBA_BLOCK
        k_own = lax.dynamic_index_in_dim(kb_b, own, axis=1, keepdims=False)
        v_own = lax.dynamic_index_in_dim(vb_b, own, axis=1, keepdims=False)
        o_pos = own * MOBA_BLOCK + jnp.arange(MOBA_BLOCK)
        dist_own = t_q[:, None] - o_pos[None, :]
        s_own = jnp.einsum('hqd,hkd->hqk', q_blk, k_own).astype(jnp.float32) * scale
        s_own = jnp.where(dist_own >= 0, s_own - slopes[:, None, None] * dist_own.astype(jnp.float32), NEG_INF)
        s_all = jnp.concatenate([s_sel.reshape(H, MOBA_QBLOCK, n_sel_keys), s_own], axis=-1)
        p = jax.nn.softmax(s_all, axis=-1).astype(v.dtype)
        p_sel = p[..., :n_sel_keys].reshape(H, MOBA_QBLOCK, k_sel, MOBA_BLOCK)
        p_own = p[..., n_sel_keys:]
        return (jnp.einsum('hqjk,hqjkd->hqd', p_sel, v_g)
                + jnp.einsum('hqk,hkd->hqd', p_own, v_own))

    out = lax.map(step, (qq, ss, b_ids, n_ids))
    return out.reshape(B, n_qb, H, MOBA_QBLOCK, Dh).transpose(0, 2, 1, 3, 4).reshape(B, H, S, Dh)


def memory_attention(q, k, v):
    s = jnp.einsum('bhsd,bhmd->bhsm', q, k).astype(jnp.float32) * (1.0 / math.sqrt(q.shape[-1]))
    p = jax.nn.softmax(s, axis=-1).astype(v.dtype)
    return jnp.einsum('bhsm,bhmd->bhsd', p, v)


def peer_ffn(h, w_q, sub1, sub2, u, v):
    B, S, D = h.shape
    half = PEER_DKEY // 2
    qry = (h @ w_q).reshape(B, S, PEER_HEADS, PEER_DKEY)
    s1 = jnp.einsum('bshd,hkd->bshk', qry[..., :half], sub1).astype(jnp.float32)
    s2 = jnp.einsum('bshd,hkd->bshk', qry[..., half:], sub2).astype(jnp.float32)
    v1, i1 = lax.top_k(s1, PEER_TOPK)
    v2, i2 = lax.top_k(s2, PEER_TOPK)
    cand = (v1[..., :, None] + v2[..., None, :]).reshape(B, S, PEER_HEADS, PEER_TOPK * PEER_TOPK)
    cand_idx = (i1[..., :, None] * PEER_NKEYS + i2[..., None, :]).reshape(B, S, PEER_HEADS, PEER_TOPK * PEER_TOPK)
    top_s, pos = lax.top_k(cand, PEER_TOPK)
    expert = jnp.take_along_axis(cand_idx, pos, axis=-1)
    gate = jax.nn.softmax(top_s, axis=-1)
    T = B * S
    E = PEER_HEADS * PEER_TOPK
    n_chunk = T // PEER_CHUNK
    hx = h.reshape(n_chunk, PEER_CHUNK, D)
    ex = expert.reshape(n_chunk, PEER_CHUNK, E)
    gx = gate.reshape(n_chunk, PEER_CHUNK, E).astype(h.dtype)

    def step(args):
        x_c, e_c, g_c = args
        a = jnp.einsum('cd,ced->ce', x_c, u[e_c])
        w = jax.nn.gelu(a.astype(jnp.float32), approximate=False).astype(x_c.dtype) * g_c
        return jnp.einsum('ce,ced->cd', w, v[e_c])

    return lax.map(step, (hx, ex, gx)).reshape(B, S, D)


def hybrid_layer(x, mem, g_mix, w_in, qg_dil, kg_dil, qg_moba, kg_moba, qg_mem, kg_mem, g_memtok, w_mem_kv,
                 og_dil, og_moba, og_mem, w_out, g_ffn, w_peer_q, sub1, sub2, peer_u, peer_v):
    slopes = alibi_slopes(N_HEADS_DIL + N_HEADS_MOBA)
    sl_dil, sl_moba = slopes[0::2], slopes[1::2]
    hn = rms_norm(x, g_mix)
    proj = hn @ w_in
    cuts = [W_DIL, 2 * W_DIL, 3 * W_DIL, 3 * W_DIL + W_MOBA, 3 * W_DIL + 2 * W_MOBA, 3 * W_DIL + 3 * W_MOBA]
    q_d, k_d, v_d, q_b, k_b, v_b, q_m = jnp.split(proj, cuts, axis=-1)
    q_d = rms_norm(split_heads(q_d, N_HEADS_DIL), qg_dil)
    k_d = rms_norm(split_heads(k_d, N_HEADS_DIL), kg_dil)
    o_dil = dilated_mixture(q_d, k_d, split_heads(v_d, N_HEADS_DIL), sl_dil)
    q_b = rms_norm(split_heads(q_b, N_HEADS_MOBA), qg_moba)
    k_b = rms_norm(split_heads(k_b, N_HEADS_MOBA), kg_moba)
    o_moba = moba_attention(q_b, k_b, split_heads(v_b, N_HEADS_MOBA), sl_moba)
    kv = rms_norm(mem, g_memtok) @ w_mem_kv
    k_m, v_m = jnp.split(kv, 2, axis=-1)
    q_m = rms_norm(split_heads(q_m, N_HEADS_MEM), qg_mem)
    k_m = rms_norm(split_heads(k_m, N_HEADS_MEM), kg_mem)
    o_mem = memory_attention(q_m, k_m, split_heads(v_m, N_HEADS_MEM))
    y = jnp.concatenate([rms_norm(merge_heads(o_dil), og_dil),
                         rms_norm(merge_heads(o_moba), og_moba),
                         rms_norm(merge_heads(o_mem), og_mem)], axis=-1) @ w_out
    x = x + y
    x = x + peer_ffn(rms_norm(x, g_ffn), w_peer_q, sub1, sub2, peer_u, peer_v)
    return x


def setup_inputs(seed: int = 0) -> dict:
    key = jax.random.key(seed)
    ks = jax.random.split(key, 24)

    def nrm(k, shape, scale):
        return jax.random.normal(k, shape, jnp.float32) * scale

    def gain(k, n):
        return 1.0 + 0.02 * jax.random.normal(k, (DEPTH, n), jnp.float32)

    return {
        'x': nrm(ks[0], (BATCH, SEQ, D_MODEL), 1.0),
        'mem': nrm(ks[1], (BATCH, MEM_LEN, D_MODEL), 1.0),
        'g_mix': gain(ks[2], D_MODEL),
        'w_in': nrm(ks[3], (DEPTH, D_MODEL, IN_WIDTH), D_MODEL ** -0.5),
        'qg_dil': gain(ks[4], HEAD_DIM),
        'kg_dil': gain(ks[5], HEAD_DIM),
        'qg_moba': gain(ks[6], HEAD_DIM),
        'kg_moba': gain(ks[7], HEAD_DIM),
        'qg_mem': gain(ks[8], HEAD_DIM),
        'kg_mem': gain(ks[9], HEAD_DIM),
        'g_memtok': gain(ks[10], D_MODEL),
        'w_mem_kv': nrm(ks[11], (DEPTH, D_MODEL, 2 * W_MEM), D_MODEL ** -0.5),
        'og_dil': gain(ks[12], W_DIL),
        'og_moba': gain(ks[13], W_MOBA),
        'og_mem': gain(ks[14], W_MEM),
        'w_out': nrm(ks[15], (DEPTH, MIX_WIDTH, D_MODEL), MIX_WIDTH ** -0.5),
        'g_ffn': gain(ks[16], D_MODEL),
        'w_peer_q': nrm(ks[17], (DEPTH, D_MODEL, PEER_HEADS * PEER_DKEY), D_MODEL ** -0.5),
        'peer_subkeys_1': nrm(ks[18], (DEPTH, PEER_HEADS, PEER_NKEYS, PEER_DKEY // 2), (PEER_DKEY // 2) ** -0.5),
        'peer_subkeys_2': nrm(ks[19], (DEPTH, PEER_HEADS, PEER_NKEYS, PEER_DKEY // 2), (PEER_DKEY // 2) ** -0.5),
        'peer_u': nrm(ks[20], (DEPTH, PEER_EXPERTS, D_MODEL), D_MODEL ** -0.5),
        'peer_v': nrm(ks[21], (DEPTH, PEER_EXPERTS, D_MODEL), PEER_HEADS ** -0.5),
    }


def reference(x, mem, g_mix, w_in, qg_dil, kg_dil, qg_moba, kg_moba, qg_mem, kg_mem, g_memtok, w_mem_kv,
              og_dil, og_moba, og_mem, w_out, g_ffn, w_peer_q, peer_subkeys_1, peer_subkeys_2, peer_u, peer_v):
    h = x
    for layer in range(DEPTH):
        h = hybrid_layer(h, mem, g_mix[layer], w_in[layer], qg_dil[layer], kg_dil[layer], qg_moba[layer],
                         kg_moba[layer], qg_mem[layer], kg_mem[layer], g_memtok[layer], w_mem_kv[layer],
                         og_dil[layer], og_moba[layer], og_mem[layer], w_out[layer], g_ffn[layer],
                         w_peer_q[layer], peer_subkeys_1[layer], peer_subkeys_2[layer], peer_u[layer],
                         peer_v[layer])
    return h
```

```python
import math
from contextlib import ExitStack

import numpy as np
import ml_dtypes

import concourse.bass as bass
import concourse.mybir as mybir
from concourse.bass_utils import run_bass_kernel_spmd

F32 = mybir.dt.float32
BF16 = mybir.dt.bfloat16
U32 = mybir.dt.uint32
I32 = mybir.dt.int32
AF = mybir.ActivationFunctionType
ALU = mybir.AluOpType
AX = mybir.AxisListType

NCORES = 8
SEQ = 4096
DM = 1024
NT = SEQ // 128
SCALE = 0.125
EPS = 1e-6
NEGM = -30000.0
SEM_LIMIT = 32000
DIL = (1, 4, 16)


class Sched:
    ENGS = ("pe", "act", "dve", "pool", "sp")

    def __init__(self, nc, stack):
        self.nc = nc
        self.stack = stack
        self.ops = {e: [] for e in self.ENGS}
        self.cnt = {}
        self.sems = {}
        self.last_w = {}
        self.readers = {}
        self.seen = {e: {} for e in self.ENGS}
        self.nops = 0

    def _sem(self, key, epoch):
        k = (key, epoch)
        if k not in self.sems:
            self.sems[k] = self.stack.enter_context(self.nc.semaphore("s%d" % len(self.sems)))
        return self.sems[k]

    def _bump(self, key, amt):
        ep, v = self.cnt.get(key, (0, 0))
        if v + amt > SEM_LIMIT:
            ep, v = ep + 1, 0
        v += amt
        self.cnt[key] = (ep, v)
        return (key, ep, v)

    def op(self, engine, fn, reads=(), writes=(), dma=False, semkey=None, pe_self=False):
        deps = set()
        for r in reads:
            if r in self.last_w:
                deps.add(self.last_w[r])
            if r.startswith("ps_"):
                for ev in self.readers.get(r, ()):
                    deps.add(ev)
        for w in writes:
            if w in self.last_w:
                deps.add(self.last_w[w])
            for ev in self.readers.get(w, ()):
                deps.add(ev)
        if dma:
            key = semkey if semkey is not None else ("dma", (writes[0] if writes else reads[0]))
            ev = self._bump(key, 16)
            amt = 16
        else:
            key = engine
            ev = self._bump(key, 1)
            amt = 1
        seen = self.seen[engine]
        best = {}
        for (k, ep, v) in deps:
            if k == "pe" and engine == "pe" and not dma and not pe_self:
                continue
            if best.get(k, (-1, -1)) < (ep, v):
                best[k] = (ep, v)
        waits = []
        for k, (ep, v) in best.items():
            if seen.get(k, (-1, -1)) >= (ep, v):
                continue
            seen[k] = (ep, v)
            waits.append((self._sem(k, ep), v))
        self.ops[engine].append((waits, fn, self._sem(ev[0], ev[1]), amt))
        for w in writes:
            self.last_w[w] = ev
            self.readers[w] = []
        for r in reads:
            if r not in writes:
                self.readers.setdefault(r, []).append(ev)
        self.nops += 1
        return ev

    def barrier(self, engines=None):
        for e in (engines or self.ENGS):
            waits = []
            seen = self.seen[e]
            for key, (ep, v) in self.cnt.items():
                if seen.get(key, (-1, -1)) >= (ep, v):
                    continue
                seen[key] = (ep, v)
                waits.append((self._sem(key, ep), v))
            if waits:
                self.ops[e].append((waits, None, None, 0))

    def emit(self):
        nc = self.nc
        with nc.Block() as block:
            def run(engname):
                def body(eng):
                    for waits, fn, sem, amt in self.ops[engname]:
                        for s, v in waits:
                            eng.wait_ge(s, v)
                        if fn is not None:
                            fn(eng).then_inc(sem, amt)
                return body
            block.tensor(run("pe"))
            block.scalar(run("act"))
            block.vector(run("dve"))
            block.gpsimd(run("pool"))
            block.sync(run("sp"))


def _bf(a):
    return np.asarray(a, dtype=np.float32).astype(ml_dtypes.bfloat16)


def _split3(a):
    a = np.asarray(a, dtype=np.float64)
    h = _bf(a)
    r = a - h.astype(np.float64)
    l = _bf(r)
    r2 = r - l.astype(np.float64)
    l2 = _bf(r2)
    return h, l, l2


def _constants():
    c = {}
    slopes = 2.0 ** (-8.0 * np.arange(1, 13, dtype=np.float64) / 12.0)
    sl_dil, sl_moba = slopes[0::2], slopes[1::2]
    c["c_ident"] = _bf(np.eye(128))
    bo = np.zeros((128, 128)); bo[:64, :64] = 1; bo[64:, 64:] = 1
    c["c_blockones"] = _bf(bo)
    c["c_ones"] = _bf(np.ones((128, 128)))
    kl = np.arange(128)[:, None]
    ql = np.arange(128)[None, :]
    tab = np.zeros((3, 128, 2, 3, 2, 2, 128), dtype=ml_dtypes.bfloat16)
    for p in range(3):
        for h in range(2):
            for di, d in enumerate(DIL):
                for jj in range(2):
                    delta = ql - kl + (128 if jj == 0 else 0)
                    valid = (delta >= 0) & (delta <= 128)
                    b = np.where(valid, -sl_dil[2 * p + h] * d * delta / SCALE, NEGM)
                    hi = _bf(b)
                    lo = _bf(b - hi.astype(np.float64))
                    tab[p, :, h, di, 0, jj, :] = hi
                    tab[p, :, h, di, 1, jj, :] = lo
    c["c_dilbias"] = tab.reshape(3, 128, 24 * 128)
    c["c_tri"] = _bf(np.where(kl <= ql, 0.0, NEGM))
    pm = np.zeros((16, 16))
    for npast in range(16):
        for n in range(16):
            pm[npast, n] = 0.0 if n < npast else (1e30 if n == npast else -1e30)
    c["c_pm2"] = np.broadcast_to(pm.reshape(1, 256), (128, 256)).astype(np.float32).copy()
    tok = np.arange(SEQ)
    mq = np.zeros((6, 6, SEQ), dtype=ml_dtypes.bfloat16)
    for h in range(6):
        cc = 1024.0 * sl_moba[h]
        a, b_, c_ = _split3(np.full(SEQ, cc))
        mq[h, 0], mq[h, 1], mq[h, 2] = a, b_, c_
        a, b_, c_ = _split3(-cc * (tok // 128))
        mq[h, 3], mq[h, 4], mq[h, 5] = a, b_, c_
    c["c_moba_q"] = mq
    mk = np.zeros((22, SEQ))
    for n in range(16):
        mk[n, n * 256:(n + 1) * 256] = 1.0
    mk[16:19] = (tok // 128)[None, :]
    mk[19:22] = 1.0
    c["c_moba_k"] = _bf(mk)
    c["c_moba_bcol"] = (sl_moba[None, :] * (np.arange(128)[:, None] - 64.0)).astype(np.float32)
    c["c_iota16"] = np.broadcast_to(np.arange(16, dtype=np.float32)[None, :], (128, 16)).copy()
    return c


def build_program(nseq=2, debug=False, do_peer=True, stages=('dil', 'moba', 'mem'), npair=3, peer_tiles=NT):
    nc = bass.Bass("TRN2", target_bir_lowering=False)

    def din(name, shape, dt):
        return nc.dram_tensor(name, list(shape), dt, kind="ExternalInput").ap()

    x_d = din("x", [nseq, SEQ, DM], F32)
    mem_d = din("mem", [nseq, 256, DM], F32)
    w_in_d = din("w_in", [DM, 2560], F32)
    w_mkv_d = din("w_mem_kv", [DM, 512], F32)
    w_out_d = din("w_out", [DM, DM], F32)
    w_pq_d = din("w_peer_q", [DM, 2048], F32)
    sub_d = din("subkeys", [16, 128, 128], F32)
    pu_d = din("peer_u", [16384 if do_peer else 128, DM], F32)
    pv_d = din("peer_v", [16384 if do_peer else 128, DM], F32)
    gbc_d = din("g_bc", [3, 128, DM], F32)
    gcol_d = din("g_cols", [128, 14], F32)
    cst = _constants()
    cd = {}
    for k, v in cst.items():
        dt = BF16 if v.dtype == ml_dtypes.bfloat16 else F32
        cd[k] = din(k, v.shape, dt)
    out_d = nc.dram_tensor("out", [nseq, SEQ, DM], F32, kind="ExternalOutput").ap()
    yscr_d = nc.dram_tensor("yscr", [nseq, 8, 128, SEQ], BF16,
                            kind=("ExternalOutput" if debug else "Internal")).ap()
    if debug:
        dbg_hnT = nc.dram_tensor("dbg_hnT", [128, 8, SEQ], BF16, kind="ExternalOutput").ap()

    with ExitStack() as top:
        S = Sched(nc, top)

        uid = [0]

        def sb(st, name, shape, dt):
            uid[0] += 1
            return st.enter_context(nc.sbuf_tensor("%s_%d" % (name, uid[0]), list(shape), dt))

        def ps(st, name, shape, dt):
            return st.enter_context(nc.psum_tensor(name, list(shape), dt))

        def mm(out, lhsT, rhs, start, stop, reads, writes, skip=False, pe_self=False):
            S.op("pe", lambda e: e.matmul(out, lhsT=lhsT, rhs=rhs, start=start, stop=stop, skip_group_check=skip), reads, writes,
                 pe_self=pe_self)

        def tr(out, in_, ident, reads, writes):
            S.op("pe", lambda e: e.transpose(out=out, in_=in_, identity=ident), reads, writes)

        def act(out, in_, func, reads, writes, bias=None, scale=None, accum_out=None):
            kw = {}
            if bias is not None:
                kw["bias"] = bias
            if scale is not None:
                kw["scale"] = scale
            if accum_out is not None:
                kw["accum_out"] = accum_out
            S.op("act", lambda e: e.activation(out=out, in_=in_, func=func, **kw), reads, writes)

        def acopy(out, in_, reads, writes):
            S.op("act", lambda e: e.copy(out=out, in_=in_), reads, writes)

        def vcopy(out, in_, reads, writes, eng="dve"):
            S.op(eng, lambda e: e.tensor_copy(out=out, in_=in_), reads, writes)

        def vtt(out, in0, in1, op, reads, writes, eng="dve"):
            S.op(eng, lambda e: e.tensor_tensor(out=out, in0=in0, in1=in1, op=op), reads, writes)

        def vts(out, in0, s1, s2, op0, op1, reads, writes, eng="dve"):
            if op1 is None:
                S.op(eng, lambda e: e.tensor_scalar(out=out, in0=in0, scalar1=s1, scalar2=None, op0=op0), reads, writes)
            else:
                S.op(eng, lambda e: e.tensor_scalar(out=out, in0=in0, scalar1=s1, scalar2=s2, op0=op0, op1=op1), reads, writes)

        def vstt(out, in0, scalar, in1, op0, op1, reads, writes):
            S.op("dve", lambda e: e.scalar_tensor_tensor(out=out, in0=in0, scalar=scalar, in1=in1, op0=op0, op1=op1),
                 reads, writes)

        def vrecip(out, in_, reads, writes):
            S.op("dve", lambda e: e.reciprocal(out=out, in_=in_), reads, writes)

        def dma(out, in_, reads, writes, eng="sp", semkey=None):
            S.op(eng, lambda e: e.dma_start(out=out, in_=in_), reads, writes, dma=True, semkey=semkey)

        ident = sb(top, "ident", [128, 128], BF16)
        blockones = sb(top, "blockones", [128, 128], BF16)
        ones = sb(top, "ones", [128, 128], BF16)
        gcols = sb(top, "gcols", [128, 14], F32)
        epsc = sb(top, "epsc", [128, 1], F32)
        tri = sb(top, "tri", [128, 128], BF16)
        pm2 = sb(top, "pm2", [128, 16, 16], F32)
        bcol = sb(top, "bcol", [128, 6], F32)
        iota16 = sb(top, "iota16", [128, 16], F32)
        dma(ident[:], cd["c_ident"], [], ["ident"])
        dma(blockones[:], cd["c_blockones"], [], ["blockones"])
        dma(ones[:], cd["c_ones"], [], ["ones"])
        dma(gcols[:], gcol_d, [], ["gcols"])
        dma(tri[:], cd["c_tri"], [], ["tri"])
        dma(pm2[:], cd["c_pm2"].rearrange("p (a b) -> p a b", a=16), [], ["pm2"])
        dma(bcol[:], cd["c_moba_bcol"], [], ["bcol"])
        dma(iota16[:], cd["c_iota16"], [], ["iota16"])
        S.op("dve", lambda e: e.memset(epsc[:], EPS), [], ["epsc"])

        ps_tr = ps(top, "ps_tr", [128, 8, 128], BF16)
        ps_pj = [ps(top, "ps_pj%d" % i, [128, 512], F32) for i in range(2)]
        ps_ss = ps(top, "ps_ss", [128, 512], F32)
        ps_s = [ps(top, "ps_s%d" % i, [128, 4, 128], F32) for i in range(2)]
        ps_o = [ps(top, "ps_o%d" % i, [128, 4, 128], F32) for i in range(2)]

        def rms_rows_to_T(st, src_dram_tile_fn, ntiles, gslot, dstT, dst_name, tag):
            gbc = sb(st, "gbc" + tag, [128, DM], F32)
            dma(gbc[:], gbc_d[gslot], [], ["gbc" + tag])
            xt = [sb(st, "xt%s%d" % (tag, i), [128, DM], F32) for i in range(2)]
            hn = [sb(st, "hn%s%d" % (tag, i), [128, DM], BF16) for i in range(2)]
            junk = sb(st, "junk" + tag, [128, DM], BF16)
            ssq = sb(st, "ssq" + tag, [128, 2], F32)
            rt = sb(st, "rt" + tag, [128, 2], F32)
            rs = sb(st, "rs" + tag, [128, 2], F32)
            for t in range(ntiles):
                sl = t % 2
                X, H = "xt%s%d" % (tag, sl), "hn%s%d" % (tag, sl)
                dma(xt[sl][:], src_dram_tile_fn(t), [], [X])
                act(junk[:], xt[sl][:], AF.Square, [X], ["junk" + tag, "ssq%s%d" % (tag, sl)], accum_out=ssq[:, sl:sl + 1])
                act(rt[:, sl:sl + 1], ssq[:, sl:sl + 1], AF.Sqrt, ["ssq%s%d" % (tag, sl), "epsc"], ["rt%s%d" % (tag, sl)],
                    bias=epsc[:], scale=1.0 / DM)
                vrecip(rs[:, sl:sl + 1], rt[:, sl:sl + 1], ["rt%s%d" % (tag, sl)], ["rs%s%d" % (tag, sl)])
                vstt(hn[sl][:], xt[sl][:], rs[:, sl:sl + 1], gbc[:], ALU.mult, ALU.mult,
                     [X, "rs%s%d" % (tag, sl), "gbc" + tag], [H])
                for kc in range(8):
                    tr(ps_tr[:, kc, :], hn[sl][:, kc * 128:(kc + 1) * 128], ident[:], [H, "ident"], ["ps_tr"])
                acopy(dstT[:, :, t * 128:(t + 1) * 128], ps_tr[:], ["ps_tr"], [dst_name])

        wctr = [0]

        def load_w(st_w, wstage, wdst, wname, w_dram, c0, ncols):
            for cc in range(0, ncols, 128):
                sl = wctr[0] % 2
                wctr[0] += 1
                dma(wstage[sl][:], w_dram[:, c0 + cc:c0 + cc + 128].rearrange("(kc p) n -> p kc n", p=128),
                    [], ["wstage%d" % sl])
                eng = "pool" if (wctr[0] % 2) else "dve"
                vcopy(wdst[:, :, cc:cc + 128], wstage[sl][:], ["wstage%d" % sl], [wname], eng=eng)

        pjctr = [0]

        def proj_norm(wbf, wname, m0, M, src, srcname, ntok, gcol, blk, blkname, dst_fn, dstname, scr):
            sqb, rtb, rsb = scr
            N = min(512, ntok)
            for tc in range(ntok // N):
                sl = pjctr[0] % 2
                pjctr[0] += 1
                P = "ps_pj%d" % sl
                for kc in range(8):
                    mm(ps_pj[sl][0:M, 0:N], wbf[:, kc, m0:m0 + M], src[:, kc, tc * N:(tc + 1) * N], kc == 0, kc == 7,
                       [wname, srcname], [P])
                act(sqb[0:M, 0:N], ps_pj[sl][0:M, 0:N], AF.Square, [P], ["sqb"])
                mm(ps_ss[0:M, 0:N], blk[0:M, 0:M], sqb[0:M, 0:N], True, True, ["sqb", blkname], ["ps_ss"])
                act(rtb[0:M, 0:N], ps_ss[0:M, 0:N], AF.Sqrt, ["ps_ss", "epsc"], ["rtb"], bias=epsc[0:M, :], scale=1.0 / 64)
                vrecip(rsb[0:M, 0:N], rtb[0:M, 0:N], ["rtb"], ["rsb"])
                vstt(dst_fn(tc, N), ps_pj[sl][0:M, 0:N], gcol, rsb[0:M, 0:N], ALU.mult, ALU.mult,
                     [P, "rsb", "gcols"], [dstname])

        def proj_v(wbf, wname, c0, src, srcname, tok_slices, dst, dstname):
            nt = len(tok_slices)
            for jb in range(0, nt, 4):
                sl = pjctr[0] % 2
                pjctr[0] += 1
                P = "ps_pj%d" % sl
                nb = min(4, nt - jb)
                for i in range(nb):
                    for kc in range(8):
                        mm(ps_pj[sl][:, i * 128:(i + 1) * 128], src[:, kc, tok_slices[jb + i]], wbf[:, kc, c0:c0 + 128],
                           kc == 0, kc == 7, [wname, srcname], [P])
                o = dst[:, jb:jb + nb, :]
                i_ = ps_pj[sl][:, 0:nb * 128].rearrange("p (a b) -> p a b", a=nb)
                import os
                if (jb // 4) % 2 == 0 and not os.environ.get("PVDVE"):
                    acopy(o, i_, [P], [dstname])
                else:
                    vcopy(o, i_, [P], [dstname])

        def finalize_pair(acc, o16p, o16name, ssqacc, first, scr):
            sqb, rtb, rsb = scr
            for tc in range(8):
                c = slice(tc * 512, (tc + 1) * 512)
                vrecip(rsb[:, :], acc[:, 1, c], ["acc"], ["rsb"])
                vtt(rtb[:, :], acc[:, 0, c], rsb[:, :], ALU.mult, ["acc", "rsb"], ["rtb"])
                act(sqb[:, :], rtb[:, :], AF.Square, ["rtb"], ["sqb"])
                vcopy(o16p[:, c], rtb[:, :], ["rtb"], [o16name], eng="pool")
                mm(ps_ss[:, :], ones[:], sqb[:, :], True, True, ["sqb", "ones"], ["ps_ss"])
                if first:
                    vcopy(ssqacc[:, c], ps_ss[:, :], ["ps_ss"], ["ssqacc"])
                else:
                    vtt(ssqacc[:, c], ps_ss[:, :], ssqacc[:, c], ALU.add, ["ps_ss", "ssqacc"], ["ssqacc"])

        def group_norm_store(b, o16, npairs, nfeat, ssqacc, chunk0, ybuf, scr):
            sqb, rtb, rsb = scr
            for tc in range(8):
                c = slice(tc * 512, (tc + 1) * 512)
                act(rtb[:, :], ssqacc[:, c], AF.Sqrt, ["ssqacc", "epsc"], ["rtb"], bias=epsc[:], scale=1.0 / nfeat)
                vrecip(ssqacc[:, c], rtb[:, :], ["rtb"], ["ssqacc"])
            for p in range(npairs):
                Y = "o16_%d" % p
                for tc in range(8):
                    c = slice(tc * 512, (tc + 1) * 512)
                    vstt(o16[p][:, c], o16[p][:, c], gcols[:, 6 + chunk0 + p:7 + chunk0 + p], ssqacc[:, c],
                         ALU.mult, ALU.mult, [Y, "ssqacc", "gcols"], [Y])
                dma(yscr_d[b, chunk0 + p], o16[p][:], [Y], ["yscr%d_%d" % (b, chunk0 + p)])

        def attn_tile(sl, qk_list, v_list, acc_view, acc_first, exp_bias=None, evac_eng="dve", acc_part=None):
            Ps = "ps_s%d" % sl
            nslots = 0
            prev_base = None
            for (si, kT_ap, qT_ap, knames, extras) in qk_list:
                nslots = max(nslots, si + 1)
                base = kT_ap.base_partition()
                mm(ps_s[sl][:, si, :], kT_ap, qT_ap, True, len(extras) == 0, knames, [Ps],
                   pe_self=(prev_base is not None and base != prev_base))
                prev_base = base
                for ei, (l_, r_, nm) in enumerate(extras):
                    mm(ps_s[sl][:, si, :], l_, r_, False, ei == len(extras) - 1, nm, [Ps])
            PT = "pT%d" % sl
            used = sorted(q[0] for q in qk_list)
            if used == list(range(nslots)):
                ssel = slice(0, nslots)
            else:
                step = used[1] - used[0] if len(used) > 1 else 1
                assert used == list(range(used[0], used[-1] + 1, step))
                ssel = slice(used[0], used[-1] + 1, step)
            if exp_bias is None:
                act(pT[sl][:, ssel, :], ps_s[sl][:, ssel, :], AF.Exp, [Ps], [PT], scale=SCALE)
            else:
                act(pT[sl][:, ssel, :], ps_s[sl][:, ssel, :], AF.Exp, [Ps, "bcol"], [PT], scale=SCALE, bias=exp_bias)
            return PT


        NU = 6

        def peer(b):
            with ExitStack() as st:
                wout = sb(st, "wout", [128, 8, 1024], BF16)
                wpq = sb(st, "wpq", [128, 8, 2048], BF16)
                subT = sb(st, "subT", [128, 16, 128], BF16)
                wstage = [sb(st, "wstageP%d" % i, [128, 8, 128], F32) for i in range(2)]
                gffn = sb(st, "gffn", [128, DM], F32)
                dma(gffn[:], gbc_d[1], [], ["gffn"])
                wctr[0] = 0
                for cc in range(0, 1024, 128):
                    sl = (cc // 128) % 2
                    dma(wstage[sl][:], w_out_d[:, cc:cc + 128].rearrange("(kc p) n -> p kc n", p=128), [], ["wstageP%d" % sl])
                    vcopy(wout[:, :, cc:cc + 128], wstage[sl][:], ["wstageP%d" % sl], ["wout"], eng=("pool" if sl else "dve"))
                for cc in range(0, 2048, 128):
                    sl = (cc // 128) % 2
                    dma(wstage[sl][:], w_pq_d[:, cc:cc + 128].rearrange("(kc p) n -> p kc n", p=128), [], ["wstageP%d" % sl])
                    vcopy(wpq[:, :, cc:cc + 128], wstage[sl][:], ["wstageP%d" % sl], ["wpq"], eng=("pool" if sl else "dve"))

                ytc = [sb(st, "ytc%d" % i, [128, 8, 512], BF16) for i in range(2)]
                xt = [sb(st, "xtP%d" % i, [128, DM], F32) for i in range(2)]
                x1 = [sb(st, "x1_%d" % i, [128, DM], F32) for i in range(2)]
                hn2f = sb(st, "hn2f", [128, DM], F32)
                hn2b = sb(st, "hn2b", [128, DM], BF16)
                junkb = sb(st, "junkP", [128, DM], BF16)
                hn2T = sb(st, "hn2T", [128, 8, 128], BF16)
                qryT = sb(st, "qryT", [128, 16, 128], BF16)
                sc = sb(st, "sc", [128, 16, 128], F32)
                dma(sc[:], sub_d.rearrange("c k d -> k c d"), [], ["sc"])
                vcopy(qryT[:], sc[:], ["sc"], ["qryT"])
                for c8 in range(2):
                    for i in range(8):
                        tr(ps_tr[:, i, :], qryT[:, c8 * 8 + i, :], ident[:], ["qryT", "ident"], ["ps_tr"])
                    acopy(subT[:, c8 * 8:(c8 + 1) * 8, :], ps_tr[:], ["ps_tr"], ["subT"])
                wk1 = sb(st, "wk1", [128, 128], F32)
                v12 = sb(st, "v12", [128, 8, 2, 16], F32)
                i12 = sb(st, "i12", [128, 8, 2, 16], U32)
                i12f = sb(st, "i12f", [128, 8, 2, 16], F32)
                cand = sb(st, "cand", [128, 8, 256], F32)
                wk2 = sb(st, "wk2", [128, 256], F32)
                ts = sb(st, "ts", [128, 8, 16], F32)
                pos = sb(st, "pos", [128, 8, 16], U32)
                pab = sb(st, "pab", [128, 2, 8, 16], U32)
                pabf = sb(st, "pabf", [128, 2, 8, 16], F32)
                oh = sb(st, "oh", [128, 8, 16, 16], F32)
                e12 = sb(st, "e12", [128, 2, 8, 16], F32)
                ef = sb(st, "ef", [128, 128], F32)
                idx = sb(st, "idx", [128, 128], U32)
                ex = sb(st, "ex", [128, 8, 16], F32)
                gs = sb(st, "gs", [128, 8], F32)
                gate = sb(st, "gate", [128, 128], F32)
                aact = sb(st, "aact", [128, 128], F32)
                wgt = sb(st, "wgt", [128, 128], F32)
                accp = sb(st, "accp", [128, DM], F32)
                ssq = sb(st, "ssqP", [128, 1], F32)
                rt = sb(st, "rtP", [128, 1], F32)
                rs = sb(st, "rsP", [128, 1], F32)
                ub = [sb(st, "ub%d" % i, [128, DM], F32) for i in range(NU)]
                vb = [sb(st, "vb%d" % i, [128, DM], F32) for i in range(NU)]
                yres = ["yscr%d_%d" % (b, c) for c in range(8)]

                for t in range(peer_tiles):
                    sl = t % 2
                    X, X1 = "xtP%d" % sl, "x1_%d" % sl
                    ysl = (t // 4) % 2
                    YT = "ytc%d" % ysl
                    if t % 4 == 0:
                        dma(ytc[ysl][:], yscr_d[b, :, :, t * 128:t * 128 + 512].rearrange("c p n -> p c n"), yres, [YT])
                    dma(xt[sl][:], x_d[b, t * 128:(t + 1) * 128, :], [], [X])
                    tcol = slice((t % 4) * 128, (t % 4 + 1) * 128)
                    for nh in range(2):
                        for kc in range(8):
                            mm(ps_pj[nh][:, :], ytc[ysl][:, kc, tcol], wout[:, kc, nh * 512:(nh + 1) * 512], kc == 0, kc == 7,
                               [YT, "wout"], ["ps_pj%d" % nh])
                    for nh in range(2):
                        c = slice(nh * 512, (nh + 1) * 512)
                        vtt(x1[sl][:, c], ps_pj[nh][:, :], xt[sl][:, c], ALU.add, ["ps_pj%d" % nh, X], [X1])
                    act(junkb[:], x1[sl][:], AF.Square, [X1], ["junkP", "ssqP"], accum_out=ssq[:, 0:1])
                    act(rt[:], ssq[:], AF.Sqrt, ["ssqP", "epsc"], ["rtP"], bias=epsc[:], scale=1.0 / DM)
                    vrecip(rs[:], rt[:], ["rtP"], ["rsP"])
                    vstt(hn2f[:], x1[sl][:], rs[:, 0:1], gffn[:], ALU.mult, ALU.mult, [X1, "rsP", "gffn"], ["hn2f"])
                    acopy(hn2b[:], hn2f[:], ["hn2f"], ["hn2b"])
                    for kc in range(8):
                        tr(ps_tr[:, kc, :], hn2b[:, kc * 128:(kc + 1) * 128], ident[:], ["hn2b", "ident"], ["ps_tr"])
                    acopy(hn2T[:], ps_tr[:], ["ps_tr"], ["hn2T"])
                    for c4 in range(4):
                        q_ = c4 % 2
                        for i in range(4):
                            c = c4 * 4 + i
                            for kc in range(8):
                                mm(ps_s[q_][:, i, :], wpq[:, kc, c * 128:(c + 1) * 128], hn2T[:, kc, :], kc == 0, kc == 7,
                                   ["wpq", "hn2T"], ["ps_s%d" % q_])
                        acopy(qryT[:, c4 * 4:(c4 + 1) * 4, :], ps_s[q_][:], ["ps_s%d" % q_], ["qryT"])
                    for c4 in range(4):
                        q_ = c4 % 2
                        for i in range(4):
                            c = c4 * 4 + i
                            mm(ps_s[q_][:, i, :], qryT[:, c, :], subT[:, c, :], True, True, ["qryT", "subT"], ["ps_s%d" % q_])
                        acopy(sc[:, c4 * 4:(c4 + 1) * 4, :], ps_s[q_][:], ["ps_s%d" % q_], ["sc"])
                    for h in range(8):
                        for hf in range(2):
                            a_ = sc[:, 2 * h + hf, :]
                            S.op("dve", lambda e, a_=a_, h=h, hf=hf: e.max(out=v12[:, h, hf, 0:8], in_=a_), ["sc"], ["v12"])
                            S.op("dve", lambda e, a_=a_, h=h, hf=hf: e.match_replace(out=wk1[:], in_to_replace=v12[:, h, hf, 0:8],
                                                                                   in_values=a_, imm_value=-1e30), ["sc", "v12"], ["wk1"])
                            S.op("dve", lambda e, h=h, hf=hf: e.max(out=v12[:, h, hf, 8:16], in_=wk1[:]), ["wk1"], ["v12"])
                            S.op("dve", lambda e, a_=a_, h=h, hf=hf: e.max_index(out=i12[:, h, hf, 0:8], in_max=v12[:, h, hf, 0:8],
                                                                               in_values=a_), ["sc", "v12"], ["i12"])
                            S.op("dve", lambda e, a_=a_, h=h, hf=hf: e.max_index(out=i12[:, h, hf, 8:16], in_max=v12[:, h, hf, 8:16],
                                                                               in_values=a_), ["sc", "v12"], ["i12"])
                        cv = cand[:, h, :].rearrange("p (a b) -> p a b", a=16)
                        vtt(cv, v12[:, h, 0, :].unsqueeze(2).to_broadcast([128, 16, 16]),
                            v12[:, h, 1, :].unsqueeze(1).to_broadcast([128, 16, 16]), ALU.add, ["v12"], ["cand"])
                        ch = cand[:, h, :]
                        S.op("dve", lambda e, ch=ch, h=h: e.max(out=ts[:, h, 0:8], in_=ch), ["cand"], ["ts"])
                        S.op("dve", lambda e, ch=ch, h=h: e.match_replace(out=wk2[:], in_to_replace=ts[:, h, 0:8], in_values=ch,
                                                                        imm_value=-1e30), ["cand", "ts"], ["wk2"])
                        S.op("dve", lambda e, h=h: e.max(out=ts[:, h, 8:16], in_=wk2[:]), ["wk2"], ["ts"])
                        S.op("dve", lambda e, ch=ch, h=h: e.max_index(out=pos[:, h, 0:8], in_max=ts[:, h, 0:8], in_values=ch),
                             ["cand", "ts"], ["pos"])
                        S.op("dve", lambda e, ch=ch, h=h: e.max_index(out=pos[:, h, 8:16], in_max=ts[:, h, 8:16], in_values=ch),
                             ["cand", "ts"], ["pos"])
                    vts(pab[:, 0], pos[:], 4, None, ALU.logical_shift_right, None, ["pos"], ["pab"])
                    vts(pab[:, 1], pos[:], 15, None, ALU.bitwise_and, None, ["pos"], ["pab"])
                    vcopy(pabf[:], pab[:], ["pab"], ["pabf"])
                    vcopy(i12f[:], i12[:], ["i12"], ["i12f"])
                    for w_ in range(2):
                        vtt(oh[:], pabf[:, w_].unsqueeze(3).to_broadcast([128, 8, 16, 16]),
                            iota16[:].unsqueeze(1).unsqueeze(1).to_broadcast([128, 8, 16, 16]), ALU.is_equal,
                            ["pabf", "iota16"], ["oh"])
                        vtt(oh[:], oh[:], i12f[:, :, w_, :].unsqueeze(2).to_broadcast([128, 8, 16, 16]), ALU.mult,
                            ["oh", "i12f"], ["oh"])
                        S.op("dve", lambda e, w_=w_: e.tensor_reduce(out=e12[:, w_], in_=oh[:], axis=AX.X, op=ALU.add), ["oh"], ["e12"])
                    vstt(ef[:], e12[:, 0].rearrange("p a b -> p (a b)"), 128.0, e12[:, 1].rearrange("p a b -> p (a b)"),
                         ALU.mult, ALU.add, ["e12"], ["ef"])
                    vcopy(idx[:], ef[:], ["ef"], ["idx"])
                    vtt(ex[:], ts[:], ts[:, :, 0:1].to_broadcast([128, 8, 16]), ALU.subtract, ["ts"], ["ex"])
                    act(ex[:], ex[:], AF.Exp, ["ex"], ["ex"])
                    S.op("dve", lambda e: e.tensor_reduce(out=gs[:], in_=ex[:], axis=AX.X, op=ALU.add), ["ex"], ["gs"])
                    vrecip(gs[:], gs[:], ["gs"], ["gs"])
                    vtt(gate[:].rearrange("p (a b) -> p a b", a=8), ex[:], gs[:].unsqueeze(2).to_broadcast([128, 8, 16]), ALU.mult,
                        ["ex", "gs"], ["gate"])
                    for j in range(128):
                        u_ = j % NU
                        S.op("pool", lambda e, u_=u_, j=j: e.indirect_dma_start(
                            out=ub[u_][:], out_offset=None, in_=pu_d,
                            in_offset=bass.IndirectOffsetOnAxis(ap=idx[:, j:j + 1], axis=0)),
                            ["idx"], ["ub%d" % u_], dma=True)
                        S.op("dve", lambda e, u_=u_, j=j: e.scalar_tensor_tensor(
                            out=junkb[:], in0=ub[u_][:], scalar=1.0, in1=hn2f[:], op0=ALU.mult, op1=ALU.mult,
                            accum_out=aact[:, j:j + 1]), ["ub%d" % u_, "hn2f"], ["junkP", "aact"])
                    act(wgt[:], aact[:], AF.Gelu, ["aact"], ["wgt"])
                    vtt(wgt[:], wgt[:], gate[:], ALU.mult, ["wgt", "gate"], ["wgt"])
                    for j in range(128):
                        u_ = j % NU
                        S.op("pool", lambda e, u_=u_, j=j: e.indirect_dma_start(
                            out=vb[u_][:], out_offset=None, in_=pv_d,
                            in_offset=bass.IndirectOffsetOnAxis(ap=idx[:, j:j + 1], axis=0)),
                            ["idx"], ["vb%d" % u_], dma=True)
                        if j == 0:
                            vts(accp[:], vb[u_][:], wgt[:, 0:1], None, ALU.mult, None, ["vb%d" % u_, "wgt"], ["accp"])
                        else:
                            vstt(accp[:], vb[u_][:], wgt[:, j:j + 1], accp[:], ALU.mult, ALU.add, ["vb%d" % u_, "wgt", "accp"], ["accp"])
                    vtt(x1[sl][:], x1[sl][:], accp[:], ALU.add, [X1, "accp"], [X1])
                    dma(out_d[b, t * 128:(t + 1) * 128, :], x1[sl][:], [X1], ["out%d" % sl], semkey=("dma", "out%d" % sl))
                S.barrier()

        for b in range(nseq):
            with ExitStack() as seqst:
                hnT = sb(seqst, "hnT", [128, 8, SEQ], BF16)
                with ExitStack() as st:
                    rms_rows_to_T(st, lambda t: x_d[b, t * 128:(t + 1) * 128, :], NT, 0, hnT, "hnT", "A")
                    if debug:
                        dma(dbg_hnT, hnT[:], ["hnT"], ["dbg_hnT"])
                    S.barrier()
                with ExitStack() as st:
                    wstage = [sb(st, "wstage%d" % i, [128, 8, 128], F32) for i in range(2)]
                    wq = sb(st, "wq", [128, 8, 128], BF16)
                    wk = sb(st, "wk", [128, 8, 128], BF16)
                    wv = sb(st, "wv", [128, 8, 128], BF16)
                    sqb = sb(st, "sqb", [128, 512], BF16)
                    rtb = sb(st, "rtb", [128, 512], F32)
                    rsb = sb(st, "rsb", [128, 512], F32)
                    scr = (sqb, rtb, rsb)
                    acc = sb(st, "acc", [128, 2, SEQ], F32)
                    ssqacc = sb(st, "ssqacc", [128, SEQ], F32)
                    o16 = [sb(st, "o16_%d" % i, [128, SEQ], BF16) for i in range(3)]
                    pT = [sb(st, "pT%d" % i, [128, 4, 128], BF16) for i in range(2)]
                    ybuf = o16
                    Vd = sb(st, "Vd", [128, NT, 128], BF16)

                    with ExitStack() as gst:
                      if "dil" in stages:
                        qT = sb(gst, "qT", [128, SEQ], BF16)
                        kT = sb(gst, "kT", [128, SEQ], BF16)
                        dbias = sb(gst, "dbias", [128, 24, 128], BF16)
                        for p in range(min(3, npair)):
                            dma(dbias[:], cd["c_dilbias"][p].rearrange("k (a q) -> k a q", a=24), [], ["dbias"])
                            load_w(st, wstage, wq, "wq", w_in_d, 0 + p * 128, 128)
                            load_w(st, wstage, wk, "wk", w_in_d, 384 + p * 128, 128)
                            load_w(st, wstage, wv, "wv", w_in_d, 768 + p * 128, 128)
                            proj_norm(wq, "wq", 0, 128, hnT, "hnT", SEQ, gcols[:, 0:1], blockones, "blockones",
                                      lambda tc, N: qT[:, tc * N:(tc + 1) * N], "qT", scr)
                            proj_norm(wk, "wk", 0, 128, hnT, "hnT", SEQ, gcols[:, 1:2], blockones, "blockones",
                                      lambda tc, N: kT[:, tc * N:(tc + 1) * N], "kT", scr)
                            for di, d in enumerate(DIL):
                                nblk = NT // d
                                tsl = []
                                for j in range(NT):
                                    r, n = divmod(j, nblk)
                                    base = d * 128 * n + r
                                    tsl.append(slice(base, base + 127 * d + 1, d))
                                proj_v(wv, "wv", 0, hnT, "hnT", tsl, Vd, "Vd")
                                for j in range(NT):
                                    r, n = divmod(j, nblk)
                                    sl = j % 2
                                    have_prev = n > 0
                                    qk = []
                                    for h in range(2):
                                        hp = 64 * h
                                        for jj in range(2):
                                            if jj == 0 and not have_prev:
                                                continue
                                            tk = tsl[j - 1] if jj == 0 else tsl[j]
                                            bi = ((h * 3 + di) * 2 + 0) * 2 + jj
                                            bl = ((h * 3 + di) * 2 + 1) * 2 + jj
                                            qk.append((h * 2 + jj, kT[hp:hp + 64, tk], qT[hp:hp + 64, tsl[j]], ["kT", "qT"],
                                                       [(ident[:], dbias[:, bi, :], ["ident", "dbias"]),
                                                        (ident[:], dbias[:, bl, :], ["ident", "dbias"])]))
                                    PT = attn_tile(sl, qk, None, None, None)
                                    Po = "ps_o%d" % sl
                                    for h in range(2):
                                        hp = 64 * h
                                        jjs = [1] if not have_prev else [0, 1]
                                        for jj in jjs:
                                            jk = j - 1 if jj == 0 else j
                                            mm(ps_o[sl][hp:hp + 64, 0, :], Vd[:, jk, hp:hp + 64], pT[sl][:, h * 2 + jj, :],
                                               jj == jjs[0], jj == 1, ["Vd", PT], [Po])
                                        for jj in jjs:
                                            mm(ps_o[sl][hp:hp + 64, 1, :], ones[:, 0:64], pT[sl][:, h * 2 + jj, :],
                                               jj == jjs[0], jj == 1, ["ones", PT], [Po])
                                    av = acc[:, :, tsl[j]]
                                    if di == 0:
                                        vcopy(av, ps_o[sl][:, 0:2, :], [Po], ["acc"])
                                    else:
                                        vtt(av, ps_o[sl][:, 0:2, :], av, ALU.add, [Po, "acc"], ["acc"])
                            finalize_pair(acc, o16[p], "o16_%d" % p, ssqacc, p == 0, scr)
                        group_norm_store(b, o16, min(3, npair), 384, ssqacc, 0, ybuf, scr)
                        S.barrier()

                    with ExitStack() as gst:
                      if "moba" in stages:
                        qa = sb(gst, "qa", [128, SEQ], BF16)
                        ka = sb(gst, "ka", [128, SEQ], BF16)
                        km32 = sb(gst, "km32", [64, 16], F32)
                        kmT = sb(gst, "kmT", [64, 16], BF16)
                        gm = sb(gst, "gm", [128, 16], F32)
                        mx8 = sb(gst, "mx8", [128, 8], F32)
                        pen = sb(gst, "pen", [128, 16], F32)
                        penb = sb(gst, "penb", [128, 16], BF16)
                        dma(ka[64:86, :], cd["c_moba_k"], [], ["ka_c"])
                        tsl = [slice(j * 128, (j + 1) * 128) for j in range(NT)]
                        for p in range(min(3, npair)):
                            load_w(st, wstage, wq, "wq", w_in_d, 1152 + p * 128, 128)
                            load_w(st, wstage, wk, "wk", w_in_d, 1536 + p * 128, 128)
                            load_w(st, wstage, wv, "wv", w_in_d, 1920 + p * 128, 128)
                            proj_v(wv, "wv", 0, hnT, "hnT", tsl, Vd, "Vd")
                            for h in range(2):
                                H = 2 * p + h
                                hp = 64 * h
                                dma(qa[80:86, :], cd["c_moba_q"][H], [], ["qa_c"])
                                proj_norm(wq, "wq", hp, 64, hnT, "hnT", SEQ, gcols[0:64, 2:3], ones, "ones",
                                          lambda tc, N: qa[0:64, tc * N:(tc + 1) * N], "qa", scr)
                                proj_norm(wk, "wk", hp, 64, hnT, "hnT", SEQ, gcols[0:64, 3:4], ones, "ones",
                                          lambda tc, N: ka[0:64, tc * N:(tc + 1) * N], "ka", scr)
                                S.op("dve", lambda e: e.tensor_reduce(out=km32[:, :], in_=ka[0:64, :].rearrange("p (a b) -> p a b", a=16),
                                                                      axis=AX.X, op=ALU.add), ["ka"], ["km32"])
                                vts(kmT[:, :], km32[:, :], 1.0 / 256, None, ALU.mult, None, ["km32"], ["kmT"])
                                for t in range(NT):
                                    mm(ps_ss[:, 0:16], qa[0:64, tsl[t]], kmT[:, :], True, True, ["qa", "kmT"], ["ps_ss"])
                                    vtt(gm[:, :], ps_ss[:, 0:16], pm2[:, t // 2, :], ALU.add, ["ps_ss", "pm2"], ["gm"])
                                    S.op("dve", lambda e: e.max(out=mx8[:, :], in_=gm[:, :]), ["gm"], ["mx8"])
                                    vts(pen[:, :], gm[:, :], mx8[:, 3:4], None, ALU.is_ge, None, ["gm", "mx8"], ["pen"])
                                    vts(penb[:, :], pen[:, :], -NEGM, NEGM, ALU.mult, ALU.add, ["pen"], ["penb"])
                                    mm(ps_ss[64:80, 128:256], penb[:, :], ident[:], True, True, ["penb", "ident"], ["ps_ss"])
                                    acopy(qa[64:80, tsl[t]], ps_ss[64:80, 128:256], ["ps_ss"], ["qa_p"])
                                cnt = 0
                                for t in range(NT):
                                    osl = t % 2
                                    Po = "ps_o%d" % osl
                                    for b0 in range(0, t + 1, 4):
                                        nb = min(4, t + 1 - b0)
                                        sl = cnt % 2
                                        cnt += 1
                                        qk = []
                                        for i in range(nb):
                                            kt = b0 + i
                                            ex = [(ident[:], tri[:], ["ident", "tri"])] if kt == t else []
                                            qk.append((i, ka[0:86, tsl[kt]], qa[0:86, tsl[t]], ["ka", "ka_c", "qa", "qa_c", "qa_p"], ex))
                                        PT = attn_tile(sl, qk, None, None, None, exp_bias=bcol[:, H:H + 1])
                                        for i in range(nb):
                                            kt = b0 + i
                                            mm(ps_o[osl][hp:hp + 64, 0, :], Vd[:, kt, hp:hp + 64], pT[sl][:, i, :], kt == 0, kt == t,
                                               ["Vd", PT], [Po], skip=True)
                                            mm(ps_o[osl][hp:hp + 64, 1, :], ones[:, 0:64], pT[sl][:, i, :], False, kt == t,
                                               ["ones", PT], [Po], skip=True)
                                    acopy(acc[hp:hp + 64, :, tsl[t]], ps_o[osl][hp:hp + 64, 0:2, :], [Po], ["acc"])
                            finalize_pair(acc, o16[p], "o16_%d" % p, ssqacc, p == 0, scr)
                        group_norm_store(b, o16, min(3, npair), 384, ssqacc, 3, ybuf, scr)
                        S.barrier()

                    with ExitStack() as gst:
                      if "mem" in stages:
                        memT = sb(gst, "memT", [128, 8, 256], BF16)
                        import os
                        MEMCUT = int(os.environ.get("MEMCUT", "9"))
                        rms_rows_to_T(gst, lambda t: mem_d[b, t * 128:(t + 1) * 128, :], 2, 2, memT, "memT", "M")
                        qT = sb(gst, "qTm", [128, SEQ], BF16)
                        kmem = sb(gst, "kmem", [128, 256], BF16)
                        Vm = sb(gst, "Vm", [128, 2, 128], BF16)
                        tsl = [slice(j * 128, (j + 1) * 128) for j in range(NT)]
                        for p in range(min(2, npair) if MEMCUT > 1 else 0):
                            load_w(st, wstage, wq, "wq", w_in_d, 2304 + p * 128, 128)
                            load_w(st, wstage, wk, "wk", w_mkv_d, 0 + p * 128, 128)
                            load_w(st, wstage, wv, "wv", w_mkv_d, 256 + p * 128, 128)
                            proj_norm(wq, "wq", 0, 128, hnT, "hnT", SEQ, gcols[:, 4:5], blockones, "blockones",
                                      lambda tc, N: qT[:, tc * N:(tc + 1) * N], "qTm", scr)
                            if MEMCUT <= 2:
                                continue
                            proj_norm(wk, "wk", 0, 128, memT, "memT", 256, gcols[:, 5:6], blockones, "blockones",
                                      lambda tc, N: kmem[:, tc * N:(tc + 1) * N], "kmem", scr)
                            if MEMCUT <= 3:
                                continue
                            if os.environ.get("PVA"):
                                proj_v(wv, "wv", 0, memT, "memT", tsl[0:2], Vd, "Vd")
                            elif os.environ.get("PVB"):
                                proj_v(wv, "wv", 0, hnT, "hnT", tsl[0:2], Vm, "Vm")
                            else:
                                proj_v(wv, "wv", 0, memT, "memT", tsl[0:2], Vm, "Vm")
                            if MEMCUT <= 4:
                                continue
                            for t in range(NT):
                                sl = t % 2
                                Po = "ps_o%d" % sl
                                qk = []
                                for h in range(2):
                                    hp = 64 * h
                                    for jj in range(2):
                                        qk.append((h * 2 + jj, kmem[hp:hp + 64, tsl[jj]], qT[hp:hp + 64, tsl[t]], ["kmem", "qTm"], []))
                                PT = attn_tile(sl, qk, None, None, None)
                                for h in range(2):
                                    hp = 64 * h
                                    for jj in range(2):
                                        mm(ps_o[sl][hp:hp + 64, 0, :], Vm[:, jj, hp:hp + 64], pT[sl][:, h * 2 + jj, :], jj == 0, jj == 1,
                                           ["Vm", PT], [Po])
                                    for jj in range(2):
                                        mm(ps_o[sl][hp:hp + 64, 1, :], ones[:, 0:64], pT[sl][:, h * 2 + jj, :], jj == 0, jj == 1,
                                           ["ones", PT], [Po])
                                if os.environ.get("MEMEVAC") == "dve":
                                    vcopy(acc[:, :, tsl[t]], ps_o[sl][:, 0:2, :], [Po], ["acc"])
                                else:
                                    acopy(acc[:, :, tsl[t]], ps_o[sl][:, 0:2, :], [Po], ["acc"])
                            finalize_pair(acc, o16[p], "o16_%d" % p, ssqacc, p == 0, scr)
                        group_norm_store(b, o16, min(2, npair), 256, ssqacc, 6, ybuf, scr)
                        S.barrier()
            S.barrier()
            if do_peer:
                peer(b)
        S.barrier()
        S.emit()
    return nc, cst


def _host_inputs(inputs, cst, nseq, core, do_peer=True):
    f = lambda a: np.ascontiguousarray(np.asarray(a, dtype=np.float32))
    m = {}
    m["x"] = f(inputs["x"][core * nseq:(core + 1) * nseq])
    m["mem"] = f(inputs["mem"][core * nseq:(core + 1) * nseq])
    m["w_in"] = f(inputs["w_in"][0])
    m["w_mem_kv"] = f(inputs["w_mem_kv"][0])
    m["w_out"] = f(inputs["w_out"][0])
    m["w_peer_q"] = f(inputs["w_peer_q"][0])
    s1 = np.asarray(inputs["peer_subkeys_1"][0]); s2 = np.asarray(inputs["peer_subkeys_2"][0])
    m["subkeys"] = f(np.stack([s1, s2], axis=1).reshape(16, 128, 128))
    m["peer_u"] = f(inputs["peer_u"][0] if do_peer else inputs["peer_u"][0][:128])
    m["peer_v"] = f(inputs["peer_v"][0] if do_peer else inputs["peer_v"][0][:128])
    gb = np.stack([np.broadcast_to(np.asarray(inputs[k][0]), (128, DM)) for k in ("g_mix", "g_ffn", "g_memtok")])
    m["g_bc"] = f(gb)
    cols = []
    for k in ("qg_dil", "kg_dil", "qg_moba", "kg_moba", "qg_mem", "kg_mem"):
        cols.append(np.tile(np.asarray(inputs[k][0]), 2))
    og = np.concatenate([np.asarray(inputs["og_dil"][0]), np.asarray(inputs["og_moba"][0]), np.asarray(inputs["og_mem"][0])])
    for c in range(8):
        cols.append(og[c * 128:(c + 1) * 128])
    m["g_cols"] = f(np.stack(cols, axis=1))
    for k, v in cst.items():
        m[k] = v
    return m


_CACHE = {}


def kernel(**inputs):
    nseq = 2
    if "prog" not in _CACHE:
        _CACHE["prog"] = build_program(nseq=nseq)
    nc, cst = _CACHE["prog"]
    in_maps = [_host_inputs(inputs, cst, nseq, c) for c in range(NCORES)]
    res = run_bass_kernel_spmd(nc, in_maps, core_ids=list(range(NCORES)))
    out = np.concatenate([np.asarray(r["out"]) for r in res.results], axis=0)
    return out.astype(np.float32)
```

```python
import math
from contextlib import ExitStack

import numpy as np
import ml_dtypes

import concourse.bass as bass
import concourse.mybir as mybir
from concourse.bass_utils import run_bass_kernel_spmd

F32 = mybir.dt.float32
BF16 = mybir.dt.bfloat16
U32 = mybir.dt.uint32
I32 = mybir.dt.int32
AF = mybir.ActivationFunctionType
ALU = mybir.AluOpType
AX = mybir.AxisListType

NCORES = 8
SEQ = 4096
DM = 1024
NT = SEQ // 128
SCALE = 0.125
EPS = 1e-6
NEGM = -30000.0
SEM_LIMIT = 32000
DIL = (1, 4, 16)


class Sched:
    ENGS = ("pe", "act", "dve", "pool", "sp")

    def __init__(self, nc, stack):
        self.nc = nc
        self.stack = stack
        self.ops = {e: [] for e in self.ENGS}
        self.cnt = {}
        self.sems = {}
        self.last_w = {}
        self.readers = {}
        self.seen = {e: {} for e in self.ENGS}
        self.nops = 0

    def _sem(self, key, epoch):
        k = (key, epoch)
        if k not in self.sems:
            self.sems[k] = self.stack.enter_context(self.nc.semaphore("s%d" % len(self.sems)))
        return self.sems[k]

    def _bump(self, key, amt):
        ep, v = self.cnt.get(key, (0, 0))
        if v + amt > SEM_LIMIT:
            ep, v = ep + 1, 0
        v += amt
        self.cnt[key] = (ep, v)
        return (key, ep, v)

    def op(self, engine, fn, reads=(), writes=(), dma=False, semkey=None, pe_self=False):
        deps = set()
        for r in reads:
            if r in self.last_w:
                deps.add(self.last_w[r])
            if r.startswith("ps_"):
                for ev in self.readers.get(r, ()):
                    deps.add(ev)
        for w in writes:
            if w in self.last_w:
                deps.add(self.last_w[w])
            for ev in self.readers.get(w, ()):
                deps.add(ev)
        if dma:
            key = semkey if semkey is not None else ("dma", (writes[0] if writes else reads[0]))
            ev = self._bump(key, 16)
            amt = 16
        else:
            key = engine
            ev = self._bump(key, 1)
            amt = 1
        seen = self.seen[engine]
        best = {}
        for (k, ep, v) in deps:
            if k == "pe" and engine == "pe" and not dma and not pe_self:
                continue
            if best.get(k, (-1, -1)) < (ep, v):
                best[k] = (ep, v)
        waits = []
        for k, (ep, v) in best.items():
            if seen.get(k, (-1, -1)) >= (ep, v):
                continue
            seen[k] = (ep, v)
            waits.append((self._sem(k, ep), v))
        self.ops[engine].append((waits, fn, self._sem(ev[0], ev[1]), amt))
        for w in writes:
            self.last_w[w] = ev
            self.readers[w] = []
        for r in reads:
            if r not in writes:
                self.readers.setdefault(r, []).append(ev)
        self.nops += 1
        return ev

    def barrier(self, engines=None):
        for e in (engines or self.ENGS):
            waits = []
            seen = self.seen[e]
            for key, (ep, v) in self.cnt.items():
                if seen.get(key, (-1, -1)) >= (ep, v):
                    continue
                seen[key] = (ep, v)
                waits.append((self._sem(key, ep), v))
            if waits:
                self.ops[e].append((waits, None, None, 0))

    def emit(self):
        nc = self.nc
        with nc.Block() as block:
            def run(engname):
                def body(eng):
                    for waits, fn, sem, amt in self.ops[engname]:
                        for s, v in waits:
                            eng.wait_ge(s, v)
                        if fn is not None:
                            fn(eng).then_inc(sem, amt)
                return body
            block.tensor(run("pe"))
            block.scalar(run("act"))
            block.vector(run("dve"))
            block.gpsimd(run("pool"))
            block.sync(run("sp"))


def _bf(a):
    return np.asarray(a, dtype=np.float32).astype(ml_dtypes.bfloat16)


def _split3(a):
    a = np.asarray(a, dtype=np.float64)
    h = _bf(a)
    r = a - h.astype(np.float64)
    l = _bf(r)
    r2 = r - l.astype(np.float64)
    l2 = _bf(r2)
    return h, l, l2


def _constants():
    c = {}
    slopes = 2.0 ** (-8.0 * np.arange(1, 13, dtype=np.float64) / 12.0)
    sl_dil, sl_moba = slopes[0::2], slopes[1::2]
    c["c_ident"] = _bf(np.eye(128))
    bo = np.zeros((128, 128)); bo[:64, :64] = 1; bo[64:, 64:] = 1
    c["c_blockones"] = _bf(bo)
    c["c_ones"] = _bf(np.ones((128, 128)))
    kl = np.arange(128)[:, None]
    ql = np.arange(128)[None, :]
    tab = np.zeros((3, 128, 2, 3, 2, 2, 128), dtype=ml_dtypes.bfloat16)
    for p in range(3):
        for h in range(2):
            for di, d in enumerate(DIL):
                for jj in range(2):
                    delta = ql - kl + (128 if jj == 0 else 0)
                    valid = (delta >= 0) & (delta <= 128)
                    b = np.where(valid, -sl_dil[2 * p + h] * d * delta / SCALE, NEGM)
                    hi = _bf(b)
                    lo = _bf(b - hi.astype(np.float64))
                    tab[p, :, h, di, 0, jj, :] = hi
                    tab[p, :, h, di, 1, jj, :] = lo
    c["c_dilbias"] = tab.reshape(3, 128, 24 * 128)
    c["c_tri"] = _bf(np.where(kl <= ql, 0.0, NEGM))
    pm = np.zeros((16, 16))
    for npast in range(16):
        for n in range(16):
            pm[npast, n] = 0.0 if n < npast else (1e30 if n == npast else -1e30)
    c["c_pm2"] = np.broadcast_to(pm.reshape(1, 256), (128, 256)).astype(np.float32).copy()
    tok = np.arange(SEQ)
    mq = np.zeros((6, 6, SEQ), dtype=ml_dtypes.bfloat16)
    for h in range(6):
        cc = 1024.0 * sl_moba[h]
        a, b_, c_ = _split3(np.full(SEQ, cc))
        mq[h, 0], mq[h, 1], mq[h, 2] = a, b_, c_
        a, b_, c_ = _split3(-cc * (tok // 128))
        mq[h, 3], mq[h, 4], mq[h, 5] = a, b_, c_
    c["c_moba_q"] = mq
    mk = np.zeros((22, SEQ))
    for n in range(16):
        mk[n, n * 256:(n + 1) * 256] = 1.0
    mk[16:19] = (tok // 128)[None, :]
    mk[19:22] = 1.0
    c["c_moba_k"] = _bf(mk)
    c["c_moba_bcol"] = (sl_moba[None, :] * (np.arange(128)[:, None] - 64.0)).astype(np.float32)
    c["c_iota16"] = np.broadcast_to(np.arange(16, dtype=np.float32)[None, :], (128, 16)).copy()
    return c


def build_program(nseq=2, debug=False, do_peer=True, stages=('dil', 'moba', 'mem'), npair=3, peer_tiles=NT):
    nc = bass.Bass("TRN2", target_bir_lowering=False)

    def din(name, shape, dt):
        return nc.dram_tensor(name, list(shape), dt, kind="ExternalInput").ap()

    x_d = din("x", [nseq, SEQ, DM], F32)
    mem_d = din("mem", [nseq, 256, DM], F32)
    w_in_d = din("w_in", [DM, 2560], F32)
    w_mkv_d = din("w_mem_kv", [DM, 512], F32)
    w_out_d = din("w_out", [DM, DM], F32)
    w_pq_d = din("w_peer_q", [DM, 2048], F32)
    sub_d = din("subkeys", [16, 128, 128], F32)
    pu_d = din("peer_u", [16384 if do_peer else 128, DM], F32)
    pv_d = din("peer_v", [16384 if do_peer else 128, DM], F32)
    gbc_d = din("g_bc", [3, 128, DM], F32)
    gcol_d = din("g_cols", [128, 14], F32)
    cst = _constants()
    cd = {}
    for k, v in cst.items():
        dt = BF16 if v.dtype == ml_dtypes.bfloat16 else F32
        cd[k] = din(k, v.shape, dt)
    out_d = nc.dram_tensor("out", [nseq, SEQ, DM], F32, kind="ExternalOutput").ap()
    yscr_d = nc.dram_tensor("yscr", [nseq, 8, 128, SEQ], BF16,
                            kind=("ExternalOutput" if debug else "Internal")).ap()
    if debug:
        dbg_hnT = nc.dram_tensor("dbg_hnT", [128, 8, SEQ], BF16, kind="ExternalOutput").ap()
    NEXP = 16384 if do_peer else 128
    u16_d = nc.dram_tensor("u16", [NEXP, DM], BF16, kind="Internal").ap()
    v16_d = nc.dram_tensor("v16", [NEXP, DM], BF16, kind="Internal").ap()

    with ExitStack() as top:
        S = Sched(nc, top)

        uid = [0]

        def sb(st, name, shape, dt):
            uid[0] += 1
            return st.enter_context(nc.sbuf_tensor("%s_%d" % (name, uid[0]), list(shape), dt))

        def ps(st, name, shape, dt):
            return st.enter_context(nc.psum_tensor(name, list(shape), dt))

        def mm(out, lhsT, rhs, start, stop, reads, writes, skip=False, pe_self=False):
            S.op("pe", lambda e: e.matmul(out, lhsT=lhsT, rhs=rhs, start=start, stop=stop, skip_group_check=skip), reads, writes,
                 pe_self=pe_self)

        def tr(out, in_, ident, reads, writes):
            S.op("pe", lambda e: e.transpose(out=out, in_=in_, identity=ident), reads, writes)

        def act(out, in_, func, reads, writes, bias=None, scale=None, accum_out=None):
            kw = {}
            if bias is not None:
                kw["bias"] = bias
            if scale is not None:
                kw["scale"] = scale
            if accum_out is not None:
                kw["accum_out"] = accum_out
            S.op("act", lambda e: e.activation(out=out, in_=in_, func=func, **kw), reads, writes)

        def acopy(out, in_, reads, writes):
            S.op("act", lambda e: e.copy(out=out, in_=in_), reads, writes)

        def vcopy(out, in_, reads, writes, eng="dve"):
            S.op(eng, lambda e: e.tensor_copy(out=out, in_=in_), reads, writes)

        def vtt(out, in0, in1, op, reads, writes, eng="dve"):
            S.op(eng, lambda e: e.tensor_tensor(out=out, in0=in0, in1=in1, op=op), reads, writes)

        def vts(out, in0, s1, s2, op0, op1, reads, writes, eng="dve"):
            if op1 is None:
                S.op(eng, lambda e: e.tensor_scalar(out=out, in0=in0, scalar1=s1, scalar2=None, op0=op0), reads, writes)
            else:
                S.op(eng, lambda e: e.tensor_scalar(out=out, in0=in0, scalar1=s1, scalar2=s2, op0=op0, op1=op1), reads, writes)

        def vstt(out, in0, scalar, in1, op0, op1, reads, writes):
            S.op("dve", lambda e: e.scalar_tensor_tensor(out=out, in0=in0, scalar=scalar, in1=in1, op0=op0, op1=op1),
                 reads, writes)

        def vrecip(out, in_, reads, writes):
            S.op("dve", lambda e: e.reciprocal(out=out, in_=in_), reads, writes)

        def dma(out, in_, reads, writes, eng="sp", semkey=None):
            S.op(eng, lambda e: e.dma_start(out=out, in_=in_), reads, writes, dma=True, semkey=semkey)

        ident = sb(top, "ident", [128, 128], BF16)
        blockones = sb(top, "blockones", [128, 128], BF16)
        ones = sb(top, "ones", [128, 128], BF16)
        gcols = sb(top, "gcols", [128, 14], F32)
        epsc = sb(top, "epsc", [128, 1], F32)
        tri = sb(top, "tri", [128, 128], BF16)
        pm2 = sb(top, "pm2", [128, 16, 16], F32)
        bcol = sb(top, "bcol", [128, 6], F32)
        iota16 = sb(top, "iota16", [128, 16], F32)
        dma(ident[:], cd["c_ident"], [], ["ident"])
        dma(blockones[:], cd["c_blockones"], [], ["blockones"])
        dma(ones[:], cd["c_ones"], [], ["ones"])
        dma(gcols[:], gcol_d, [], ["gcols"])
        dma(tri[:], cd["c_tri"], [], ["tri"])
        dma(pm2[:], cd["c_pm2"].rearrange("p (a b) -> p a b", a=16), [], ["pm2"])
        dma(bcol[:], cd["c_moba_bcol"], [], ["bcol"])
        dma(iota16[:], cd["c_iota16"], [], ["iota16"])
        S.op("dve", lambda e: e.memset(epsc[:], EPS), [], ["epsc"])

        ps_tr = ps(top, "ps_tr", [128, 8, 128], BF16)
        ps_pj = [ps(top, "ps_pj%d" % i, [128, 512], F32) for i in range(2)]
        ps_ss = ps(top, "ps_ss", [128, 512], F32)
        ps_s = [ps(top, "ps_s%d" % i, [128, 4, 128], F32) for i in range(2)]
        ps_o = [ps(top, "ps_o%d" % i, [128, 4, 128], F32) for i in range(2)]

        def rms_rows_to_T(st, src_dram_tile_fn, ntiles, gslot, dstT, dst_name, tag):
            gbc = sb(st, "gbc" + tag, [128, DM], F32)
            dma(gbc[:], gbc_d[gslot], [], ["gbc" + tag])
            xt = [sb(st, "xt%s%d" % (tag, i), [128, DM], F32) for i in range(2)]
            hn = [sb(st, "hn%s%d" % (tag, i), [128, DM], BF16) for i in range(2)]
            junk = sb(st, "junk" + tag, [128, DM], BF16)
            ssq = sb(st, "ssq" + tag, [128, 2], F32)
            rt = sb(st, "rt" + tag, [128, 2], F32)
            rs = sb(st, "rs" + tag, [128, 2], F32)
            for t in range(ntiles):
                sl = t % 2
                X, H = "xt%s%d" % (tag, sl), "hn%s%d" % (tag, sl)
                dma(xt[sl][:], src_dram_tile_fn(t), [], [X])
                act(junk[:], xt[sl][:], AF.Square, [X], ["junk" + tag, "ssq%s%d" % (tag, sl)], accum_out=ssq[:, sl:sl + 1])
                act(rt[:, sl:sl + 1], ssq[:, sl:sl + 1], AF.Sqrt, ["ssq%s%d" % (tag, sl), "epsc"], ["rt%s%d" % (tag, sl)],
                    bias=epsc[:], scale=1.0 / DM)
                vrecip(rs[:, sl:sl + 1], rt[:, sl:sl + 1], ["rt%s%d" % (tag, sl)], ["rs%s%d" % (tag, sl)])
                vstt(hn[sl][:], xt[sl][:], rs[:, sl:sl + 1], gbc[:], ALU.mult, ALU.mult,
                     [X, "rs%s%d" % (tag, sl), "gbc" + tag], [H])
                for kc in range(8):
                    tr(ps_tr[:, kc, :], hn[sl][:, kc * 128:(kc + 1) * 128], ident[:], [H, "ident"], ["ps_tr"])
                acopy(dstT[:, :, t * 128:(t + 1) * 128], ps_tr[:], ["ps_tr"], [dst_name])

        wctr = [0]
        pend = [None]

        def load_w(st_w, wstage, wdst, wname, w_dram, c0, ncols):
            for cc in range(0, ncols, 128):
                sl = wctr[0] % 2
                wctr[0] += 1
                dma(wstage[sl][:], w_dram[:, c0 + cc:c0 + cc + 128].rearrange("(kc p) n -> p kc n", p=128),
                    [], ["wstage%d" % sl])
                eng = "pool" if (wctr[0] % 2) else "dve"
                vcopy(wdst[:, :, cc:cc + 128], wstage[sl][:], ["wstage%d" % sl], [wname], eng=eng)

        pjctr = [0]

        def proj_norm(wbf, wname, m0, M, src, srcname, ntok, gcol, blk, blkname, dst_fn, dstname, scr):
            sqb, rtb, rsb = scr
            N = min(512, ntok)
            for tc in range(ntok // N):
                sl = pjctr[0] % 2
                pjctr[0] += 1
                P = "ps_pj%d" % sl
                for kc in range(8):
                    mm(ps_pj[sl][0:M, 0:N], wbf[:, kc, m0:m0 + M], src[:, kc, tc * N:(tc + 1) * N], kc == 0, kc == 7,
                       [wname, srcname], [P])
                act(sqb[0:M, 0:N], ps_pj[sl][0:M, 0:N], AF.Square, [P], ["sqb"])
                mm(ps_ss[0:M, 0:N], blk[0:M, 0:M], sqb[0:M, 0:N], True, True, ["sqb", blkname], ["ps_ss"])
                act(rtb[0:M, 0:N], ps_ss[0:M, 0:N], AF.Sqrt, ["ps_ss", "epsc"], ["rtb"], bias=epsc[0:M, :], scale=1.0 / 64)
                vrecip(rsb[0:M, 0:N], rtb[0:M, 0:N], ["rtb"], ["rsb"])
                vstt(dst_fn(tc, N), ps_pj[sl][0:M, 0:N], gcol, rsb[0:M, 0:N], ALU.mult, ALU.mult,
                     [P, "rsb", "gcols"], [dstname])

        def proj_v(wbf, wname, c0, src, srcname, tok_slices, dst, dstname):
            nt = len(tok_slices)
            for jb in range(0, nt, 4):
                sl = pjctr[0] % 2
                pjctr[0] += 1
                P = "ps_pj%d" % sl
                nb = min(4, nt - jb)
                for i in range(nb):
                    for kc in range(8):
                        mm(ps_pj[sl][:, i * 128:(i + 1) * 128], src[:, kc, tok_slices[jb + i]], wbf[:, kc, c0:c0 + 128],
                           kc == 0, kc == 7, [wname, srcname], [P])
                o = dst[:, jb:jb + nb, :]
                i_ = ps_pj[sl][:, 0:nb * 128].rearrange("p (a b) -> p a b", a=nb)
                import os
                if (jb // 4) % 2 == 0 and not os.environ.get("PVDVE"):
                    acopy(o, i_, [P], [dstname])
                else:
                    vcopy(o, i_, [P], [dstname])

        def finalize_pair(acc, o16p, o16name, ssqacc, first, scr):
            sqb, rtb, rsb = scr
            for tc in range(8):
                c = slice(tc * 512, (tc + 1) * 512)
                vrecip(rsb[:, :], acc[:, 1, c], ["acc"], ["rsb"])
                vtt(rtb[:, :], acc[:, 0, c], rsb[:, :], ALU.mult, ["acc", "rsb"], ["rtb"])
                act(sqb[:, :], rtb[:, :], AF.Square, ["rtb"], ["sqb"])
                vcopy(o16p[:, c], rtb[:, :], ["rtb"], [o16name], eng="pool")
                mm(ps_ss[:, :], ones[:], sqb[:, :], True, True, ["sqb", "ones"], ["ps_ss"])
                if first:
                    vcopy(ssqacc[:, c], ps_ss[:, :], ["ps_ss"], ["ssqacc"])
                else:
                    vtt(ssqacc[:, c], ps_ss[:, :], ssqacc[:, c], ALU.add, ["ps_ss", "ssqacc"], ["ssqacc"])

        def group_norm_store(b, o16, npairs, nfeat, ssqacc, chunk0, ybuf, scr):
            sqb, rtb, rsb = scr
            for tc in range(8):
                c = slice(tc * 512, (tc + 1) * 512)
                act(rtb[:, :], ssqacc[:, c], AF.Sqrt, ["ssqacc", "epsc"], ["rtb"], bias=epsc[:], scale=1.0 / nfeat)
                vrecip(ssqacc[:, c], rtb[:, :], ["rtb"], ["ssqacc"])
            for p in range(npairs):
                Y = "o16_%d" % p
                for tc in range(8):
                    c = slice(tc * 512, (tc + 1) * 512)
                    vstt(o16[p][:, c], o16[p][:, c], gcols[:, 6 + chunk0 + p:7 + chunk0 + p], ssqacc[:, c],
                         ALU.mult, ALU.mult, [Y, "ssqacc", "gcols"], [Y])
                dma(yscr_d[b, chunk0 + p], o16[p][:], [Y], ["yscr%d_%d" % (b, chunk0 + p)])

        def attn_tile(sl, qk_list, v_list, acc_view, acc_first, exp_bias=None, evac_eng="dve", acc_part=None):
            Ps = "ps_s%d" % sl
            nslots = 0
            prev_base = None
            for (si, kT_ap, qT_ap, knames, extras) in qk_list:
                nslots = max(nslots, si + 1)
                base = kT_ap.base_partition()
                mm(ps_s[sl][:, si, :], kT_ap, qT_ap, True, len(extras) == 0, knames, [Ps],
                   pe_self=(prev_base is not None and base != prev_base))
                prev_base = base
                for ei, (l_, r_, nm) in enumerate(extras):
                    mm(ps_s[sl][:, si, :], l_, r_, False, ei == len(extras) - 1, nm, [Ps])
            PT = "pT%d" % sl
            used = sorted(q[0] for q in qk_list)
            if used == list(range(nslots)):
                ssel = slice(0, nslots)
            else:
                step = used[1] - used[0] if len(used) > 1 else 1
                assert used == list(range(used[0], used[-1] + 1, step))
                ssel = slice(used[0], used[-1] + 1, step)
            if exp_bias is None:
                act(pT[sl][:, ssel, :], ps_s[sl][:, ssel, :], AF.Exp, [Ps], [PT], scale=SCALE)
            else:
                act(pT[sl][:, ssel, :], ps_s[sl][:, ssel, :], AF.Exp, [Ps, "bcol"], [PT], scale=SCALE, bias=exp_bias)
            return PT


        NU = 8

        def peer(b):
            with ExitStack() as st:
                wout = sb(st, "wout", [128, 8, 1024], BF16)
                wpq = sb(st, "wpq", [128, 8, 2048], BF16)
                subT = sb(st, "subT", [128, 16, 128], BF16)
                wstage = [sb(st, "wstageP%d" % i, [128, 8, 128], F32) for i in range(2)]
                gffn = sb(st, "gffn", [128, DM], F32)
                dma(gffn[:], gbc_d[1], [], ["gffn"])
                wctr[0] = 0
                for cc in range(0, 1024, 128):
                    sl = (cc // 128) % 2
                    dma(wstage[sl][:], w_out_d[:, cc:cc + 128].rearrange("(kc p) n -> p kc n", p=128), [], ["wstageP%d" % sl])
                    vcopy(wout[:, :, cc:cc + 128], wstage[sl][:], ["wstageP%d" % sl], ["wout"], eng=("pool" if sl else "dve"))
                for cc in range(0, 2048, 128):
                    sl = (cc // 128) % 2
                    dma(wstage[sl][:], w_pq_d[:, cc:cc + 128].rearrange("(kc p) n -> p kc n", p=128), [], ["wstageP%d" % sl])
                    vcopy(wpq[:, :, cc:cc + 128], wstage[sl][:], ["wstageP%d" % sl], ["wpq"], eng=("pool" if sl else "dve"))

                ytc = [sb(st, "ytc%d" % i, [128, 8, 512], BF16) for i in range(2)]
                xt = [sb(st, "xtP%d" % i, [128, DM], F32) for i in range(2)]
                x1 = [sb(st, "x1_%d" % i, [128, DM], F32) for i in range(2)]
                hn2b = sb(st, "hn2b", [128, DM], BF16)
                junkb = sb(st, "junkP", [128, DM], BF16)
                hn2T = sb(st, "hn2T", [128, 8, 128], BF16)
                qryT = sb(st, "qryT", [128, 16, 128], BF16)
                sc = sb(st, "sc", [128, 16, 128], F32)
                dma(sc[:], sub_d.rearrange("c k d -> k c d"), [], ["sc"])
                vcopy(qryT[:], sc[:], ["sc"], ["qryT"])
                for c8 in range(2):
                    for i in range(8):
                        tr(ps_tr[:, i, :], qryT[:, c8 * 8 + i, :], ident[:], ["qryT", "ident"], ["ps_tr"])
                    acopy(subT[:, c8 * 8:(c8 + 1) * 8, :], ps_tr[:], ["ps_tr"], ["subT"])
                wk1 = sb(st, "wk1", [128, 128], F32)
                v12 = sb(st, "v12", [128, 8, 2, 16], F32)
                i12 = sb(st, "i12", [128, 8, 2, 16], U32)
                i12f = sb(st, "i12f", [128, 8, 2, 16], F32)
                cand = sb(st, "cand", [128, 8, 256], F32)
                wk2 = sb(st, "wk2", [128, 256], F32)
                ts = sb(st, "ts", [128, 8, 16], F32)
                pos = sb(st, "pos", [128, 8, 16], U32)
                pab = sb(st, "pab", [128, 2, 8, 16], U32)
                pabf = sb(st, "pabf", [128, 2, 8, 16], F32)
                oh = sb(st, "oh", [128, 8, 16, 16], F32)
                e12 = sb(st, "e12", [128, 2, 8, 16], F32)
                ef = sb(st, "ef", [128, 128], F32)
                idx = sb(st, "idx", [128, 128], U32)
                ex = sb(st, "ex", [128, 8, 16], F32)
                gs = sb(st, "gs", [128, 8], F32)
                gate = sb(st, "gate", [128, 128], F32)
                aact = sb(st, "aact", [128, 128], F32)
                wgt = sb(st, "wgt", [128, 128], F32)
                ssq = sb(st, "ssqP", [128, 1], F32)
                rt = sb(st, "rtP", [128, 1], F32)
                rs = sb(st, "rsP", [128, 1], F32)
                ub = [sb(st, "ub%d" % i, [128, DM], BF16) for i in range(NU)]
                vb = [sb(st, "vb%d" % i, [128, DM], BF16) for i in range(NU)]
                dg = [sb(st, "dg%d" % i, [128, 128], BF16) for i in range(4)]
                yres = ["yscr%d_%d" % (b, c) for c in range(8)]

                for t in range(peer_tiles):
                    sl = t % 2
                    X, X1 = "xtP%d" % sl, "x1_%d" % sl
                    ysl = (t // 4) % 2
                    YT = "ytc%d" % ysl
                    if t % 4 == 0:
                        dma(ytc[ysl][:], yscr_d[b, :, :, t * 128:t * 128 + 512].rearrange("c p n -> p c n"), yres, [YT])
                    dma(xt[sl][:], x_d[b, t * 128:(t + 1) * 128, :], [], [X])
                    tcol = slice((t % 4) * 128, (t % 4 + 1) * 128)
                    for nh in range(2):
                        for kc in range(8):
                            mm(ps_pj[nh][:, :], ytc[ysl][:, kc, tcol], wout[:, kc, nh * 512:(nh + 1) * 512], kc == 0, kc == 7,
                               [YT, "wout"], ["ps_pj%d" % nh])
                    for nh in range(2):
                        c = slice(nh * 512, (nh + 1) * 512)
                        vtt(x1[sl][:, c], ps_pj[nh][:, :], xt[sl][:, c], ALU.add, ["ps_pj%d" % nh, X], [X1])
                    act(junkb[:], x1[sl][:], AF.Square, [X1], ["junkP", "ssqP"], accum_out=ssq[:, 0:1])
                    act(rt[:], ssq[:], AF.Sqrt, ["ssqP", "epsc"], ["rtP"], bias=epsc[:], scale=1.0 / DM)
                    vrecip(rs[:], rt[:], ["rtP"], ["rsP"])
                    vstt(hn2b[:], x1[sl][:], rs[:, 0:1], gffn[:], ALU.mult, ALU.mult, [X1, "rsP", "gffn"], ["hn2b"])
                    for kc in range(8):
                        tr(ps_tr[:, kc, :], hn2b[:, kc * 128:(kc + 1) * 128], ident[:], ["hn2b", "ident"], ["ps_tr"])
                    acopy(hn2T[:], ps_tr[:], ["ps_tr"], ["hn2T"])
                    for c4 in range(4):
                        q_ = c4 % 2
                        for i in range(4):
                            c = c4 * 4 + i
                            for kc in range(8):
                                mm(ps_s[q_][:, i, :], wpq[:, kc, c * 128:(c + 1) * 128], hn2T[:, kc, :], kc == 0, kc == 7,
                                   ["wpq", "hn2T"], ["ps_s%d" % q_])
                        acopy(qryT[:, c4 * 4:(c4 + 1) * 4, :], ps_s[q_][:], ["ps_s%d" % q_], ["qryT"])
                    for c4 in range(4):
                        q_ = c4 % 2
                        for i in range(4):
                            c = c4 * 4 + i
                            mm(ps_s[q_][:, i, :], qryT[:, c, :], subT[:, c, :], True, True, ["qryT", "subT"], ["ps_s%d" % q_])
                        acopy(sc[:, c4 * 4:(c4 + 1) * 4, :], ps_s[q_][:], ["ps_s%d" % q_], ["sc"])
                    for h in range(8):
                        for hf in range(2):
                            a_ = sc[:, 2 * h + hf, :]
                            S.op("dve", lambda e, a_=a_, h=h, hf=hf: e.max(out=v12[:, h, hf, 0:8], in_=a_), ["sc"], ["v12"])
                            S.op("dve", lambda e, a_=a_, h=h, hf=hf: e.match_replace(out=wk1[:], in_to_replace=v12[:, h, hf, 0:8],
                                                                                   in_values=a_, imm_value=-1e30), ["sc", "v12"], ["wk1"])
                            S.op("dve", lambda e, h=h, hf=hf: e.max(out=v12[:, h, hf, 8:16], in_=wk1[:]), ["wk1"], ["v12"])
                            S.op("dve", lambda e, a_=a_, h=h, hf=hf: e.max_index(out=i12[:, h, hf, 0:8], in_max=v12[:, h, hf, 0:8],
                                                                               in_values=a_), ["sc", "v12"], ["i12"])
                            S.op("dve", lambda e, a_=a_, h=h, hf=hf: e.max_index(out=i12[:, h, hf, 8:16], in_max=v12[:, h, hf, 8:16],
                                                                               in_values=a_), ["sc", "v12"], ["i12"])
                        cv = cand[:, h, :].rearrange("p (a b) -> p a b", a=16)
                        vtt(cv, v12[:, h, 0, :].unsqueeze(2).to_broadcast([128, 16, 16]),
                            v12[:, h, 1, :].unsqueeze(1).to_broadcast([128, 16, 16]), ALU.add, ["v12"], ["cand"])
                        ch = cand[:, h, :]
                        S.op("dve", lambda e, ch=ch, h=h: e.max(out=ts[:, h, 0:8], in_=ch), ["cand"], ["ts"])
                        S.op("dve", lambda e, ch=ch, h=h: e.match_replace(out=wk2[:], in_to_replace=ts[:, h, 0:8], in_values=ch,
                                                                        imm_value=-1e30), ["cand", "ts"], ["wk2"])
                        S.op("dve", lambda e, h=h: e.max(out=ts[:, h, 8:16], in_=wk2[:]), ["wk2"], ["ts"])
                        S.op("dve", lambda e, ch=ch, h=h: e.max_index(out=pos[:, h, 0:8], in_max=ts[:, h, 0:8], in_values=ch),
                             ["cand", "ts"], ["pos"])
                        S.op("dve", lambda e, ch=ch, h=h: e.max_index(out=pos[:, h, 8:16], in_max=ts[:, h, 8:16], in_values=ch),
                             ["cand", "ts"], ["pos"])
                    vts(pab[:, 0], pos[:], 4, None, ALU.logical_shift_right, None, ["pos"], ["pab"])
                    vts(pab[:, 1], pos[:], 15, None, ALU.bitwise_and, None, ["pos"], ["pab"])
                    vcopy(pabf[:], pab[:], ["pab"], ["pabf"])
                    vcopy(i12f[:], i12[:], ["i12"], ["i12f"])
                    for w_ in range(2):
                        vtt(oh[:], pabf[:, w_].unsqueeze(3).to_broadcast([128, 8, 16, 16]),
                            iota16[:].unsqueeze(1).unsqueeze(1).to_broadcast([128, 8, 16, 16]), ALU.is_equal,
                            ["pabf", "iota16"], ["oh"])
                        vtt(oh[:], oh[:], i12f[:, :, w_, :].unsqueeze(2).to_broadcast([128, 8, 16, 16]), ALU.mult,
                            ["oh", "i12f"], ["oh"])
                        S.op("dve", lambda e, w_=w_: e.tensor_reduce(out=e12[:, w_], in_=oh[:], axis=AX.X, op=ALU.add), ["oh"], ["e12"])
                    vstt(ef[:], e12[:, 0].rearrange("p a b -> p (a b)"), 128.0, e12[:, 1].rearrange("p a b -> p (a b)"),
                         ALU.mult, ALU.add, ["e12"], ["ef"])
                    vcopy(idx[:], ef[:], ["ef"], ["idx"])
                    vtt(ex[:], ts[:], ts[:, :, 0:1].to_broadcast([128, 8, 16]), ALU.subtract, ["ts"], ["ex"])
                    act(ex[:], ex[:], AF.Exp, ["ex"], ["ex"])
                    S.op("dve", lambda e: e.tensor_reduce(out=gs[:], in_=ex[:], axis=AX.X, op=ALU.add), ["ex"], ["gs"])
                    vrecip(gs[:], gs[:], ["gs"], ["gs"])
                    vtt(gate[:].rearrange("p (a b) -> p a b", a=8), ex[:], gs[:].unsqueeze(2).to_broadcast([128, 8, 16]), ALU.mult,
                        ["ex", "gs"], ["gate"])
                    for j in range(128):
                        u_ = j % NU
                        S.op("pool", lambda e, u_=u_, j=j: e.indirect_dma_start(
                            out=ub[u_][:], out_offset=None, in_=u16_d,
                            in_offset=bass.IndirectOffsetOnAxis(ap=idx[:, j:j + 1], axis=0)),
                            ["idx", "u16"], ["ub%d" % u_], dma=True)
                        S.op("dve", lambda e, u_=u_, j=j: e.scalar_tensor_tensor(
                            out=junkb[:], in0=ub[u_][:], scalar=1.0, in1=hn2b[:], op0=ALU.mult, op1=ALU.mult,
                            accum_out=aact[:, j:j + 1]), ["ub%d" % u_, "hn2b"], ["junkP", "aact"])
                    act(wgt[:], aact[:], AF.Gelu, ["aact"], ["wgt"])
                    vtt(wgt[:], wgt[:], gate[:], ALU.mult, ["wgt", "gate"], ["wgt"])
                    for j in range(128):
                        u_ = j % NU
                        d_ = j % 4
                        S.op("pool", lambda e, u_=u_, j=j: e.indirect_dma_start(
                            out=vb[u_][:], out_offset=None, in_=v16_d,
                            in_offset=bass.IndirectOffsetOnAxis(ap=idx[:, j:j + 1], axis=0)),
                            ["idx", "v16"], ["vb%d" % u_], dma=True)
                        vts(dg[d_][:], ident[:], wgt[:, j:j + 1], None, ALU.mult, None, ["ident", "wgt"], ["dg%d" % d_])
                        for nh in range(2):
                            mm(ps_pj[nh][:, :], dg[d_][:], vb[u_][:, nh * 512:(nh + 1) * 512], j == 0, j == 127,
                               ["dg%d" % d_, "vb%d" % u_], ["ps_pj%d" % nh])
                    for nh in range(2):
                        c = slice(nh * 512, (nh + 1) * 512)
                        vtt(x1[sl][:, c], ps_pj[nh][:, :], x1[sl][:, c], ALU.add, ["ps_pj%d" % nh, X1], [X1])
                    dma(out_d[b, t * 128:(t + 1) * 128, :], x1[sl][:], [X1], ["out%d" % sl], semkey=("dma", "out%d" % sl))
                S.barrier()

        if do_peer:
            with ExitStack() as cst_:
                stg = [sb(cst_, "cstg%d" % i, [128, 2, DM], F32) for i in range(3)]
                cbf = [sb(cst_, "cbf%d" % i, [128, 2, DM], BF16) for i in range(3)]
                k_ = 0
                for (src_, dst_, nm_) in ((pu_d, u16_d, "u16"), (pv_d, v16_d, "v16")):
                    for ch in range(NEXP // 256):
                        s_ = k_ % 3
                        k_ += 1
                        rows = slice(ch * 256, (ch + 1) * 256)
                        dma(stg[s_][:], src_[rows, :].rearrange("(p r) d -> p r d", r=2), [], ["cstg%d" % s_])
                        if s_ == 0:
                            vcopy(cbf[s_][:], stg[s_][:], ["cstg%d" % s_], ["cbf%d" % s_])
                        elif s_ == 1:
                            acopy(cbf[s_][:], stg[s_][:], ["cstg%d" % s_], ["cbf%d" % s_])
                        else:
                            vcopy(cbf[s_][:], stg[s_][:], ["cstg%d" % s_], ["cbf%d" % s_], eng="pool")
                        dma(dst_[rows, :].rearrange("(p r) d -> p r d", r=2), cbf[s_][:], ["cbf%d" % s_], [nm_],
                            semkey=("dma", "cbfo%d" % s_))
                S.barrier()

        for b in range(nseq):
            with ExitStack() as seqst:
                hnT = sb(seqst, "hnT", [128, 8, SEQ], BF16)
                with ExitStack() as st:
                    rms_rows_to_T(st, lambda t: x_d[b, t * 128:(t + 1) * 128, :], NT, 0, hnT, "hnT", "A")
                    if debug:
                        dma(dbg_hnT, hnT[:], ["hnT"], ["dbg_hnT"])
                    S.barrier()
                with ExitStack() as st:
                    wstage = [sb(st, "wstage%d" % i, [128, 8, 128], F32) for i in range(2)]
                    wq = sb(st, "wq", [128, 8, 128], BF16)
                    wk = sb(st, "wk", [128, 8, 128], BF16)
                    wv = sb(st, "wv", [128, 8, 128], BF16)
                    sqb = sb(st, "sqb", [128, 512], BF16)
                    rtb = sb(st, "rtb", [128, 512], F32)
                    rsb = sb(st, "rsb", [128, 512], F32)
                    scr = (sqb, rtb, rsb)
                    acc = sb(st, "acc", [128, 2, SEQ], F32)
                    ssqacc = sb(st, "ssqacc", [128, SEQ], F32)
                    o16 = [sb(st, "o16_%d" % i, [128, SEQ], BF16) for i in range(3)]
                    pT = [sb(st, "pT%d" % i, [128, 4, 128], BF16) for i in range(2)]
                    ybuf = o16
                    Vd = sb(st, "Vd", [128, NT, 128], BF16)

                    with ExitStack() as gst:
                      if "dil" in stages:
                        qT = sb(gst, "qT", [128, SEQ], BF16)
                        kT = sb(gst, "kT", [128, SEQ], BF16)
                        dbias = sb(gst, "dbias", [128, 24, 128], BF16)
                        for p in range(min(3, npair)):
                            dma(dbias[:], cd["c_dilbias"][p].rearrange("k (a q) -> k a q", a=24), [], ["dbias"])
                            load_w(st, wstage, wq, "wq", w_in_d, 0 + p * 128, 128)
                            load_w(st, wstage, wk, "wk", w_in_d, 384 + p * 128, 128)
                            load_w(st, wstage, wv, "wv", w_in_d, 768 + p * 128, 128)
                            proj_norm(wq, "wq", 0, 128, hnT, "hnT", SEQ, gcols[:, 0:1], blockones, "blockones",
                                      lambda tc, N: qT[:, tc * N:(tc + 1) * N], "qT", scr)
                            proj_norm(wk, "wk", 0, 128, hnT, "hnT", SEQ, gcols[:, 1:2], blockones, "blockones",
                                      lambda tc, N: kT[:, tc * N:(tc + 1) * N], "kT", scr)
                            for di, d in enumerate(DIL):
                                nblk = NT // d
                                tsl = []
                                for j in range(NT):
                                    r, n = divmod(j, nblk)
                                    base = d * 128 * n + r
                                    tsl.append(slice(base, base + 127 * d + 1, d))
                                proj_v(wv, "wv", 0, hnT, "hnT", tsl, Vd, "Vd")
                                for j in range(NT):
                                    r, n = divmod(j, nblk)
                                    sl = j % 2
                                    have_prev = n > 0
                                    qk = []
                                    for h in range(2):
                                        hp = 64 * h
                                        for jj in range(2):
                                            if jj == 0 and not have_prev:
                                                continue
                                            tk = tsl[j - 1] if jj == 0 else tsl[j]
                                            bi = ((h * 3 + di) * 2 + 0) * 2 + jj
                                            bl = ((h * 3 + di) * 2 + 1) * 2 + jj
                                            qk.append((h * 2 + jj, kT[hp:hp + 64, tk], qT[hp:hp + 64, tsl[j]], ["kT", "qT"],
                                                       [(ident[:], dbias[:, bi, :], ["ident", "dbias"]),
                                                        (ident[:], dbias[:, bl, :], ["ident", "dbias"])]))
                                    PT = attn_tile(sl, qk, None, None, None)

                                    def pv_dil(j=j, sl=sl, have_prev=have_prev, PT=PT, di=di, tsl=tsl):
                                        Po = "ps_o%d" % sl
                                        for h in range(2):
                                            hp = 64 * h
                                            jjs = [1] if not have_prev else [0, 1]
                                            for jj in jjs:
                                                jk = j - 1 if jj == 0 else j
                                                mm(ps_o[sl][hp:hp + 64, 0, :], Vd[:, jk, hp:hp + 64], pT[sl][:, h * 2 + jj, :],
                                                   jj == jjs[0], jj == 1, ["Vd", PT], [Po])
                                            for jj in jjs:
                                                mm(ps_o[sl][hp:hp + 64, 1, :], ones[:, 0:64], pT[sl][:, h * 2 + jj, :],
                                                   jj == jjs[0], jj == 1, ["ones", PT], [Po])
                                        av = acc[:, :, tsl[j]]
                                        if di == 0:
                                            vcopy(av, ps_o[sl][:, 0:2, :], [Po], ["acc"])
                                        else:
                                            vtt(av, ps_o[sl][:, 0:2, :], av, ALU.add, [Po, "acc"], ["acc"])
                                    if pend[0] is not None:
                                        pend[0]()
                                    pend[0] = pv_dil
                                if pend[0] is not None:
                                    pend[0]()
                                    pend[0] = None
                            finalize_pair(acc, o16[p], "o16_%d" % p, ssqacc, p == 0, scr)
                        group_norm_store(b, o16, min(3, npair), 384, ssqacc, 0, ybuf, scr)
                        S.barrier()

                    with ExitStack() as gst:
                      if "moba" in stages:
                        qa = sb(gst, "qa", [128, SEQ], BF16)
                        ka = sb(gst, "ka", [128, SEQ], BF16)
                        km32 = sb(gst, "km32", [64, 16], F32)
                        kmT = sb(gst, "kmT", [64, 16], BF16)
                        gm = sb(gst, "gm", [128, 16], F32)
                        mx8 = sb(gst, "mx8", [128, 8], F32)
                        pen = sb(gst, "pen", [128, 16], F32)
                        penb = sb(gst, "penb", [128, 16], BF16)
                        dma(ka[64:86, :], cd["c_moba_k"], [], ["ka_c"])
                        tsl = [slice(j * 128, (j + 1) * 128) for j in range(NT)]
                        for p in range(min(3, npair)):
                            load_w(st, wstage, wq, "wq", w_in_d, 1152 + p * 128, 128)
                            load_w(st, wstage, wk, "wk", w_in_d, 1536 + p * 128, 128)
                            load_w(st, wstage, wv, "wv", w_in_d, 1920 + p * 128, 128)
                            proj_v(wv, "wv", 0, hnT, "hnT", tsl, Vd, "Vd")
                            for h in range(2):
                                H = 2 * p + h
                                hp = 64 * h
                                dma(qa[80:86, :], cd["c_moba_q"][H], [], ["qa_c"])
                                proj_norm(wq, "wq", hp, 64, hnT, "hnT", SEQ, gcols[0:64, 2:3], ones, "ones",
                                          lambda tc, N: qa[0:64, tc * N:(tc + 1) * N], "qa", scr)
                                proj_norm(wk, "wk", hp, 64, hnT, "hnT", SEQ, gcols[0:64, 3:4], ones, "ones",
                                          lambda tc, N: ka[0:64, tc * N:(tc + 1) * N], "ka", scr)
                                S.op("dve", lambda e: e.tensor_reduce(out=km32[:, :], in_=ka[0:64, :].rearrange("p (a b) -> p a b", a=16),
                                                                      axis=AX.X, op=ALU.add), ["ka"], ["km32"])
                                vts(kmT[:, :], km32[:, :], 1.0 / 256, None, ALU.mult, None, ["km32"], ["kmT"])
                                for t in range(NT):
                                    mm(ps_ss[:, 0:16], qa[0:64, tsl[t]], kmT[:, :], True, True, ["qa", "kmT"], ["ps_ss"])
                                    vtt(gm[:, :], ps_ss[:, 0:16], pm2[:, t // 2, :], ALU.add, ["ps_ss", "pm2"], ["gm"])
                                    S.op("dve", lambda e: e.max(out=mx8[:, :], in_=gm[:, :]), ["gm"], ["mx8"])
                                    vts(pen[:, :], gm[:, :], mx8[:, 3:4], None, ALU.is_ge, None, ["gm", "mx8"], ["pen"])
                                    vts(penb[:, :], pen[:, :], -NEGM, NEGM, ALU.mult, ALU.add, ["pen"], ["penb"])
                                    mm(ps_ss[64:80, 128:256], penb[:, :], ident[:], True, True, ["penb", "ident"], ["ps_ss"])
                                    acopy(qa[64:80, tsl[t]], ps_ss[64:80, 128:256], ["ps_ss"], ["qa_p"])
                                cnt = 0
                                for t in range(NT):
                                    osl = t % 2
                                    Po = "ps_o%d" % osl
                                    for b0 in range(0, t + 1, 4):
                                        nb = min(4, t + 1 - b0)
                                        sl = cnt % 2
                                        cnt += 1
                                        qk = []
                                        for i in range(nb):
                                            kt = b0 + i
                                            ex = [(ident[:], tri[:], ["ident", "tri"])] if kt == t else []
                                            qk.append((i, ka[0:86, tsl[kt]], qa[0:86, tsl[t]], ["ka", "ka_c", "qa", "qa_c", "qa_p"], ex))
                                        PT = attn_tile(sl, qk, None, None, None, exp_bias=bcol[:, H:H + 1])

                                        def pv_moba(t=t, b0=b0, nb=nb, sl=sl, osl=osl, Po=Po, PT=PT, hp=hp, tsl=tsl):
                                            for i in range(nb):
                                                kt = b0 + i
                                                mm(ps_o[osl][hp:hp + 64, 0, :], Vd[:, kt, hp:hp + 64], pT[sl][:, i, :], kt == 0, kt == t,
                                                   ["Vd", PT], [Po], skip=True)
                                                mm(ps_o[osl][hp:hp + 64, 1, :], ones[:, 0:64], pT[sl][:, i, :], False, kt == t,
                                                   ["ones", PT], [Po], skip=True)
                                            if b0 + nb == t + 1:
                                                acopy(acc[hp:hp + 64, :, tsl[t]], ps_o[osl][hp:hp + 64, 0:2, :], [Po], ["acc"])
                                        if pend[0] is not None:
                                            pend[0]()
                                        pend[0] = pv_moba
                                if pend[0] is not None:
                                    pend[0]()
                                    pend[0] = None
                            finalize_pair(acc, o16[p], "o16_%d" % p, ssqacc, p == 0, scr)
                        group_norm_store(b, o16, min(3, npair), 384, ssqacc, 3, ybuf, scr)
                        S.barrier()

                    with ExitStack() as gst:
                      if "mem" in stages:
                        memT = sb(gst, "memT", [128, 8, 256], BF16)
                        import os
                        MEMCUT = int(os.environ.get("MEMCUT", "9"))
                        rms_rows_to_T(gst, lambda t: mem_d[b, t * 128:(t + 1) * 128, :], 2, 2, memT, "memT", "M")
                        qT = sb(gst, "qTm", [128, SEQ], BF16)
                        kmem = sb(gst, "kmem", [128, 256], BF16)
                        Vm = sb(gst, "Vm", [128, 2, 128], BF16)
                        tsl = [slice(j * 128, (j + 1) * 128) for j in range(NT)]
                        for p in range(min(2, npair) if MEMCUT > 1 else 0):
                            load_w(st, wstage, wq, "wq", w_in_d, 2304 + p * 128, 128)
                            load_w(st, wstage, wk, "wk", w_mkv_d, 0 + p * 128, 128)
                            load_w(st, wstage, wv, "wv", w_mkv_d, 256 + p * 128, 128)
                            proj_norm(wq, "wq", 0, 128, hnT, "hnT", SEQ, gcols[:, 4:5], blockones, "blockones",
                                      lambda tc, N: qT[:, tc * N:(tc + 1) * N], "qTm", scr)
                            if MEMCUT <= 2:
                                continue
                            proj_norm(wk, "wk", 0, 128, memT, "memT", 256, gcols[:, 5:6], blockones, "blockones",
                                      lambda tc, N: kmem[:, tc * N:(tc + 1) * N], "kmem", scr)
                            if MEMCUT <= 3:
                                continue
                            if os.environ.get("PVA"):
                                proj_v(wv, "wv", 0, memT, "memT", tsl[0:2], Vd, "Vd")
                            elif os.environ.get("PVB"):
                                proj_v(wv, "wv", 0, hnT, "hnT", tsl[0:2], Vm, "Vm")
                            else:
                                proj_v(wv, "wv", 0, memT, "memT", tsl[0:2], Vm, "Vm")
                            if MEMCUT <= 4:
                                continue
                            for t in range(NT):
                                sl = t % 2
                                Po = "ps_o%d" % sl
                                qk = []
                                for h in range(2):
                                    hp = 64 * h
                                    for jj in range(2):
                                        qk.append((h * 2 + jj, kmem[hp:hp + 64, tsl[jj]], qT[hp:hp + 64, tsl[t]], ["kmem", "qTm"], []))
                                PT = attn_tile(sl, qk, None, None, None)

                                def pv_mem(t=t, sl=sl, Po=Po, PT=PT, tsl=tsl):
                                    for h in range(2):
                                        hp = 64 * h
                                        for jj in range(2):
                                            mm(ps_o[sl][hp:hp + 64, 0, :], Vm[:, jj, hp:hp + 64], pT[sl][:, h * 2 + jj, :], jj == 0, jj == 1,
                                               ["Vm", PT], [Po])
                                        for jj in range(2):
                                            mm(ps_o[sl][hp:hp + 64, 1, :], ones[:, 0:64], pT[sl][:, h * 2 + jj, :], jj == 0, jj == 1,
                                               ["ones", PT], [Po])
                                    vcopy(acc[:, :, tsl[t]], ps_o[sl][:, 0:2, :], [Po], ["acc"])
                                if pend[0] is not None:
                                    pend[0]()
                                pend[0] = pv_mem
                            if pend[0] is not None:
                                pend[0]()
                                pend[0] = None
                            finalize_pair(acc, o16[p], "o16_%d" % p, ssqacc, p == 0, scr)
                        group_norm_store(b, o16, min(2, npair), 256, ssqacc, 6, ybuf, scr)
                        S.barrier()
            S.barrier()
            if do_peer:
                peer(b)
        S.barrier()
        S.emit()
    return nc, cst


def _host_inputs(inputs, cst, nseq, core, do_peer=True):
    f = lambda a: np.ascontiguousarray(np.asarray(a, dtype=np.float32))
    m = {}
    m["x"] = f(inputs["x"][core * nseq:(core + 1) * nseq])
    m["mem"] = f(inputs["mem"][core * nseq:(core + 1) * nseq])
    m["w_in"] = f(inputs["w_in"][0])
    m["w_mem_kv"] = f(inputs["w_mem_kv"][0])
    m["w_out"] = f(inputs["w_out"][0])
    m["w_peer_q"] = f(inputs["w_peer_q"][0])
    s1 = np.asarray(inputs["peer_subkeys_1"][0]); s2 = np.asarray(inputs["peer_subkeys_2"][0])
    m["subkeys"] = f(np.stack([s1, s2], axis=1).reshape(16, 128, 128))
    m["peer_u"] = f(inputs["peer_u"][0] if do_peer else inputs["peer_u"][0][:128])
    m["peer_v"] = f(inputs["peer_v"][0] if do_peer else inputs["peer_v"][0][:128])
    gb = np.stack([np.broadcast_to(np.asarray(inputs[k][0]), (128, DM)) for k in ("g_mix", "g_ffn", "g_memtok")])
    m["g_bc"] = f(gb)
    cols = []
    for k in ("qg_dil", "kg_dil", "qg_moba", "kg_moba", "qg_mem", "kg_mem"):
        cols.append(np.tile(np.asarray(inputs[k][0]), 2))
    og = np.concatenate([np.asarray(inputs["og_dil"][0]), np.asarray(inputs["og_moba"][0]), np.asarray(inputs["og_mem"][0])])
    for c in range(8):
        cols.append(og[c * 128:(c + 1) * 128])
    m["g_cols"] = f(np.stack(cols, axis=1))
    for k, v in cst.items():
        m[k] = v
    return m


_CACHE = {}


def kernel(**inputs):
    nseq = 2
    if "prog" not in _CACHE:
        _CACHE["prog"] = build_program(nseq=nseq)
    nc, cst = _CACHE["prog"]
    in_maps = [_host_inputs(inputs, cst, nseq, c) for c in range(NCORES)]
    res = run_bass_kernel_spmd(nc, in_maps, core_ids=list(range(NCORES)))
    out = np.concatenate([np.asarray(r["out"]) for r in res.results], axis=0)
    return out.astype(np.float32)
```

```python
import math
from contextlib import ExitStack

import numpy as np
import ml_dtypes

import concourse.bass as bass
import concourse.mybir as mybir
from concourse.bass_utils import run_bass_kernel_spmd

F32 = mybir.dt.float32
BF16 = mybir.dt.bfloat16
U32 = mybir.dt.uint32
I32 = mybir.dt.int32
AF = mybir.ActivationFunctionType
ALU = mybir.AluOpType
AX = mybir.AxisListType

NCORES = 8
SEQ = 4096
DM = 1024
NT = SEQ // 128
SCALE = 0.125
EPS = 1e-6
NEGM = -30000.0
SEM_LIMIT = 32000
DIL = (1, 4, 16)


class Sched:
    ENGS = ("pe", "act", "dve", "pool", "sp")

    def __init__(self, nc, stack):
        self.nc = nc
        self.stack = stack
        self.ops = {e: [] for e in self.ENGS}
        self.cnt = {}
        self.sems = {}
        self.last_w = {}
        self.readers = {}
        self.seen = {e: {} for e in self.ENGS}
        self.nops = 0

    def _sem(self, key, epoch):
        k = (key, epoch)
        if k not in self.sems:
            self.sems[k] = self.stack.enter_context(self.nc.semaphore("s%d" % len(self.sems)))
        return self.sems[k]

    def _bump(self, key, amt):
        ep, v = self.cnt.get(key, (0, 0))
        if v + amt > SEM_LIMIT:
            ep, v = ep + 1, 0
        v += amt
        self.cnt[key] = (ep, v)
        return (key, ep, v)

    def op(self, engine, fn, reads=(), writes=(), dma=False, semkey=None, pe_self=False):
        deps = set()
        for r in reads:
            if r in self.last_w:
                deps.add(self.last_w[r])
            if r.startswith("ps_"):
                for ev in self.readers.get(r, ()):
                    deps.add(ev)
        for w in writes:
            if w in self.last_w:
                deps.add(self.last_w[w])
            for ev in self.readers.get(w, ()):
                deps.add(ev)
        if dma:
            key = semkey if semkey is not None else ("dma", (writes[0] if writes else reads[0]))
            ev = self._bump(key, 16)
            amt = 16
        else:
            key = engine
            ev = self._bump(key, 1)
            amt = 1
        seen = self.seen[engine]
        best = {}
        for (k, ep, v) in deps:
            if k == "pe" and engine == "pe" and not dma and not pe_self:
                continue
            if best.get(k, (-1, -1)) < (ep, v):
                best[k] = (ep, v)
        waits = []
        for k, (ep, v) in best.items():
            if seen.get(k, (-1, -1)) >= (ep, v):
                continue
            seen[k] = (ep, v)
            waits.append((self._sem(k, ep), v))
        self.ops[engine].append((waits, fn, self._sem(ev[0], ev[1]), amt))
        for w in writes:
            self.last_w[w] = ev
            self.readers[w] = []
        for r in reads:
            if r not in writes:
                self.readers.setdefault(r, []).append(ev)
        self.nops += 1
        return ev

    def barrier(self, engines=None):
        for e in (engines or self.ENGS):
            waits = []
            seen = self.seen[e]
            for key, (ep, v) in self.cnt.items():
                if seen.get(key, (-1, -1)) >= (ep, v):
                    continue
                seen[key] = (ep, v)
                waits.append((self._sem(key, ep), v))
            if waits:
                self.ops[e].append((waits, None, None, 0))

    def emit(self):
        nc = self.nc
        with nc.Block() as block:
            def run(engname):
                def body(eng):
                    for waits, fn, sem, amt in self.ops[engname]:
                        for s, v in waits:
                            eng.wait_ge(s, v)
                        if fn is not None:
                            fn(eng).then_inc(sem, amt)
                return body
            block.tensor(run("pe"))
            block.scalar(run("act"))
            block.vector(run("dve"))
            block.gpsimd(run("pool"))
            block.sync(run("sp"))


def _bf(a):
    return np.asarray(a, dtype=np.float32).astype(ml_dtypes.bfloat16)


def _split3(a):
    a = np.asarray(a, dtype=np.float64)
    h = _bf(a)
    r = a - h.astype(np.float64)
    l = _bf(r)
    r2 = r - l.astype(np.float64)
    l2 = _bf(r2)
    return h, l, l2


def _constants():
    c = {}
    slopes = 2.0 ** (-8.0 * np.arange(1, 13, dtype=np.float64) / 12.0)
    sl_dil, sl_moba = slopes[0::2], slopes[1::2]
    c["c_ident"] = _bf(np.eye(128))
    bo = np.zeros((128, 128)); bo[:64, :64] = 1; bo[64:, 64:] = 1
    c["c_blockones"] = _bf(bo)
    c["c_ones"] = _bf(np.ones((128, 128)))
    kl = np.arange(128)[:, None]
    ql = np.arange(128)[None, :]
    tab = np.zeros((3, 128, 2, 3, 2, 2, 128), dtype=ml_dtypes.bfloat16)
    for p in range(3):
        for h in range(2):
            for di, d in enumerate(DIL):
                for jj in range(2):
                    delta = ql - kl + (128 if jj == 0 else 0)
                    valid = (delta >= 0) & (delta <= 128)
                    b = np.where(valid, -sl_dil[2 * p + h] * d * delta / SCALE, NEGM)
                    hi = _bf(b)
                    lo = _bf(b - hi.astype(np.float64))
                    tab[p, :, h, di, 0, jj, :] = hi
                    tab[p, :, h, di, 1, jj, :] = lo
    c["c_dilbias"] = tab.reshape(3, 128, 24 * 128)
    c["c_tri"] = _bf(np.where(kl <= ql, 0.0, NEGM))
    pm = np.zeros((16, 16))
    for npast in range(16):
        for n in range(16):
            pm[npast, n] = 0.0 if n < npast else (1e30 if n == npast else -1e30)
    c["c_pm2"] = np.broadcast_to(pm.reshape(1, 256), (128, 256)).astype(np.float32).copy()
    tok = np.arange(SEQ)
    mq = np.zeros((6, 6, SEQ), dtype=ml_dtypes.bfloat16)
    for h in range(6):
        cc = 1024.0 * sl_moba[h]
        a, b_, c_ = _split3(np.full(SEQ, cc))
        mq[h, 0], mq[h, 1], mq[h, 2] = a, b_, c_
        a, b_, c_ = _split3(-cc * (tok // 128))
        mq[h, 3], mq[h, 4], mq[h, 5] = a, b_, c_
    c["c_moba_q"] = mq
    mk = np.zeros((22, SEQ))
    for n in range(16):
        mk[n, n * 256:(n + 1) * 256] = 1.0
    mk[16:19] = (tok // 128)[None, :]
    mk[19:22] = 1.0
    c["c_moba_k"] = _bf(mk)
    c["c_moba_bcol"] = (sl_moba[None, :] * (np.arange(128)[:, None] - 64.0)).astype(np.float32)
    c["c_iota16"] = np.broadcast_to(np.arange(16, dtype=np.float32)[None, :], (128, 16)).copy()
    return c


def build_program(nseq=2, debug=False, do_peer=True, stages=('dil', 'moba', 'mem'), npair=3, peer_tiles=NT):
    nc = bass.Bass("TRN2", target_bir_lowering=False)

    def din(name, shape, dt):
        return nc.dram_tensor(name, list(shape), dt, kind="ExternalInput").ap()

    x_d = din("x", [nseq, SEQ, DM], F32)
    mem_d = din("mem", [nseq, 256, DM], F32)
    w_in_d = din("w_in", [DM, 2560], F32)
    w_mkv_d = din("w_mem_kv", [DM, 512], F32)
    w_out_d = din("w_out", [DM, DM], F32)
    w_pq_d = din("w_peer_q", [DM, 2048], F32)
    sub_d = din("subkeys", [16, 128, 128], F32)
    pu_d = din("peer_u", [16384 if do_peer else 128, DM], F32)
    pv_d = din("peer_v", [16384 if do_peer else 128, DM], F32)
    gbc_d = din("g_bc", [3, 128, DM], F32)
    gcol_d = din("g_cols", [128, 14], F32)
    cst = _constants()
    cd = {}
    for k, v in cst.items():
        dt = BF16 if v.dtype == ml_dtypes.bfloat16 else F32
        cd[k] = din(k, v.shape, dt)
    out_d = nc.dram_tensor("out", [nseq, SEQ, DM], F32, kind="ExternalOutput").ap()
    yscr_d = nc.dram_tensor("yscr", [nseq, 8, 128, SEQ], BF16,
                            kind=("ExternalOutput" if debug else "Internal")).ap()
    if debug:
        dbg_hnT = nc.dram_tensor("dbg_hnT", [128, 8, SEQ], BF16, kind="ExternalOutput").ap()
    NEXP = 16384 if do_peer else 128
    u16_d = nc.dram_tensor("u16", [NEXP, DM], BF16, kind="Internal").ap()
    v16_d = nc.dram_tensor("v16", [NEXP, DM], BF16, kind="Internal").ap()

    with ExitStack() as top:
        S = Sched(nc, top)

        uid = [0]

        def sb(st, name, shape, dt):
            uid[0] += 1
            return st.enter_context(nc.sbuf_tensor("%s_%d" % (name, uid[0]), list(shape), dt))

        def ps(st, name, shape, dt):
            return st.enter_context(nc.psum_tensor(name, list(shape), dt))

        def mm(out, lhsT, rhs, start, stop, reads, writes, skip=False, pe_self=False):
            S.op("pe", lambda e: e.matmul(out, lhsT=lhsT, rhs=rhs, start=start, stop=stop, skip_group_check=skip), reads, writes,
                 pe_self=pe_self)

        def tr(out, in_, ident, reads, writes):
            S.op("pe", lambda e: e.transpose(out=out, in_=in_, identity=ident), reads, writes)

        def act(out, in_, func, reads, writes, bias=None, scale=None, accum_out=None):
            kw = {}
            if bias is not None:
                kw["bias"] = bias
            if scale is not None:
                kw["scale"] = scale
            if accum_out is not None:
                kw["accum_out"] = accum_out
            S.op("act", lambda e: e.activation(out=out, in_=in_, func=func, **kw), reads, writes)

        def acopy(out, in_, reads, writes):
            S.op("act", lambda e: e.copy(out=out, in_=in_), reads, writes)

        def vcopy(out, in_, reads, writes, eng="dve"):
            S.op(eng, lambda e: e.tensor_copy(out=out, in_=in_), reads, writes)

        def vtt(out, in0, in1, op, reads, writes, eng="dve"):
            S.op(eng, lambda e: e.tensor_tensor(out=out, in0=in0, in1=in1, op=op), reads, writes)

        def vts(out, in0, s1, s2, op0, op1, reads, writes, eng="dve"):
            if op1 is None:
                S.op(eng, lambda e: e.tensor_scalar(out=out, in0=in0, scalar1=s1, scalar2=None, op0=op0), reads, writes)
            else:
                S.op(eng, lambda e: e.tensor_scalar(out=out, in0=in0, scalar1=s1, scalar2=s2, op0=op0, op1=op1), reads, writes)

        def vstt(out, in0, scalar, in1, op0, op1, reads, writes):
            S.op("dve", lambda e: e.scalar_tensor_tensor(out=out, in0=in0, scalar=scalar, in1=in1, op0=op0, op1=op1),
                 reads, writes)

        def vrecip(out, in_, reads, writes):
            S.op("dve", lambda e: e.reciprocal(out=out, in_=in_), reads, writes)

        def dma(out, in_, reads, writes, eng="sp", semkey=None):
            S.op(eng, lambda e: e.dma_start(out=out, in_=in_), reads, writes, dma=True, semkey=semkey)

        ident = sb(top, "ident", [128, 128], BF16)
        blockones = sb(top, "blockones", [128, 128], BF16)
        ones = sb(top, "ones", [128, 128], BF16)
        gcols = sb(top, "gcols", [128, 14], F32)
        epsc = sb(top, "epsc", [128, 1], F32)
        tri = sb(top, "tri", [128, 128], BF16)
        pm2 = sb(top, "pm2", [128, 16, 16], F32)
        bcol = sb(top, "bcol", [128, 6], F32)
        iota16 = sb(top, "iota16", [128, 16], F32)
        dma(ident[:], cd["c_ident"], [], ["ident"])
        dma(blockones[:], cd["c_blockones"], [], ["blockones"])
        dma(ones[:], cd["c_ones"], [], ["ones"])
        dma(gcols[:], gcol_d, [], ["gcols"])
        dma(tri[:], cd["c_tri"], [], ["tri"])
        dma(pm2[:], cd["c_pm2"].rearrange("p (a b) -> p a b", a=16), [], ["pm2"])
        dma(bcol[:], cd["c_moba_bcol"], [], ["bcol"])
        dma(iota16[:], cd["c_iota16"], [], ["iota16"])
        S.op("dve", lambda e: e.memset(epsc[:], EPS), [], ["epsc"])

        ps_tr = ps(top, "ps_tr", [128, 8, 128], BF16)
        ps_pj = [ps(top, "ps_pj%d" % i, [128, 512], F32) for i in range(2)]
        ps_ss = ps(top, "ps_ss", [128, 512], F32)
        ps_s = [ps(top, "ps_s%d" % i, [128, 4, 128], F32) for i in range(2)]
        ps_o = [ps(top, "ps_o%d" % i, [128, 4, 128], F32) for i in range(2)]

        def rms_rows_to_T(st, src_dram_tile_fn, ntiles, gslot, dstT, dst_name, tag):
            gbc = sb(st, "gbc" + tag, [128, DM], F32)
            dma(gbc[:], gbc_d[gslot], [], ["gbc" + tag])
            xt = [sb(st, "xt%s%d" % (tag, i), [128, DM], F32) for i in range(2)]
            hn = [sb(st, "hn%s%d" % (tag, i), [128, DM], BF16) for i in range(2)]
            junk = sb(st, "junk" + tag, [128, DM], BF16)
            ssq = sb(st, "ssq" + tag, [128, 2], F32)
            rt = sb(st, "rt" + tag, [128, 2], F32)
            rs = sb(st, "rs" + tag, [128, 2], F32)
            for t in range(ntiles):
                sl = t % 2
                X, H = "xt%s%d" % (tag, sl), "hn%s%d" % (tag, sl)
                dma(xt[sl][:], src_dram_tile_fn(t), [], [X])
                act(junk[:], xt[sl][:], AF.Square, [X], ["junk" + tag, "ssq%s%d" % (tag, sl)], accum_out=ssq[:, sl:sl + 1])
                act(rt[:, sl:sl + 1], ssq[:, sl:sl + 1], AF.Sqrt, ["ssq%s%d" % (tag, sl), "epsc"], ["rt%s%d" % (tag, sl)],
                    bias=epsc[:], scale=1.0 / DM)
                vrecip(rs[:, sl:sl + 1], rt[:, sl:sl + 1], ["rt%s%d" % (tag, sl)], ["rs%s%d" % (tag, sl)])
                vstt(hn[sl][:], xt[sl][:], rs[:, sl:sl + 1], gbc[:], ALU.mult, ALU.mult,
                     [X, "rs%s%d" % (tag, sl), "gbc" + tag], [H])
                for kc in range(8):
                    tr(ps_tr[:, kc, :], hn[sl][:, kc * 128:(kc + 1) * 128], ident[:], [H, "ident"], ["ps_tr"])
                acopy(dstT[:, :, t * 128:(t + 1) * 128], ps_tr[:], ["ps_tr"], [dst_name])

        wctr = [0]
        pend = [None]

        def load_w(st_w, wstage, wdst, wname, w_dram, c0, ncols):
            for cc in range(0, ncols, 128):
                sl = wctr[0] % 2
                wctr[0] += 1
                dma(wstage[sl][:], w_dram[:, c0 + cc:c0 + cc + 128].rearrange("(kc p) n -> p kc n", p=128),
                    [], ["wstage%d" % sl])
                eng = "pool" if (wctr[0] % 2) else "dve"
                vcopy(wdst[:, :, cc:cc + 128], wstage[sl][:], ["wstage%d" % sl], [wname], eng=eng)

        pjctr = [0]

        def proj_norm(wbf, wname, m0, M, src, srcname, ntok, gcol, blk, blkname, dst_fn, dstname, scr):
            sqb, rtb, rsb = scr
            N = min(512, ntok)
            for tc in range(ntok // N):
                sl = pjctr[0] % 2
                pjctr[0] += 1
                P = "ps_pj%d" % sl
                for kc in range(8):
                    mm(ps_pj[sl][0:M, 0:N], wbf[:, kc, m0:m0 + M], src[:, kc, tc * N:(tc + 1) * N], kc == 0, kc == 7,
                       [wname, srcname], [P])
                act(sqb[0:M, 0:N], ps_pj[sl][0:M, 0:N], AF.Square, [P], ["sqb"])
                mm(ps_ss[0:M, 0:N], blk[0:M, 0:M], sqb[0:M, 0:N], True, True, ["sqb", blkname], ["ps_ss"])
                act(rtb[0:M, 0:N], ps_ss[0:M, 0:N], AF.Sqrt, ["ps_ss", "epsc"], ["rtb"], bias=epsc[0:M, :], scale=1.0 / 64)
                vrecip(rsb[0:M, 0:N], rtb[0:M, 0:N], ["rtb"], ["rsb"])
                vstt(dst_fn(tc, N), ps_pj[sl][0:M, 0:N], gcol, rsb[0:M, 0:N], ALU.mult, ALU.mult,
                     [P, "rsb", "gcols"], [dstname])

        def proj_v(wbf, wname, c0, src, srcname, tok_slices, dst, dstname):
            nt = len(tok_slices)
            for jb in range(0, nt, 4):
                sl = pjctr[0] % 2
                pjctr[0] += 1
                P = "ps_pj%d" % sl
                nb = min(4, nt - jb)
                for i in range(nb):
                    for kc in range(8):
                        mm(ps_pj[sl][:, i * 128:(i + 1) * 128], src[:, kc, tok_slices[jb + i]], wbf[:, kc, c0:c0 + 128],
                           kc == 0, kc == 7, [wname, srcname], [P])
                o = dst[:, jb:jb + nb, :]
                i_ = ps_pj[sl][:, 0:nb * 128].rearrange("p (a b) -> p a b", a=nb)
                import os
                if (jb // 4) % 2 == 0 and not os.environ.get("PVDVE"):
                    acopy(o, i_, [P], [dstname])
                else:
                    vcopy(o, i_, [P], [dstname])

        def finalize_pair(acc, o16p, o16name, ssqacc, first, scr):
            sqb, rtb, rsb = scr
            for tc in range(8):
                c = slice(tc * 512, (tc + 1) * 512)
                vrecip(rsb[:, :], acc[:, 1, c], ["acc"], ["rsb"])
                vtt(rtb[:, :], acc[:, 0, c], rsb[:, :], ALU.mult, ["acc", "rsb"], ["rtb"])
                act(sqb[:, :], rtb[:, :], AF.Square, ["rtb"], ["sqb"])
                vcopy(o16p[:, c], rtb[:, :], ["rtb"], [o16name], eng="pool")
                mm(ps_ss[:, :], ones[:], sqb[:, :], True, True, ["sqb", "ones"], ["ps_ss"])
                if first:
                    vcopy(ssqacc[:, c], ps_ss[:, :], ["ps_ss"], ["ssqacc"])
                else:
                    vtt(ssqacc[:, c], ps_ss[:, :], ssqacc[:, c], ALU.add, ["ps_ss", "ssqacc"], ["ssqacc"])

        def group_norm_store(b, o16, npairs, nfeat, ssqacc, chunk0, ybuf, scr):
            sqb, rtb, rsb = scr
            for tc in range(8):
                c = slice(tc * 512, (tc + 1) * 512)
                act(rtb[:, :], ssqacc[:, c], AF.Sqrt, ["ssqacc", "epsc"], ["rtb"], bias=epsc[:], scale=1.0 / nfeat)
                vrecip(ssqacc[:, c], rtb[:, :], ["rtb"], ["ssqacc"])
            for p in range(npairs):
                Y = "o16_%d" % p
                for tc in range(8):
                    c = slice(tc * 512, (tc + 1) * 512)
                    vstt(o16[p][:, c], o16[p][:, c], gcols[:, 6 + chunk0 + p:7 + chunk0 + p], ssqacc[:, c],
                         ALU.mult, ALU.mult, [Y, "ssqacc", "gcols"], [Y])
                dma(yscr_d[b, chunk0 + p], o16[p][:], [Y], ["yscr%d_%d" % (b, chunk0 + p)])

        def attn_tile(sl, qk_list, v_list, acc_view, acc_first, exp_bias=None, evac_eng="dve", acc_part=None):
            Ps = "ps_s%d" % sl
            nslots = 0
            prev_base = None
            for (si, kT_ap, qT_ap, knames, extras) in qk_list:
                nslots = max(nslots, si + 1)
                base = kT_ap.base_partition()
                mm(ps_s[sl][:, si, :], kT_ap, qT_ap, True, len(extras) == 0, knames, [Ps],
                   pe_self=(prev_base is not None and base != prev_base))
                prev_base = base
                for ei, (l_, r_, nm) in enumerate(extras):
                    mm(ps_s[sl][:, si, :], l_, r_, False, ei == len(extras) - 1, nm, [Ps])
            PT = "pT%d" % sl
            used = sorted(q[0] for q in qk_list)
            if used == list(range(nslots)):
                ssel = slice(0, nslots)
            else:
                step = used[1] - used[0] if len(used) > 1 else 1
                assert used == list(range(used[0], used[-1] + 1, step))
                ssel = slice(used[0], used[-1] + 1, step)
            if exp_bias is None:
                act(pT[sl][:, ssel, :], ps_s[sl][:, ssel, :], AF.Exp, [Ps], [PT], scale=SCALE)
            else:
                act(pT[sl][:, ssel, :], ps_s[sl][:, ssel, :], AF.Exp, [Ps, "bcol"], [PT], scale=SCALE, bias=exp_bias)
            return PT


        NU = 8

        def peer(b):
            with ExitStack() as st:
                wout = sb(st, "wout", [128, 8, 1024], BF16)
                wpq = sb(st, "wpq", [128, 8, 2048], BF16)
                subT = sb(st, "subT", [128, 16, 128], BF16)
                wstage = [sb(st, "wstageP%d" % i, [128, 8, 128], F32) for i in range(2)]
                gffn = sb(st, "gffn", [128, DM], F32)
                dma(gffn[:], gbc_d[1], [], ["gffn"])
                wctr[0] = 0
                for cc in range(0, 1024, 128):
                    sl = (cc // 128) % 2
                    dma(wstage[sl][:], w_out_d[:, cc:cc + 128].rearrange("(kc p) n -> p kc n", p=128), [], ["wstageP%d" % sl])
                    vcopy(wout[:, :, cc:cc + 128], wstage[sl][:], ["wstageP%d" % sl], ["wout"], eng=("pool" if sl else "dve"))
                for cc in range(0, 2048, 128):
                    sl = (cc // 128) % 2
                    dma(wstage[sl][:], w_pq_d[:, cc:cc + 128].rearrange("(kc p) n -> p kc n", p=128), [], ["wstageP%d" % sl])
                    vcopy(wpq[:, :, cc:cc + 128], wstage[sl][:], ["wstageP%d" % sl], ["wpq"], eng=("pool" if sl else "dve"))

                ytc = [sb(st, "ytc%d" % i, [128, 8, 512], BF16) for i in range(2)]
                xt = [sb(st, "xtP%d" % i, [128, DM], F32) for i in range(2)]
                x1 = [sb(st, "x1_%d" % i, [128, DM], F32) for i in range(2)]
                hn2b = sb(st, "hn2b", [128, DM], BF16)
                junkb = sb(st, "junkP", [128, DM], BF16)
                hn2T = sb(st, "hn2T", [128, 8, 128], BF16)
                qryT = sb(st, "qryT", [128, 16, 128], BF16)
                sc = sb(st, "sc", [128, 16, 128], F32)
                dma(sc[:], sub_d.rearrange("c k d -> k c d"), [], ["sc"])
                vcopy(qryT[:], sc[:], ["sc"], ["qryT"])
                for c8 in range(2):
                    for i in range(8):
                        tr(ps_tr[:, i, :], qryT[:, c8 * 8 + i, :], ident[:], ["qryT", "ident"], ["ps_tr"])
                    acopy(subT[:, c8 * 8:(c8 + 1) * 8, :], ps_tr[:], ["ps_tr"], ["subT"])
                wk1 = sb(st, "wk1", [128, 128], F32)
                v12 = sb(st, "v12", [128, 8, 2, 16], F32)
                i12 = sb(st, "i12", [128, 8, 2, 16], U32)
                i12f = sb(st, "i12f", [128, 8, 2, 16], F32)
                cand = sb(st, "cand", [128, 8, 256], F32)
                wk2 = sb(st, "wk2", [128, 256], F32)
                ts = sb(st, "ts", [128, 8, 16], F32)
                pos = sb(st, "pos", [128, 8, 16], U32)
                pab = sb(st, "pab", [128, 2, 8, 16], U32)
                pabf = sb(st, "pabf", [128, 2, 8, 16], F32)
                oh = sb(st, "oh", [128, 8, 16, 16], F32)
                e12 = sb(st, "e12", [128, 2, 8, 16], F32)
                ef = sb(st, "ef", [128, 128], F32)
                idx = sb(st, "idx", [128, 128], U32)
                ex = sb(st, "ex", [128, 8, 16], F32)
                gs = sb(st, "gs", [128, 8], F32)
                gate = sb(st, "gate", [128, 128], F32)
                aact = sb(st, "aact", [128, 128], F32)
                wgt = sb(st, "wgt", [128, 128], F32)
                ssq = sb(st, "ssqP", [128, 1], F32)
                rt = sb(st, "rtP", [128, 1], F32)
                rs = sb(st, "rsP", [128, 1], F32)
                ub = [sb(st, "ub%d" % i, [128, DM], BF16) for i in range(NU)]
                vb = [sb(st, "vb%d" % i, [128, DM], BF16) for i in range(NU)]
                dg = [sb(st, "dg%d" % i, [128, 128], BF16) for i in range(4)]
                yres = ["yscr%d_%d" % (b, c) for c in range(8)]

                hn2bs = [hn2b, sb(st, "hn2b_b", [128, DM], BF16)]
                idxs = [idx, sb(st, "idx_b", [128, 128], U32)]
                gates = [gate, sb(st, "gate_b", [128, 128], F32)]

                def stage1(t):
                    sl = t % 2
                    X, X1 = "xtP%d" % sl, "x1_%d" % sl
                    HB, IDX, GT = "hn2b%d" % sl, "idx%d" % sl, "gate%d" % sl
                    hn2b_, idx_, gate_ = hn2bs[sl], idxs[sl], gates[sl]
                    ysl = (t // 4) % 2
                    YT = "ytc%d" % ysl
                    if t % 4 == 0:
                        dma(ytc[ysl][:], yscr_d[b, :, :, t * 128:t * 128 + 512].rearrange("c p n -> p c n"), yres, [YT])
                    dma(xt[sl][:], x_d[b, t * 128:(t + 1) * 128, :], [], [X])
                    yield
                    tcol = slice((t % 4) * 128, (t % 4 + 1) * 128)
                    for nh in range(2):
                        for kc in range(8):
                            mm(ps_o[nh][:].rearrange("p a b -> p (a b)"), ytc[ysl][:, kc, tcol], wout[:, kc, nh * 512:(nh + 1) * 512],
                               kc == 0, kc == 7, [YT, "wout"], ["ps_o%d" % nh])
                        yield
                    for nh in range(2):
                        c = slice(nh * 512, (nh + 1) * 512)
                        vtt(x1[sl][:, c], ps_o[nh][:].rearrange("p a b -> p (a b)"), xt[sl][:, c], ALU.add, ["ps_o%d" % nh, X], [X1])
                        yield
                    act(junkb[:], x1[sl][:], AF.Square, [X1], ["junkP", "ssqP"], accum_out=ssq[:, 0:1])
                    act(rt[:], ssq[:], AF.Sqrt, ["ssqP", "epsc"], ["rtP"], bias=epsc[:], scale=1.0 / DM)
                    vrecip(rs[:], rt[:], ["rtP"], ["rsP"])
                    yield
                    vstt(hn2b_[:], x1[sl][:], rs[:, 0:1], gffn[:], ALU.mult, ALU.mult, [X1, "rsP", "gffn"], [HB])
                    yield
                    for kc in range(8):
                        tr(ps_tr[:, kc, :], hn2b_[:, kc * 128:(kc + 1) * 128], ident[:], [HB, "ident"], ["ps_tr"])
                    acopy(hn2T[:], ps_tr[:], ["ps_tr"], ["hn2T"])
                    yield
                    for c4 in range(4):
                        q_ = c4 % 2
                        for i in range(4):
                            c = c4 * 4 + i
                            for kc in range(8):
                                mm(ps_s[q_][:, i, :], wpq[:, kc, c * 128:(c + 1) * 128], hn2T[:, kc, :], kc == 0, kc == 7,
                                   ["wpq", "hn2T"], ["ps_s%d" % q_])
                            yield
                        acopy(qryT[:, c4 * 4:(c4 + 1) * 4, :], ps_s[q_][:], ["ps_s%d" % q_], ["qryT"])
                    for c4 in range(4):
                        q_ = c4 % 2
                        for i in range(4):
                            c = c4 * 4 + i
                            mm(ps_s[q_][:, i, :], qryT[:, c, :], subT[:, c, :], True, True, ["qryT", "subT"], ["ps_s%d" % q_])
                        acopy(sc[:, c4 * 4:(c4 + 1) * 4, :], ps_s[q_][:], ["ps_s%d" % q_], ["sc"])
                        yield
                    for h in range(8):
                        for hf in range(2):
                            a_ = sc[:, 2 * h + hf, :]
                            S.op("dve", lambda e, a_=a_, h=h, hf=hf: e.max(out=v12[:, h, hf, 0:8], in_=a_), ["sc"], ["v12"])
                            yield
                            S.op("dve", lambda e, a_=a_, h=h, hf=hf: e.match_replace(out=wk1[:], in_to_replace=v12[:, h, hf, 0:8],
                                                                                   in_values=a_, imm_value=-1e30), ["sc", "v12"], ["wk1"])
                            yield
                            S.op("dve", lambda e, h=h, hf=hf: e.max(out=v12[:, h, hf, 8:16], in_=wk1[:]), ["wk1"], ["v12"])
                            yield
                            S.op("dve", lambda e, a_=a_, h=h, hf=hf: e.max_index(out=i12[:, h, hf, 0:8], in_max=v12[:, h, hf, 0:8],
                                                                               in_values=a_), ["sc", "v12"], ["i12"])
                            yield
                            S.op("dve", lambda e, a_=a_, h=h, hf=hf: e.max_index(out=i12[:, h, hf, 8:16], in_max=v12[:, h, hf, 8:16],
                                                                               in_values=a_), ["sc", "v12"], ["i12"])
                            yield
                        cv = cand[:, h, :].rearrange("p (a b) -> p a b", a=16)
                        vtt(cv, v12[:, h, 0, :].unsqueeze(2).to_broadcast([128, 16, 16]),
                            v12[:, h, 1, :].unsqueeze(1).to_broadcast([128, 16, 16]), ALU.add, ["v12"], ["cand"])
                        yield
                        ch = cand[:, h, :]
                        S.op("dve", lambda e, ch=ch, h=h: e.max(out=ts[:, h, 0:8], in_=ch), ["cand"], ["ts"])
                        yield
                        S.op("dve", lambda e, ch=ch, h=h: e.match_replace(out=wk2[:], in_to_replace=ts[:, h, 0:8], in_values=ch,
                                                                        imm_value=-1e30), ["cand", "ts"], ["wk2"])
                        yield
                        S.op("dve", lambda e, h=h: e.max(out=ts[:, h, 8:16], in_=wk2[:]), ["wk2"], ["ts"])
                        yield
                        S.op("dve", lambda e, ch=ch, h=h: e.max_index(out=pos[:, h, 0:8], in_max=ts[:, h, 0:8], in_values=ch),
                             ["cand", "ts"], ["pos"])
                        yield
                        S.op("dve", lambda e, ch=ch, h=h: e.max_index(out=pos[:, h, 8:16], in_max=ts[:, h, 8:16], in_values=ch),
                             ["cand", "ts"], ["pos"])
                        yield
                    vts(pab[:, 0], pos[:], 4, None, ALU.logical_shift_right, None, ["pos"], ["pab"])
                    vts(pab[:, 1], pos[:], 15, None, ALU.bitwise_and, None, ["pos"], ["pab"])
                    yield
                    vcopy(pabf[:], pab[:], ["pab"], ["pabf"])
                    vcopy(i12f[:], i12[:], ["i12"], ["i12f"])
                    yield
                    for w_ in range(2):
                        vtt(oh[:], pabf[:, w_].unsqueeze(3).to_broadcast([128, 8, 16, 16]),
                            iota16[:].unsqueeze(1).unsqueeze(1).to_broadcast([128, 8, 16, 16]), ALU.is_equal,
                            ["pabf", "iota16"], ["oh"])
                        yield
                        vtt(oh[:], oh[:], i12f[:, :, w_, :].unsqueeze(2).to_broadcast([128, 8, 16, 16]), ALU.mult,
                            ["oh", "i12f"], ["oh"])
                        yield
                        S.op("dve", lambda e, w_=w_: e.tensor_reduce(out=e12[:, w_], in_=oh[:], axis=AX.X, op=ALU.add), ["oh"], ["e12"])
                        yield
                    vstt(ef[:], e12[:, 0].rearrange("p a b -> p (a b)"), 128.0, e12[:, 1].rearrange("p a b -> p (a b)"),
                         ALU.mult, ALU.add, ["e12"], ["ef"])
                    vcopy(idx_[:], ef[:], ["ef"], [IDX])
                    yield
                    vtt(ex[:], ts[:], ts[:, :, 0:1].to_broadcast([128, 8, 16]), ALU.subtract, ["ts"], ["ex"])
                    act(ex[:], ex[:], AF.Exp, ["ex"], ["ex"])
                    yield
                    S.op("dve", lambda e: e.tensor_reduce(out=gs[:], in_=ex[:], axis=AX.X, op=ALU.add), ["ex"], ["gs"])
                    vrecip(gs[:], gs[:], ["gs"], ["gs"])
                    vtt(gate_[:].rearrange("p (a b) -> p a b", a=8), ex[:], gs[:].unsqueeze(2).to_broadcast([128, 8, 16]), ALU.mult,
                        ["ex", "gs"], [GT])
                    yield

                def advance(g, n=1):
                    if g is None:
                        return None
                    try:
                        for _ in range(n):
                            next(g)
                    except StopIteration:
                        return None
                    return g

                g = stage1(0)
                while g is not None:
                    g = advance(g, 8)
                for t in range(peer_tiles):
                    sl = t % 2
                    X1 = "x1_%d" % sl
                    HB, IDX, GT = "hn2b%d" % sl, "idx%d" % sl, "gate%d" % sl
                    hn2b_, idx_, gate_ = hn2bs[sl], idxs[sl], gates[sl]
                    g = stage1(t + 1) if t + 1 < peer_tiles else None
                    for j in range(128):
                        u_ = j % NU
                        S.op("pool", lambda e, u_=u_, j=j, idx_=idx_: e.indirect_dma_start(
                            out=ub[u_][:], out_offset=None, in_=u16_d,
                            in_offset=bass.IndirectOffsetOnAxis(ap=idx_[:, j:j + 1], axis=0)),
                            [IDX, "u16"], ["ub%d" % u_], dma=True)
                        S.op("dve", lambda e, u_=u_, j=j, hn2b_=hn2b_: e.scalar_tensor_tensor(
                            out=junkb[:], in0=ub[u_][:], scalar=1.0, in1=hn2b_[:], op0=ALU.mult, op1=ALU.mult,
                            accum_out=aact[:, j:j + 1]), ["ub%d" % u_, HB], ["junkP", "aact"])
                        g = advance(g, 1)
                    act(wgt[:], aact[:], AF.Gelu, ["aact"], ["wgt"])
                    vtt(wgt[:], wgt[:], gate_[:], ALU.mult, ["wgt", GT], ["wgt"])
                    for j in range(128):
                        u_ = j % NU
                        d_ = j % 4
                        S.op("pool", lambda e, u_=u_, j=j, idx_=idx_: e.indirect_dma_start(
                            out=vb[u_][:], out_offset=None, in_=v16_d,
                            in_offset=bass.IndirectOffsetOnAxis(ap=idx_[:, j:j + 1], axis=0)),
                            [IDX, "v16"], ["vb%d" % u_], dma=True)
                        vts(dg[d_][:], ident[:], wgt[:, j:j + 1], None, ALU.mult, None, ["ident", "wgt"], ["dg%d" % d_])
                        for nh in range(2):
                            mm(ps_pj[nh][:, :], dg[d_][:], vb[u_][:, nh * 512:(nh + 1) * 512], j == 0, j == 127,
                               ["dg%d" % d_, "vb%d" % u_], ["ps_pj%d" % nh], skip=True)
                        g = advance(g, 1)
                    while g is not None:
                        g = advance(g, 8)
                    for nh in range(2):
                        c = slice(nh * 512, (nh + 1) * 512)
                        vtt(x1[sl][:, c], ps_pj[nh][:, :], x1[sl][:, c], ALU.add, ["ps_pj%d" % nh, X1], [X1])
                    dma(out_d[b, t * 128:(t + 1) * 128, :], x1[sl][:], [X1], ["out%d" % sl], semkey=("dma", "out%d" % sl))
                S.barrier()

        if do_peer:
            with ExitStack() as cst_:
                stg = [sb(cst_, "cstg%d" % i, [128, 2, DM], F32) for i in range(3)]
                cbf = [sb(cst_, "cbf%d" % i, [128, 2, DM], BF16) for i in range(3)]
                k_ = 0
                for (src_, dst_, nm_) in ((pu_d, u16_d, "u16"), (pv_d, v16_d, "v16")):
                    for ch in range(NEXP // 256):
                        s_ = k_ % 3
                        k_ += 1
                        rows = slice(ch * 256, (ch + 1) * 256)
                        dma(stg[s_][:], src_[rows, :].rearrange("(p r) d -> p r d", r=2), [], ["cstg%d" % s_])
                        if s_ == 0:
                            vcopy(cbf[s_][:], stg[s_][:], ["cstg%d" % s_], ["cbf%d" % s_])
                        elif s_ == 1:
                            acopy(cbf[s_][:], stg[s_][:], ["cstg%d" % s_], ["cbf%d" % s_])
                        else:
                            vcopy(cbf[s_][:], stg[s_][:], ["cstg%d" % s_], ["cbf%d" % s_], eng="pool")
                        dma(dst_[rows, :].rearrange("(p r) d -> p r d", r=2), cbf[s_][:], ["cbf%d" % s_], [nm_],
                            semkey=("dma", "cbfo%d" % s_))
                S.barrier()

        for b in range(nseq):
            with ExitStack() as seqst:
                hnT = sb(seqst, "hnT", [128, 8, SEQ], BF16)
                with ExitStack() as st:
                    rms_rows_to_T(st, lambda t: x_d[b, t * 128:(t + 1) * 128, :], NT, 0, hnT, "hnT", "A")
                    if debug:
                        dma(dbg_hnT, hnT[:], ["hnT"], ["dbg_hnT"])
                    S.barrier()
                with ExitStack() as st:
                    wstage = [sb(st, "wstage%d" % i, [128, 8, 128], F32) for i in range(2)]
                    wq = sb(st, "wq", [128, 8, 128], BF16)
                    wk = sb(st, "wk", [128, 8, 128], BF16)
                    wv = sb(st, "wv", [128, 8, 128], BF16)
                    sqb = sb(st, "sqb", [128, 512], BF16)
                    rtb = sb(st, "rtb", [128, 512], F32)
                    rsb = sb(st, "rsb", [128, 512], F32)
                    scr = (sqb, rtb, rsb)
                    acc = sb(st, "acc", [128, 2, SEQ], F32)
                    ssqacc = sb(st, "ssqacc", [128, SEQ], F32)
                    o16 = [sb(st, "o16_%d" % i, [128, SEQ], BF16) for i in range(3)]
                    pT = [sb(st, "pT%d" % i, [128, 4, 128], BF16) for i in range(2)]
                    ybuf = o16
                    Vd = sb(st, "Vd", [128, NT, 128], BF16)

                    with ExitStack() as gst:
                      if "dil" in stages:
                        qT = sb(gst, "qT", [128, SEQ], BF16)
                        kT = sb(gst, "kT", [128, SEQ], BF16)
                        dbias = sb(gst, "dbias", [128, 24, 128], BF16)
                        for p in range(min(3, npair)):
                            dma(dbias[:], cd["c_dilbias"][p].rearrange("k (a q) -> k a q", a=24), [], ["dbias"])
                            load_w(st, wstage, wq, "wq", w_in_d, 0 + p * 128, 128)
                            load_w(st, wstage, wk, "wk", w_in_d, 384 + p * 128, 128)
                            load_w(st, wstage, wv, "wv", w_in_d, 768 + p * 128, 128)
                            proj_norm(wq, "wq", 0, 128, hnT, "hnT", SEQ, gcols[:, 0:1], blockones, "blockones",
                                      lambda tc, N: qT[:, tc * N:(tc + 1) * N], "qT", scr)
                            proj_norm(wk, "wk", 0, 128, hnT, "hnT", SEQ, gcols[:, 1:2], blockones, "blockones",
                                      lambda tc, N: kT[:, tc * N:(tc + 1) * N], "kT", scr)
                            for di, d in enumerate(DIL):
                                nblk = NT // d
                                tsl = []
                                for j in range(NT):
                                    r, n = divmod(j, nblk)
                                    base = d * 128 * n + r
                                    tsl.append(slice(base, base + 127 * d + 1, d))
                                proj_v(wv, "wv", 0, hnT, "hnT", tsl, Vd, "Vd")
                                for j in range(NT):
                                    r, n = divmod(j, nblk)
                                    sl = j % 2
                                    have_prev = n > 0
                                    qk = []
                                    for h in range(2):
                                        hp = 64 * h
                                        for jj in range(2):
                                            if jj == 0 and not have_prev:
                                                continue
                                            tk = tsl[j - 1] if jj == 0 else tsl[j]
                                            bi = ((h * 3 + di) * 2 + 0) * 2 + jj
                                            bl = ((h * 3 + di) * 2 + 1) * 2 + jj
                                            qk.append((h * 2 + jj, kT[hp:hp + 64, tk], qT[hp:hp + 64, tsl[j]], ["kT", "qT"],
                                                       [(ident[:], dbias[:, bi, :], ["ident", "dbias"]),
                                                        (ident[:], dbias[:, bl, :], ["ident", "dbias"])]))
                                    PT = attn_tile(sl, qk, None, None, None)

                                    def pv_dil(j=j, sl=sl, have_prev=have_prev, PT=PT, di=di, tsl=tsl):
                                        Po = "ps_o%d" % sl
                                        for h in range(2):
                                            hp = 64 * h
                                            jjs = [1] if not have_prev else [0, 1]
                                            for jj in jjs:
                                                jk = j - 1 if jj == 0 else j
                                                mm(ps_o[sl][hp:hp + 64, 0, :], Vd[:, jk, hp:hp + 64], pT[sl][:, h * 2 + jj, :],
                                                   jj == jjs[0], jj == 1, ["Vd", PT], [Po])
                                            for jj in jjs:
                                                mm(ps_o[sl][hp:hp + 64, 1, :], ones[:, 0:64], pT[sl][:, h * 2 + jj, :],
                                                   jj == jjs[0], jj == 1, ["ones", PT], [Po])
                                        av = acc[:, :, tsl[j]]
                                        if di == 0:
                                            vcopy(av, ps_o[sl][:, 0:2, :], [Po], ["acc"])
                                        else:
                                            vtt(av, ps_o[sl][:, 0:2, :], av, ALU.add, [Po, "acc"], ["acc"])
                                    if pend[0] is not None:
                                        pend[0]()
                                    pend[0] = pv_dil
                                if pend[0] is not None:
                                    pend[0]()
                                    pend[0] = None
                            finalize_pair(acc, o16[p], "o16_%d" % p, ssqacc, p == 0, scr)
                        group_norm_store(b, o16, min(3, npair), 384, ssqacc, 0, ybuf, scr)
                        S.barrier()

                    with ExitStack() as gst:
                      if "moba" in stages:
                        qa = sb(gst, "qa", [128, SEQ], BF16)
                        ka = sb(gst, "ka", [128, SEQ], BF16)
                        km32 = sb(gst, "km32", [64, 16], F32)
                        kmT = sb(gst, "kmT", [64, 16], BF16)
                        gm = sb(gst, "gm", [128, 16], F32)
                        mx8 = sb(gst, "mx8", [128, 8], F32)
                        pen = sb(gst, "pen", [128, 16], F32)
                        penb = sb(gst, "penb", [128, 16], BF16)
                        dma(ka[64:86, :], cd["c_moba_k"], [], ["ka_c"])
                        tsl = [slice(j * 128, (j + 1) * 128) for j in range(NT)]
                        for p in range(min(3, npair)):
                            load_w(st, wstage, wq, "wq", w_in_d, 1152 + p * 128, 128)
                            load_w(st, wstage, wk, "wk", w_in_d, 1536 + p * 128, 128)
                            load_w(st, wstage, wv, "wv", w_in_d, 1920 + p * 128, 128)
                            proj_v(wv, "wv", 0, hnT, "hnT", tsl, Vd, "Vd")
                            for h in range(2):
                                H = 2 * p + h
                                hp = 64 * h
                                dma(qa[80:86, :], cd["c_moba_q"][H], [], ["qa_c"])
                                proj_norm(wq, "wq", hp, 64, hnT, "hnT", SEQ, gcols[0:64, 2:3], ones, "ones",
                                          lambda tc, N: qa[0:64, tc * N:(tc + 1) * N], "qa", scr)
                                proj_norm(wk, "wk", hp, 64, hnT, "hnT", SEQ, gcols[0:64, 3:4], ones, "ones",
                                          lambda tc, N: ka[0:64, tc * N:(tc + 1) * N], "ka", scr)
                                S.op("dve", lambda e: e.tensor_reduce(out=km32[:, :], in_=ka[0:64, :].rearrange("p (a b) -> p a b", a=16),
                                                                      axis=AX.X, op=ALU.add), ["ka"], ["km32"])
                                vts(kmT[:, :], km32[:, :], 1.0 / 256, None, ALU.mult, None, ["km32"], ["kmT"])
                                for t in range(NT):
                                    mm(ps_ss[:, 0:16], qa[0:64, tsl[t]], kmT[:, :], True, True, ["qa", "kmT"], ["ps_ss"])
                                    vtt(gm[:, :], ps_ss[:, 0:16], pm2[:, t // 2, :], ALU.add, ["ps_ss", "pm2"], ["gm"])
                                    S.op("dve", lambda e: e.max(out=mx8[:, :], in_=gm[:, :]), ["gm"], ["mx8"])
                                    vts(pen[:, :], gm[:, :], mx8[:, 3:4], None, ALU.is_ge, None, ["gm", "mx8"], ["pen"])
                                    vts(penb[:, :], pen[:, :], -NEGM, NEGM, ALU.mult, ALU.add, ["pen"], ["penb"])
                                    mm(ps_ss[64:80, 128:256], penb[:, :], ident[:], True, True, ["penb", "ident"], ["ps_ss"])
                                    acopy(qa[64:80, tsl[t]], ps_ss[64:80, 128:256], ["ps_ss"], ["qa_p"])
                                cnt = 0
                                for t in range(NT):
                                    osl = t % 2
                                    Po = "ps_o%d" % osl
                                    for b0 in range(0, t + 1, 4):
                                        nb = min(4, t + 1 - b0)
                                        sl = cnt % 2
                                        cnt += 1
                                        qk = []
                                        for i in range(nb):
                                            kt = b0 + i
                                            ex = [(ident[:], tri[:], ["ident", "tri"])] if kt == t else []
                                            qk.append((i, ka[0:86, tsl[kt]], qa[0:86, tsl[t]], ["ka", "ka_c", "qa", "qa_c", "qa_p"], ex))
                                        PT = attn_tile(sl, qk, None, None, None, exp_bias=bcol[:, H:H + 1])

                                        def pv_moba(t=t, b0=b0, nb=nb, sl=sl, osl=osl, Po=Po, PT=PT, hp=hp, tsl=tsl):
                                            for i in range(nb):
                                                kt = b0 + i
                                                mm(ps_o[osl][hp:hp + 64, 0, :], Vd[:, kt, hp:hp + 64], pT[sl][:, i, :], kt == 0, kt == t,
                                                   ["Vd", PT], [Po], skip=True)
                                                mm(ps_o[osl][hp:hp + 64, 1, :], ones[:, 0:64], pT[sl][:, i, :], False, kt == t,
                                                   ["ones", PT], [Po], skip=True)
                                            if b0 + nb == t + 1:
                                                acopy(acc[hp:hp + 64, :, tsl[t]], ps_o[osl][hp:hp + 64, 0:2, :], [Po], ["acc"])
                                        if pend[0] is not None:
                                            pend[0]()
                                        pend[0] = pv_moba
                                if pend[0] is not None:
                                    pend[0]()
                                    pend[0] = None
                            finalize_pair(acc, o16[p], "o16_%d" % p, ssqacc, p == 0, scr)
                        group_norm_store(b, o16, min(3, npair), 384, ssqacc, 3, ybuf, scr)
                        S.barrier()

                    with ExitStack() as gst:
                      if "mem" in stages:
                        memT = sb(gst, "memT", [128, 8, 256], BF16)
                        import os
                        MEMCUT = int(os.environ.get("MEMCUT", "9"))
                        rms_rows_to_T(gst, lambda t: mem_d[b, t * 128:(t + 1) * 128, :], 2, 2, memT, "memT", "M")
                        qT = sb(gst, "qTm", [128, SEQ], BF16)
                        kmem = sb(gst, "kmem", [128, 256], BF16)
                        Vm = sb(gst, "Vm", [128, 2, 128], BF16)
                        tsl = [slice(j * 128, (j + 1) * 128) for j in range(NT)]
                        for p in range(min(2, npair) if MEMCUT > 1 else 0):
                            load_w(st, wstage, wq, "wq", w_in_d, 2304 + p * 128, 128)
                            load_w(st, wstage, wk, "wk", w_mkv_d, 0 + p * 128, 128)
                            load_w(st, wstage, wv, "wv", w_mkv_d, 256 + p * 128, 128)
                            proj_norm(wq, "wq", 0, 128, hnT, "hnT", SEQ, gcols[:, 4:5], blockones, "blockones",
                                      lambda tc, N: qT[:, tc * N:(tc + 1) * N], "qTm", scr)
                            if MEMCUT <= 2:
                                continue
                            proj_norm(wk, "wk", 0, 128, memT, "memT", 256, gcols[:, 5:6], blockones, "blockones",
                                      lambda tc, N: kmem[:, tc * N:(tc + 1) * N], "kmem", scr)
                            if MEMCUT <= 3:
                                continue
                            if os.environ.get("PVA"):
                                proj_v(wv, "wv", 0, memT, "memT", tsl[0:2], Vd, "Vd")
                            elif os.environ.get("PVB"):
                                proj_v(wv, "wv", 0, hnT, "hnT", tsl[0:2], Vm, "Vm")
                            else:
                                proj_v(wv, "wv", 0, memT, "memT", tsl[0:2], Vm, "Vm")
                            if MEMCUT <= 4:
                                continue
                            for t in range(NT):
                                sl = t % 2
                                Po = "ps_o%d" % sl
                                qk = []
                                for h in range(2):
                                    hp = 64 * h
                                    for jj in range(2):
                                        qk.append((h * 2 + jj, kmem[hp:hp + 64, tsl[jj]], qT[hp:hp + 64, tsl[t]], ["kmem", "qTm"], []))
                                PT = attn_tile(sl, qk, None, None, None)

                                def pv_mem(t=t, sl=sl, Po=Po, PT=PT, tsl=tsl):
                                    for h in range(2):
                                        hp = 64 * h
                                        for jj in range(2):
                                            mm(ps_o[sl][hp:hp + 64, 0, :], Vm[:, jj, hp:hp + 64], pT[sl][:, h * 2 + jj, :], jj == 0, jj == 1,
                                               ["Vm", PT], [Po])
                                        for jj in range(2):
                                            mm(ps_o[sl][hp:hp + 64, 1, :], ones[:, 0:64], pT[sl][:, h * 2 + jj, :], jj == 0, jj == 1,
                                               ["ones", PT], [Po])
                                    vcopy(acc[:, :, tsl[t]], ps_o[sl][:, 0:2, :], [Po], ["acc"])
                                if pend[0] is not None:
                                    pend[0]()
                                pend[0] = pv_mem
                            if pend[0] is not None:
                                pend[0]()
                                pend[0] = None
                            finalize_pair(acc, o16[p], "o16_%d" % p, ssqacc, p == 0, scr)
                        group_norm_store(b, o16, min(2, npair), 256, ssqacc, 6, ybuf, scr)
                        S.barrier()
            S.barrier()
            if do_peer:
                peer(b)
        S.barrier()
        S.emit()
    return nc, cst


def _host_inputs(inputs, cst, nseq, core, do_peer=True):
    f = lambda a: np.ascontiguousarray(np.asarray(a, dtype=np.float32))
    m = {}
    m["x"] = f(inputs["x"][core * nseq:(core + 1) * nseq])
    m["mem"] = f(inputs["mem"][core * nseq:(core + 1) * nseq])
    m["w_in"] = f(inputs["w_in"][0])
    m["w_mem_kv"] = f(inputs["w_mem_kv"][0])
    m["w_out"] = f(inputs["w_out"][0])
    m["w_peer_q"] = f(inputs["w_peer_q"][0])
    s1 = np.asarray(inputs["peer_subkeys_1"][0]); s2 = np.asarray(inputs["peer_subkeys_2"][0])
    m["subkeys"] = f(np.stack([s1, s2], axis=1).reshape(16, 128, 128))
    m["peer_u"] = f(inputs["peer_u"][0] if do_peer else inputs["peer_u"][0][:128])
    m["peer_v"] = f(inputs["peer_v"][0] if do_peer else inputs["peer_v"][0][:128])
    gb = np.stack([np.broadcast_to(np.asarray(inputs[k][0]), (128, DM)) for k in ("g_mix", "g_ffn", "g_memtok")])
    m["g_bc"] = f(gb)
    cols = []
    for k in ("qg_dil", "kg_dil", "qg_moba", "kg_moba", "qg_mem", "kg_mem"):
        cols.append(np.tile(np.asarray(inputs[k][0]), 2))
    og = np.concatenate([np.asarray(inputs["og_dil"][0]), np.asarray(inputs["og_moba"][0]), np.asarray(inputs["og_mem"][0])])
    for c in range(8):
        cols.append(og[c * 128:(c + 1) * 128])
    m["g_cols"] = f(np.stack(cols, axis=1))
    for k, v in cst.items():
        m[k] = v
    return m


_CACHE = {}


def kernel(**inputs):
    nseq = 2
    if "prog" not in _CACHE:
        _CACHE["prog"] = build_program(nseq=nseq)
    nc, cst = _CACHE["prog"]
    in_maps = [_host_inputs(inputs, cst, nseq, c) for c in range(NCORES)]
    res = run_bass_kernel_spmd(nc, in_maps, core_ids=list(range(NCORES)))
    out = np.concatenate([np.asarray(r["out"]) for r in res.results], axis=0)
    return out.astype(np.float32)
```

```python
import math
from contextlib import ExitStack

import numpy as np
import ml_dtypes

import concourse.bass as bass
import concourse.mybir as mybir
from concourse.bass_utils import run_bass_kernel_spmd

F32 = mybir.dt.float32
BF16 = mybir.dt.bfloat16
U32 = mybir.dt.uint32
I32 = mybir.dt.int32
AF = mybir.ActivationFunctionType
ALU = mybir.AluOpType
AX = mybir.AxisListType

NCORES = 8
SEQ = 4096
DM = 1024
NT = SEQ // 128
SCALE = 0.125
EPS = 1e-6
NEGM = -30000.0
SEM_LIMIT = 32000
DIL = (1, 4, 16)


class Sched:
    ENGS = ("pe", "act", "dve", "pool", "sp")

    def __init__(self, nc, stack):
        self.nc = nc
        self.stack = stack
        self.ops = {e: [] for e in self.ENGS}
        self.cnt = {}
        self.sems = {}
        self.last_w = {}
        self.readers = {}
        self.seen = {e: {} for e in self.ENGS}
        self.nops = 0

    def _sem(self, key, epoch):
        k = (key, epoch)
        if k not in self.sems:
            self.sems[k] = self.stack.enter_context(self.nc.semaphore("s%d" % len(self.sems)))
        return self.sems[k]

    def _bump(self, key, amt):
        ep, v = self.cnt.get(key, (0, 0))
        if v + amt > SEM_LIMIT:
            ep, v = ep + 1, 0
        v += amt
        self.cnt[key] = (ep, v)
        return (key, ep, v)

    def op(self, engine, fn, reads=(), writes=(), dma=False, semkey=None, pe_self=False):
        deps = set()
        for r in reads:
            if r in self.last_w:
                deps.add(self.last_w[r])
            if r.startswith("ps_"):
                for ev in self.readers.get(r, ()):
                    deps.add(ev)
        for w in writes:
            if w in self.last_w:
                deps.add(self.last_w[w])
            for ev in self.readers.get(w, ()):
                deps.add(ev)
        if dma:
            key = semkey if semkey is not None else ("dma", (writes[0] if writes else reads[0]))
            ev = self._bump(key, 16)
            amt = 16
        else:
            key = engine
            ev = self._bump(key, 1)
            amt = 1
        seen = self.seen[engine]
        best = {}
        for (k, ep, v) in deps:
            if k == "pe" and engine == "pe" and not dma and not pe_self:
                continue
            if best.get(k, (-1, -1)) < (ep, v):
                best[k] = (ep, v)
        waits = []
        for k, (ep, v) in best.items():
            if seen.get(k, (-1, -1)) >= (ep, v):
                continue
            seen[k] = (ep, v)
            waits.append((self._sem(k, ep), v))
        self.ops[engine].append((waits, fn, self._sem(ev[0], ev[1]), amt))
        for w in writes:
            self.last_w[w] = ev
            self.readers[w] = []
        for r in reads:
            if r not in writes:
                self.readers.setdefault(r, []).append(ev)
        self.nops += 1
        return ev

    def barrier(self, engines=None):
        for e in (engines or self.ENGS):
            waits = []
            seen = self.seen[e]
            for key, (ep, v) in self.cnt.items():
                if seen.get(key, (-1, -1)) >= (ep, v):
                    continue
                seen[key] = (ep, v)
                waits.append((self._sem(key, ep), v))
            if waits:
                self.ops[e].append((waits, None, None, 0))

    def emit(self):
        nc = self.nc
        with nc.Block() as block:
            def run(engname):
                def body(eng):
                    for waits, fn, sem, amt in self.ops[engname]:
                        for s, v in waits:
                            eng.wait_ge(s, v)
                        if fn is not None:
                            fn(eng).then_inc(sem, amt)
                return body
            block.tensor(run("pe"))
            block.scalar(run("act"))
            block.vector(run("dve"))
            block.gpsimd(run("pool"))
            block.sync(run("sp"))


def _bf(a):
    return np.asarray(a, dtype=np.float32).astype(ml_dtypes.bfloat16)


def _split3(a):
    a = np.asarray(a, dtype=np.float64)
    h = _bf(a)
    r = a - h.astype(np.float64)
    l = _bf(r)
    r2 = r - l.astype(np.float64)
    l2 = _bf(r2)
    return h, l, l2


def _constants():
    c = {}
    slopes = 2.0 ** (-8.0 * np.arange(1, 13, dtype=np.float64) / 12.0)
    sl_dil, sl_moba = slopes[0::2], slopes[1::2]
    c["c_ident"] = _bf(np.eye(128))
    bo = np.zeros((128, 128)); bo[:64, :64] = 1; bo[64:, 64:] = 1
    c["c_blockones"] = _bf(bo)
    c["c_ones"] = _bf(np.ones((128, 128)))
    kl = np.arange(128)[:, None]
    ql = np.arange(128)[None, :]
    tab = np.zeros((3, 128, 2, 3, 2, 2, 128), dtype=ml_dtypes.bfloat16)
    for p in range(3):
        for h in range(2):
            for di, d in enumerate(DIL):
                for jj in range(2):
                    delta = ql - kl + (128 if jj == 0 else 0)
                    valid = (delta >= 0) & (delta <= 128)
                    b = np.where(valid, -sl_dil[2 * p + h] * d * delta / SCALE, NEGM)
                    hi = _bf(b)
                    lo = _bf(b - hi.astype(np.float64))
                    tab[p, :, h, di, 0, jj, :] = hi
                    tab[p, :, h, di, 1, jj, :] = lo
    c["c_dilbias"] = tab.reshape(3, 128, 24 * 128)
    c["c_tri"] = _bf(np.where(kl <= ql, 0.0, NEGM))
    pm = np.zeros((16, 16))
    for npast in range(16):
        for n in range(16):
            pm[npast, n] = 0.0 if n < npast else (1e30 if n == npast else -1e30)
    c["c_pm2"] = np.broadcast_to(pm.reshape(1, 256), (128, 256)).astype(np.float32).copy()
    tok = np.arange(SEQ)
    mq = np.zeros((6, 6, SEQ), dtype=ml_dtypes.bfloat16)
    for h in range(6):
        cc = 1024.0 * sl_moba[h]
        a, b_, c_ = _split3(np.full(SEQ, cc))
        mq[h, 0], mq[h, 1], mq[h, 2] = a, b_, c_
        a, b_, c_ = _split3(-cc * (tok // 128))
        mq[h, 3], mq[h, 4], mq[h, 5] = a, b_, c_
    c["c_moba_q"] = mq
    mk = np.zeros((22, SEQ))
    for n in range(16):
        mk[n, n * 256:(n + 1) * 256] = 1.0
    mk[16:19] = (tok // 128)[None, :]
    mk[19:22] = 1.0
    c["c_moba_k"] = _bf(mk)
    c["c_moba_bcol"] = (sl_moba[None, :] * (np.arange(128)[:, None] - 64.0)).astype(np.float32)
    c["c_iota16"] = np.broadcast_to(np.arange(16, dtype=np.float32)[None, :], (128, 16)).copy()
    return c


def build_program(nseq=2, debug=False, do_peer=True, stages=('dil', 'moba', 'mem'), npair=3, peer_tiles=NT):
    nc = bass.Bass("TRN2", target_bir_lowering=False)

    def din(name, shape, dt):
        return nc.dram_tensor(name, list(shape), dt, kind="ExternalInput").ap()

    x_d = din("x", [nseq, SEQ, DM], F32)
    mem_d = din("mem", [nseq, 256, DM], F32)
    w_in_d = din("w_in", [DM, 2560], F32)
    w_mkv_d = din("w_mem_kv", [DM, 512], F32)
    w_out_d = din("w_out", [DM, DM], F32)
    w_pq_d = din("w_peer_q", [DM, 2048], F32)
    sub_d = din("subkeys", [16, 128, 128], F32)
    pu_d = din("peer_u", [16384 if do_peer else 128, DM], F32)
    pv_d = din("peer_v", [16384 if do_peer else 128, DM], F32)
    gbc_d = din("g_bc", [3, 128, DM], F32)
    gcol_d = din("g_cols", [128, 14], F32)
    cst = _constants()
    cd = {}
    for k, v in cst.items():
        dt = BF16 if v.dtype == ml_dtypes.bfloat16 else F32
        cd[k] = din(k, v.shape, dt)
    out_d = nc.dram_tensor("out", [nseq, SEQ, DM], F32, kind="ExternalOutput").ap()
    yscr_d = nc.dram_tensor("yscr", [nseq, 8, 128, SEQ], BF16,
                            kind=("ExternalOutput" if debug else "Internal")).ap()
    if debug:
        dbg_hnT = nc.dram_tensor("dbg_hnT", [128, 8, SEQ], BF16, kind="ExternalOutput").ap()
    NEXP = 16384 if do_peer else 128
    uv16_d = nc.dram_tensor("uv16", [NEXP, 2 * DM], BF16, kind="Internal").ap()

    with ExitStack() as top:
        S = Sched(nc, top)

        uid = [0]

        def sb(st, name, shape, dt):
            uid[0] += 1
            return st.enter_context(nc.sbuf_tensor("%s_%d" % (name, uid[0]), list(shape), dt))

        def ps(st, name, shape, dt):
            return st.enter_context(nc.psum_tensor(name, list(shape), dt))

        def mm(out, lhsT, rhs, start, stop, reads, writes, skip=False, pe_self=False):
            S.op("pe", lambda e: e.matmul(out, lhsT=lhsT, rhs=rhs, start=start, stop=stop, skip_group_check=skip), reads, writes,
                 pe_self=pe_self)

        def tr(out, in_, ident, reads, writes):
            S.op("pe", lambda e: e.transpose(out=out, in_=in_, identity=ident), reads, writes)

        def act(out, in_, func, reads, writes, bias=None, scale=None, accum_out=None):
            kw = {}
            if bias is not None:
                kw["bias"] = bias
            if scale is not None:
                kw["scale"] = scale
            if accum_out is not None:
                kw["accum_out"] = accum_out
            S.op("act", lambda e: e.activation(out=out, in_=in_, func=func, **kw), reads, writes)

        def acopy(out, in_, reads, writes):
            S.op("act", lambda e: e.copy(out=out, in_=in_), reads, writes)

        def vcopy(out, in_, reads, writes, eng="dve"):
            S.op(eng, lambda e: e.tensor_copy(out=out, in_=in_), reads, writes)

        def vtt(out, in0, in1, op, reads, writes, eng="dve"):
            S.op(eng, lambda e: e.tensor_tensor(out=out, in0=in0, in1=in1, op=op), reads, writes)

        def vts(out, in0, s1, s2, op0, op1, reads, writes, eng="dve"):
            if op1 is None:
                S.op(eng, lambda e: e.tensor_scalar(out=out, in0=in0, scalar1=s1, scalar2=None, op0=op0), reads, writes)
            else:
                S.op(eng, lambda e: e.tensor_scalar(out=out, in0=in0, scalar1=s1, scalar2=s2, op0=op0, op1=op1), reads, writes)

        def vstt(out, in0, scalar, in1, op0, op1, reads, writes):
            S.op("dve", lambda e: e.scalar_tensor_tensor(out=out, in0=in0, scalar=scalar, in1=in1, op0=op0, op1=op1),
                 reads, writes)

        def vrecip(out, in_, reads, writes):
            S.op("dve", lambda e: e.reciprocal(out=out, in_=in_), reads, writes)

        def dma(out, in_, reads, writes, eng="sp", semkey=None):
            S.op(eng, lambda e: e.dma_start(out=out, in_=in_), reads, writes, dma=True, semkey=semkey)

        ident = sb(top, "ident", [128, 128], BF16)
        blockones = sb(top, "blockones", [128, 128], BF16)
        ones = sb(top, "ones", [128, 128], BF16)
        gcols = sb(top, "gcols", [128, 14], F32)
        epsc = sb(top, "epsc", [128, 1], F32)
        tri = sb(top, "tri", [128, 128], BF16)
        pm2 = sb(top, "pm2", [128, 16, 16], F32)
        bcol = sb(top, "bcol", [128, 6], F32)
        iota16 = sb(top, "iota16", [128, 16], F32)
        dma(ident[:], cd["c_ident"], [], ["ident"])
        dma(blockones[:], cd["c_blockones"], [], ["blockones"])
        dma(ones[:], cd["c_ones"], [], ["ones"])
        dma(gcols[:], gcol_d, [], ["gcols"])
        dma(tri[:], cd["c_tri"], [], ["tri"])
        dma(pm2[:], cd["c_pm2"].rearrange("p (a b) -> p a b", a=16), [], ["pm2"])
        dma(bcol[:], cd["c_moba_bcol"], [], ["bcol"])
        dma(iota16[:], cd["c_iota16"], [], ["iota16"])
        S.op("dve", lambda e: e.memset(epsc[:], EPS), [], ["epsc"])

        ps_tr = ps(top, "ps_tr", [128, 8, 128], BF16)
        ps_pj = [ps(top, "ps_pj%d" % i, [128, 512], F32) for i in range(2)]
        ps_ss = ps(top, "ps_ss", [128, 512], F32)
        ps_s = [ps(top, "ps_s%d" % i, [128, 4, 128], F32) for i in range(2)]
        ps_o = [ps(top, "ps_o%d" % i, [128, 4, 128], F32) for i in range(2)]

        def rms_rows_to_T(st, src_dram_tile_fn, ntiles, gslot, dstT, dst_name, tag):
            gbc = sb(st, "gbc" + tag, [128, DM], F32)
            dma(gbc[:], gbc_d[gslot], [], ["gbc" + tag])
            xt = [sb(st, "xt%s%d" % (tag, i), [128, DM], F32) for i in range(2)]
            hn = [sb(st, "hn%s%d" % (tag, i), [128, DM], BF16) for i in range(2)]
            junk = sb(st, "junk" + tag, [128, DM], BF16)
            ssq = sb(st, "ssq" + tag, [128, 2], F32)
            rt = sb(st, "rt" + tag, [128, 2], F32)
            rs = sb(st, "rs" + tag, [128, 2], F32)
            for t in range(ntiles):
                sl = t % 2
                X, H = "xt%s%d" % (tag, sl), "hn%s%d" % (tag, sl)
                dma(xt[sl][:], src_dram_tile_fn(t), [], [X])
                act(junk[:], xt[sl][:], AF.Square, [X], ["junk" + tag, "ssq%s%d" % (tag, sl)], accum_out=ssq[:, sl:sl + 1])
                act(rt[:, sl:sl + 1], ssq[:, sl:sl + 1], AF.Sqrt, ["ssq%s%d" % (tag, sl), "epsc"], ["rt%s%d" % (tag, sl)],
                    bias=epsc[:], scale=1.0 / DM)
                vrecip(rs[:, sl:sl + 1], rt[:, sl:sl + 1], ["rt%s%d" % (tag, sl)], ["rs%s%d" % (tag, sl)])
                vstt(hn[sl][:], xt[sl][:], rs[:, sl:sl + 1], gbc[:], ALU.mult, ALU.mult,
                     [X, "rs%s%d" % (tag, sl), "gbc" + tag], [H])
                for kc in range(8):
                    tr(ps_tr[:, kc, :], hn[sl][:, kc * 128:(kc + 1) * 128], ident[:], [H, "ident"], ["ps_tr"])
                acopy(dstT[:, :, t * 128:(t + 1) * 128], ps_tr[:], ["ps_tr"], [dst_name])

        wctr = [0]
        pend = [None]

        def load_w(st_w, wstage, wdst, wname, w_dram, c0, ncols):
            for cc in range(0, ncols, 128):
                sl = wctr[0] % 2
                wctr[0] += 1
                dma(wstage[sl][:], w_dram[:, c0 + cc:c0 + cc + 128].rearrange("(kc p) n -> p kc n", p=128),
                    [], ["wstage%d" % sl])
                eng = "pool" if (wctr[0] % 2) else "dve"
                vcopy(wdst[:, :, cc:cc + 128], wstage[sl][:], ["wstage%d" % sl], [wname], eng=eng)

        pjctr = [0]

        def proj_norm(wbf, wname, m0, M, src, srcname, ntok, gcol, blk, blkname, dst_fn, dstname, scr):
            sqb, rtb, rsb = scr
            N = min(512, ntok)
            for tc in range(ntok // N):
                sl = pjctr[0] % 2
                pjctr[0] += 1
                P = "ps_pj%d" % sl
                for kc in range(8):
                    mm(ps_pj[sl][0:M, 0:N], wbf[:, kc, m0:m0 + M], src[:, kc, tc * N:(tc + 1) * N], kc == 0, kc == 7,
                       [wname, srcname], [P])
                act(sqb[0:M, 0:N], ps_pj[sl][0:M, 0:N], AF.Square, [P], ["sqb"])
                mm(ps_ss[0:M, 0:N], blk[0:M, 0:M], sqb[0:M, 0:N], True, True, ["sqb", blkname], ["ps_ss"])
                act(rtb[0:M, 0:N], ps_ss[0:M, 0:N], AF.Sqrt, ["ps_ss", "epsc"], ["rtb"], bias=epsc[0:M, :], scale=1.0 / 64)
                vrecip(rsb[0:M, 0:N], rtb[0:M, 0:N], ["rtb"], ["rsb"])
                vstt(dst_fn(tc, N), ps_pj[sl][0:M, 0:N], gcol, rsb[0:M, 0:N], ALU.mult, ALU.mult,
                     [P, "rsb", "gcols"], [dstname])

        def proj_v(wbf, wname, c0, src, srcname, tok_slices, dst, dstname):
            nt = len(tok_slices)
            for jb in range(0, nt, 4):
                sl = pjctr[0] % 2
                pjctr[0] += 1
                P = "ps_pj%d" % sl
                nb = min(4, nt - jb)
                for i in range(nb):
                    for kc in range(8):
                        mm(ps_pj[sl][:, i * 128:(i + 1) * 128], src[:, kc, tok_slices[jb + i]], wbf[:, kc, c0:c0 + 128],
                           kc == 0, kc == 7, [wname, srcname], [P])
                o = dst[:, jb:jb + nb, :]
                i_ = ps_pj[sl][:, 0:nb * 128].rearrange("p (a b) -> p a b", a=nb)
                import os
                if (jb // 4) % 2 == 0 and not os.environ.get("PVDVE"):
                    acopy(o, i_, [P], [dstname])
                else:
                    vcopy(o, i_, [P], [dstname])

        def finalize_pair(acc, o16p, o16name, ssqacc, first, scr):
            sqb, rtb, rsb = scr
            for tc in range(8):
                c = slice(tc * 512, (tc + 1) * 512)
                vrecip(rsb[:, :], acc[:, 1, c], ["acc"], ["rsb"])
                vtt(rtb[:, :], acc[:, 0, c], rsb[:, :], ALU.mult, ["acc", "rsb"], ["rtb"])
                act(sqb[:, :], rtb[:, :], AF.Square, ["rtb"], ["sqb"])
                vcopy(o16p[:, c], rtb[:, :], ["rtb"], [o16name], eng="pool")
                mm(ps_ss[:, :], ones[:], sqb[:, :], True, True, ["sqb", "ones"], ["ps_ss"])
                if first:
                    vcopy(ssqacc[:, c], ps_ss[:, :], ["ps_ss"], ["ssqacc"])
                else:
                    vtt(ssqacc[:, c], ps_ss[:, :], ssqacc[:, c], ALU.add, ["ps_ss", "ssqacc"], ["ssqacc"])

        def group_norm_store(b, o16, npairs, nfeat, ssqacc, chunk0, ybuf, scr):
            sqb, rtb, rsb = scr
            for tc in range(8):
                c = slice(tc * 512, (tc + 1) * 512)
                act(rtb[:, :], ssqacc[:, c], AF.Sqrt, ["ssqacc", "epsc"], ["rtb"], bias=epsc[:], scale=1.0 / nfeat)
                vrecip(ssqacc[:, c], rtb[:, :], ["rtb"], ["ssqacc"])
            for p in range(npairs):
                Y = "o16_%d" % p
                for tc in range(8):
                    c = slice(tc * 512, (tc + 1) * 512)
                    vstt(o16[p][:, c], o16[p][:, c], gcols[:, 6 + chunk0 + p:7 + chunk0 + p], ssqacc[:, c],
                         ALU.mult, ALU.mult, [Y, "ssqacc", "gcols"], [Y])
                dma(yscr_d[b, chunk0 + p], o16[p][:], [Y], ["yscr%d_%d" % (b, chunk0 + p)])

        def attn_tile(sl, qk_list, v_list, acc_view, acc_first, exp_bias=None, evac_eng="dve", acc_part=None):
            Ps = "ps_s%d" % sl
            nslots = 0
            prev_base = None
            for (si, kT_ap, qT_ap, knames, extras) in qk_list:
                nslots = max(nslots, si + 1)
                base = kT_ap.base_partition()
                mm(ps_s[sl][:, si, :], kT_ap, qT_ap, True, len(extras) == 0, knames, [Ps],
                   pe_self=(prev_base is not None and base != prev_base))
                prev_base = base
                for ei, (l_, r_, nm) in enumerate(extras):
                    mm(ps_s[sl][:, si, :], l_, r_, False, ei == len(extras) - 1, nm, [Ps])
            PT = "pT%d" % sl
            used = sorted(q[0] for q in qk_list)
            if used == list(range(nslots)):
                ssel = slice(0, nslots)
            else:
                step = used[1] - used[0] if len(used) > 1 else 1
                assert used == list(range(used[0], used[-1] + 1, step))
                ssel = slice(used[0], used[-1] + 1, step)
            if exp_bias is None:
                act(pT[sl][:, ssel, :], ps_s[sl][:, ssel, :], AF.Exp, [Ps], [PT], scale=SCALE)
            else:
                act(pT[sl][:, ssel, :], ps_s[sl][:, ssel, :], AF.Exp, [Ps, "bcol"], [PT], scale=SCALE, bias=exp_bias)
            return PT


        NU = 8

        def peer(b):
            with ExitStack() as st:
                wout = sb(st, "wout", [128, 8, 1024], BF16)
                wpq = sb(st, "wpq", [128, 8, 2048], BF16)
                subT = sb(st, "subT", [128, 16, 128], BF16)
                wstage = [sb(st, "wstageP%d" % i, [128, 8, 128], F32) for i in range(2)]
                gffn = sb(st, "gffn", [128, DM], F32)
                dma(gffn[:], gbc_d[1], [], ["gffn"])
                wctr[0] = 0
                for cc in range(0, 1024, 128):
                    sl = (cc // 128) % 2
                    dma(wstage[sl][:], w_out_d[:, cc:cc + 128].rearrange("(kc p) n -> p kc n", p=128), [], ["wstageP%d" % sl])
                    vcopy(wout[:, :, cc:cc + 128], wstage[sl][:], ["wstageP%d" % sl], ["wout"], eng=("pool" if sl else "dve"))
                for cc in range(0, 2048, 128):
                    sl = (cc // 128) % 2
                    dma(wstage[sl][:], w_pq_d[:, cc:cc + 128].rearrange("(kc p) n -> p kc n", p=128), [], ["wstageP%d" % sl])
                    vcopy(wpq[:, :, cc:cc + 128], wstage[sl][:], ["wstageP%d" % sl], ["wpq"], eng=("pool" if sl else "dve"))

                ytc = [sb(st, "ytc%d" % i, [128, 8, 512], BF16) for i in range(2)]
                xt = [sb(st, "xtP%d" % i, [128, DM], F32) for i in range(2)]
                x1 = [sb(st, "x1_%d" % i, [128, DM], F32) for i in range(2)]
                hn2b = sb(st, "hn2b", [128, DM], BF16)
                junkb = sb(st, "junkP", [128, DM], BF16)
                hn2T = sb(st, "hn2T", [128, 8, 128], BF16)
                qryT = sb(st, "qryT", [128, 16, 128], BF16)
                sc = sb(st, "sc", [128, 16, 128], F32)
                dma(sc[:], sub_d.rearrange("c k d -> k c d"), [], ["sc"])
                vcopy(qryT[:], sc[:], ["sc"], ["qryT"])
                for c8 in range(2):
                    for i in range(8):
                        tr(ps_tr[:, i, :], qryT[:, c8 * 8 + i, :], ident[:], ["qryT", "ident"], ["ps_tr"])
                    acopy(subT[:, c8 * 8:(c8 + 1) * 8, :], ps_tr[:], ["ps_tr"], ["subT"])
                wk1 = sb(st, "wk1", [128, 128], F32)
                v12 = sb(st, "v12", [128, 8, 2, 16], F32)
                i12 = sb(st, "i12", [128, 8, 2, 16], U32)
                i12f = sb(st, "i12f", [128, 8, 2, 16], F32)
                cand = sb(st, "cand", [128, 8, 256], F32)
                wk2 = sb(st, "wk2", [128, 256], F32)
                ts = sb(st, "ts", [128, 8, 16], F32)
                pos = sb(st, "pos", [128, 8, 16], U32)
                pab = sb(st, "pab", [128, 2, 8, 16], U32)
                pabf = sb(st, "pabf", [128, 2, 8, 16], F32)
                oh = sb(st, "oh", [128, 8, 16, 16], F32)
                e12 = sb(st, "e12", [128, 2, 8, 16], F32)
                ef = sb(st, "ef", [128, 128], F32)
                idx = sb(st, "idx", [128, 128], U32)
                ex = sb(st, "ex", [128, 8, 16], F32)
                gs = sb(st, "gs", [128, 8], F32)
                gate = sb(st, "gate", [128, 128], F32)
                aact = sb(st, "aact", [128, 128], F32)
                wgt = sb(st, "wgt", [128, 128], F32)
                ssq = sb(st, "ssqP", [128, 1], F32)
                rt = sb(st, "rtP", [128, 1], F32)
                rs = sb(st, "rsP", [128, 1], F32)
                uvb = [sb(st, "uvb%d" % i, [128, 2 * DM], BF16) for i in range(NU)]
                ga = sb(st, "ga", [128, 128], F32)
                dg = [sb(st, "dg%d" % i, [128, 128], BF16) for i in range(4)]
                yres = ["yscr%d_%d" % (b, c) for c in range(8)]

                hn2bs = [hn2b, sb(st, "hn2b_b", [128, DM], BF16)]
                idxs = [idx, sb(st, "idx_b", [128, 128], U32)]
                gates = [gate, sb(st, "gate_b", [128, 128], F32)]

                def stage1(t):
                    sl = t % 2
                    X, X1 = "xtP%d" % sl, "x1_%d" % sl
                    HB, IDX, GT = "hn2b%d" % sl, "idx%d" % sl, "gate%d" % sl
                    hn2b_, idx_, gate_ = hn2bs[sl], idxs[sl], gates[sl]
                    ysl = (t // 4) % 2
                    YT = "ytc%d" % ysl
                    if t % 4 == 0:
                        dma(ytc[ysl][:], yscr_d[b, :, :, t * 128:t * 128 + 512].rearrange("c p n -> p c n"), yres, [YT])
                    dma(xt[sl][:], x_d[b, t * 128:(t + 1) * 128, :], [], [X])
                    yield
                    tcol = slice((t % 4) * 128, (t % 4 + 1) * 128)
                    for nh in range(2):
                        for kc in range(8):
                            mm(ps_o[nh][:].rearrange("p a b -> p (a b)"), ytc[ysl][:, kc, tcol], wout[:, kc, nh * 512:(nh + 1) * 512],
                               kc == 0, kc == 7, [YT, "wout"], ["ps_o%d" % nh])
                        yield
                    for nh in range(2):
                        c = slice(nh * 512, (nh + 1) * 512)
                        vtt(x1[sl][:, c], ps_o[nh][:].rearrange("p a b -> p (a b)"), xt[sl][:, c], ALU.add, ["ps_o%d" % nh, X], [X1])
                        yield
                    act(junkb[:], x1[sl][:], AF.Square, [X1], ["junkP", "ssqP"], accum_out=ssq[:, 0:1])
                    act(rt[:], ssq[:], AF.Sqrt, ["ssqP", "epsc"], ["rtP"], bias=epsc[:], scale=1.0 / DM)
                    vrecip(rs[:], rt[:], ["rtP"], ["rsP"])
                    yield
                    vstt(hn2b_[:], x1[sl][:], rs[:, 0:1], gffn[:], ALU.mult, ALU.mult, [X1, "rsP", "gffn"], [HB])
                    yield
                    for kc in range(8):
                        tr(ps_tr[:, kc, :], hn2b_[:, kc * 128:(kc + 1) * 128], ident[:], [HB, "ident"], ["ps_tr"])
                    acopy(hn2T[:], ps_tr[:], ["ps_tr"], ["hn2T"])
                    yield
                    for c4 in range(4):
                        q_ = c4 % 2
                        for i in range(4):
                            c = c4 * 4 + i
                            for kc in range(8):
                                mm(ps_s[q_][:, i, :], wpq[:, kc, c * 128:(c + 1) * 128], hn2T[:, kc, :], kc == 0, kc == 7,
                                   ["wpq", "hn2T"], ["ps_s%d" % q_])
                            yield
                        acopy(qryT[:, c4 * 4:(c4 + 1) * 4, :], ps_s[q_][:], ["ps_s%d" % q_], ["qryT"])
                    for c4 in range(4):
                        q_ = c4 % 2
                        for i in range(4):
                            c = c4 * 4 + i
                            mm(ps_s[q_][:, i, :], qryT[:, c, :], subT[:, c, :], True, True, ["qryT", "subT"], ["ps_s%d" % q_])
                        acopy(sc[:, c4 * 4:(c4 + 1) * 4, :], ps_s[q_][:], ["ps_s%d" % q_], ["sc"])
                        yield
                    for h in range(8):
                        for hf in range(2):
                            a_ = sc[:, 2 * h + hf, :]
                            S.op("dve", lambda e, a_=a_, h=h, hf=hf: e.max(out=v12[:, h, hf, 0:8], in_=a_), ["sc"], ["v12"])
                            yield
                            S.op("dve", lambda e, a_=a_, h=h, hf=hf: e.match_replace(out=wk1[:], in_to_replace=v12[:, h, hf, 0:8],
                                                                                   in_values=a_, imm_value=-1e30), ["sc", "v12"], ["wk1"])
                            yield
                            S.op("dve", lambda e, h=h, hf=hf: e.max(out=v12[:, h, hf, 8:16], in_=wk1[:]), ["wk1"], ["v12"])
                            yield
                            S.op("dve", lambda e, a_=a_, h=h, hf=hf: e.max_index(out=i12[:, h, hf, 0:8], in_max=v12[:, h, hf, 0:8],
                                                                               in_values=a_), ["sc", "v12"], ["i12"])
                            yield
                            S.op("dve", lambda e, a_=a_, h=h, hf=hf: e.max_index(out=i12[:, h, hf, 8:16], in_max=v12[:, h, hf, 8:16],
                                                                               in_values=a_), ["sc", "v12"], ["i12"])
                            yield
                        cv = cand[:, h, :].rearrange("p (a b) -> p a b", a=16)
                        vtt(cv, v12[:, h, 0, :].unsqueeze(2).to_broadcast([128, 16, 16]),
                            v12[:, h, 1, :].unsqueeze(1).to_broadcast([128, 16, 16]), ALU.add, ["v12"], ["cand"])
                        yield
                        ch = cand[:, h, :]
                        S.op("dve", lambda e, ch=ch, h=h: e.max(out=ts[:, h, 0:8], in_=ch), ["cand"], ["ts"])
                        yield
                        S.op("dve", lambda e, ch=ch, h=h: e.match_replace(out=wk2[:], in_to_replace=ts[:, h, 0:8], in_values=ch,
                                                                        imm_value=-1e30), ["cand", "ts"], ["wk2"])
                        yield
                        S.op("dve", lambda e, h=h: e.max(out=ts[:, h, 8:16], in_=wk2[:]), ["wk2"], ["ts"])
                        yield
                        S.op("dve", lambda e, ch=ch, h=h: e.max_index(out=pos[:, h, 0:8], in_max=ts[:, h, 0:8], in_values=ch),
                             ["cand", "ts"], ["pos"])
                        yield
                        S.op("dve", lambda e, ch=ch, h=h: e.max_index(out=pos[:, h, 8:16], in_max=ts[:, h, 8:16], in_values=ch),
                             ["cand", "ts"], ["pos"])
                        yield
                    vts(pab[:, 0], pos[:], 4, None, ALU.logical_shift_right, None, ["pos"], ["pab"])
                    vts(pab[:, 1], pos[:], 15, None, ALU.bitwise_and, None, ["pos"], ["pab"])
                    yield
                    vcopy(pabf[:], pab[:], ["pab"], ["pabf"])
                    vcopy(i12f[:], i12[:], ["i12"], ["i12f"])
                    yield
                    for w_ in range(2):
                        vtt(oh[:], pabf[:, w_].unsqueeze(3).to_broadcast([128, 8, 16, 16]),
                            iota16[:].unsqueeze(1).unsqueeze(1).to_broadcast([128, 8, 16, 16]), ALU.is_equal,
                            ["pabf", "iota16"], ["oh"])
                        yield
                        vtt(oh[:], oh[:], i12f[:, :, w_, :].unsqueeze(2).to_broadcast([128, 8, 16, 16]), ALU.mult,
                            ["oh", "i12f"], ["oh"])
                        yield
                        S.op("dve", lambda e, w_=w_: e.tensor_reduce(out=e12[:, w_], in_=oh[:], axis=AX.X, op=ALU.add), ["oh"], ["e12"])
                        yield
                    vstt(ef[:], e12[:, 0].rearrange("p a b -> p (a b)"), 128.0, e12[:, 1].rearrange("p a b -> p (a b)"),
                         ALU.mult, ALU.add, ["e12"], ["ef"])
                    vcopy(idx_[:], ef[:], ["ef"], [IDX])
                    yield
                    vtt(ex[:], ts[:], ts[:, :, 0:1].to_broadcast([128, 8, 16]), ALU.subtract, ["ts"], ["ex"])
                    act(ex[:], ex[:], AF.Exp, ["ex"], ["ex"])
                    yield
                    S.op("dve", lambda e: e.tensor_reduce(out=gs[:], in_=ex[:], axis=AX.X, op=ALU.add), ["ex"], ["gs"])
                    vrecip(gs[:], gs[:], ["gs"], ["gs"])
                    vtt(gate_[:].rearrange("p (a b) -> p a b", a=8), ex[:], gs[:].unsqueeze(2).to_broadcast([128, 8, 16]), ALU.mult,
                        ["ex", "gs"], [GT])
                    yield

                def advance(g, n=1):
                    if g is None:
                        return None
                    try:
                        for _ in range(n):
                            next(g)
                    except StopIteration:
                        return None
                    return g

                g = stage1(0)
                while g is not None:
                    g = advance(g, 8)
                for t in range(peer_tiles):
                    sl = t % 2
                    X1 = "x1_%d" % sl
                    HB, IDX, GT = "hn2b%d" % sl, "idx%d" % sl, "gate%d" % sl
                    hn2b_, idx_, gate_ = hn2bs[sl], idxs[sl], gates[sl]
                    g = stage1(t + 1) if t + 1 < peer_tiles else None
                    LAG = 3
                    for j in range(128 + LAG):
                        if j < 128:
                            u_ = j % NU
                            A_, G_ = "aact%d" % (j % 8), "ga%d" % (j % 8)
                            S.op("pool", lambda e, u_=u_, j=j, idx_=idx_: e.indirect_dma_start(
                                out=uvb[u_][:], out_offset=None, in_=uv16_d,
                                in_offset=bass.IndirectOffsetOnAxis(ap=idx_[:, j:j + 1], axis=0)),
                                [IDX, "uv16"], ["uvb%d" % u_], dma=True)
                            S.op("dve", lambda e, u_=u_, j=j, hn2b_=hn2b_: e.scalar_tensor_tensor(
                                out=junkb[:], in0=uvb[u_][:, 0:DM], scalar=1.0, in1=hn2b_[:], op0=ALU.mult, op1=ALU.mult,
                                accum_out=aact[:, j:j + 1]), ["uvb%d" % u_, HB], ["junkP", A_])
                            act(ga[:, j:j + 1], aact[:, j:j + 1], AF.Gelu, [A_], [G_])
                        jj = j - LAG
                        if jj >= 0:
                            u2 = jj % NU
                            d_ = jj % 4
                            vts(dg[d_][:], ident[:], ga[:, jj:jj + 1], gate_[:, jj:jj + 1], ALU.mult, ALU.mult,
                                ["ident", "ga%d" % (jj % 8), GT], ["dg%d" % d_])
                            for nh in range(2):
                                mm(ps_pj[nh][:, :], dg[d_][:], uvb[u2][:, DM + nh * 512:DM + (nh + 1) * 512], jj == 0, jj == 127,
                                   ["dg%d" % d_, "uvb%d" % u2], ["ps_pj%d" % nh], skip=True)
                        g = advance(g, 2 if (j % 2) else 1)
                    while g is not None:
                        g = advance(g, 8)
                    for nh in range(2):
                        c = slice(nh * 512, (nh + 1) * 512)
                        vtt(x1[sl][:, c], ps_pj[nh][:, :], x1[sl][:, c], ALU.add, ["ps_pj%d" % nh, X1], [X1])
                    dma(out_d[b, t * 128:(t + 1) * 128, :], x1[sl][:], [X1], ["out%d" % sl], semkey=("dma", "out%d" % sl))
                S.barrier()

        if do_peer:
            with ExitStack() as cst_:
                stg = [sb(cst_, "cstg%d" % i, [128, 2, DM], F32) for i in range(3)]
                cbf = [sb(cst_, "cbf%d" % i, [128, 2, DM], BF16) for i in range(3)]
                k_ = 0
                for (src_, dst_, nm_) in ((pu_d, uv16_d[:, 0:DM], "uv16"), (pv_d, uv16_d[:, DM:2 * DM], "uv16")):
                    for ch in range(NEXP // 256):
                        s_ = k_ % 3
                        k_ += 1
                        rows = slice(ch * 256, (ch + 1) * 256)
                        dma(stg[s_][:], src_[rows, :].rearrange("(p r) d -> p r d", r=2), [], ["cstg%d" % s_])
                        if s_ == 0:
                            vcopy(cbf[s_][:], stg[s_][:], ["cstg%d" % s_], ["cbf%d" % s_])
                        elif s_ == 1:
                            acopy(cbf[s_][:], stg[s_][:], ["cstg%d" % s_], ["cbf%d" % s_])
                        else:
                            vcopy(cbf[s_][:], stg[s_][:], ["cstg%d" % s_], ["cbf%d" % s_], eng="pool")
                        dma(dst_[rows, :].rearrange("(p r) d -> p r d", r=2), cbf[s_][:], ["cbf%d" % s_], [nm_],
                            semkey=("dma", "cbfo%d" % s_))
                S.barrier()

        for b in range(nseq):
            with ExitStack() as seqst:
                hnT = sb(seqst, "hnT", [128, 8, SEQ], BF16)
                with ExitStack() as st:
                    rms_rows_to_T(st, lambda t: x_d[b, t * 128:(t + 1) * 128, :], NT, 0, hnT, "hnT", "A")
                    if debug:
                        dma(dbg_hnT, hnT[:], ["hnT"], ["dbg_hnT"])
                    S.barrier()
                with ExitStack() as st:
                    wstage = [sb(st, "wstage%d" % i, [128, 8, 128], F32) for i in range(2)]
                    wq = sb(st, "wq", [128, 8, 128], BF16)
                    wk = sb(st, "wk", [128, 8, 128], BF16)
                    wv = sb(st, "wv", [128, 8, 128], BF16)
                    sqb = sb(st, "sqb", [128, 512], BF16)
                    rtb = sb(st, "rtb", [128, 512], F32)
                    rsb = sb(st, "rsb", [128, 512], F32)
                    scr = (sqb, rtb, rsb)
                    acc = sb(st, "acc", [128, 2, SEQ], F32)
                    ssqacc = sb(st, "ssqacc", [128, SEQ], F32)
                    o16 = [sb(st, "o16_%d" % i, [128, SEQ], BF16) for i in range(3)]
                    pT = [sb(st, "pT%d" % i, [128, 4, 128], BF16) for i in range(2)]
                    ybuf = o16
                    Vd = sb(st, "Vd", [128, NT, 128], BF16)

                    with ExitStack() as gst:
                      if "dil" in stages:
                        qT = sb(gst, "qT", [128, SEQ], BF16)
                        kT = sb(gst, "kT", [128, SEQ], BF16)
                        dbias = sb(gst, "dbias", [128, 24, 128], BF16)
                        for p in range(min(3, npair)):
                            dma(dbias[:], cd["c_dilbias"][p].rearrange("k (a q) -> k a q", a=24), [], ["dbias"])
                            load_w(st, wstage, wq, "wq", w_in_d, 0 + p * 128, 128)
                            load_w(st, wstage, wk, "wk", w_in_d, 384 + p * 128, 128)
                            load_w(st, wstage, wv, "wv", w_in_d, 768 + p * 128, 128)
                            proj_norm(wq, "wq", 0, 128, hnT, "hnT", SEQ, gcols[:, 0:1], blockones, "blockones",
                                      lambda tc, N: qT[:, tc * N:(tc + 1) * N], "qT", scr)
                            proj_norm(wk, "wk", 0, 128, hnT, "hnT", SEQ, gcols[:, 1:2], blockones, "blockones",
                                      lambda tc, N: kT[:, tc * N:(tc + 1) * N], "kT", scr)
                            for di, d in enumerate(DIL):
                                nblk = NT // d
                                tsl = []
                                for j in range(NT):
                                    r, n = divmod(j, nblk)
                                    base = d * 128 * n + r
                                    tsl.append(slice(base, base + 127 * d + 1, d))
                                proj_v(wv, "wv", 0, hnT, "hnT", tsl, Vd, "Vd")
                                for j in range(NT):
                                    r, n = divmod(j, nblk)
                                    sl = j % 2
                                    have_prev = n > 0
                                    qk = []
                                    for h in range(2):
                                        hp = 64 * h
                                        for jj in range(2):
                                            if jj == 0 and not have_prev:
                                                continue
                                            tk = tsl[j - 1] if jj == 0 else tsl[j]
                                            bi = ((h * 3 + di) * 2 + 0) * 2 + jj
                                            bl = ((h * 3 + di) * 2 + 1) * 2 + jj
                                            qk.append((h * 2 + jj, kT[hp:hp + 64, tk], qT[hp:hp + 64, tsl[j]], ["kT", "qT"],
                                                       [(ident[:], dbias[:, bi, :], ["ident", "dbias"]),
                                                        (ident[:], dbias[:, bl, :], ["ident", "dbias"])]))
                                    PT = attn_tile(sl, qk, None, None, None)

                                    def pv_dil(j=j, sl=sl, have_prev=have_prev, PT=PT, di=di, tsl=tsl):
                                        Po = "ps_o%d" % sl
                                        for h in range(2):
                                            hp = 64 * h
                                            jjs = [1] if not have_prev else [0, 1]
                                            for jj in jjs:
                                                jk = j - 1 if jj == 0 else j
                                                mm(ps_o[sl][hp:hp + 64, 0, :], Vd[:, jk, hp:hp + 64], pT[sl][:, h * 2 + jj, :],
                                                   jj == jjs[0], jj == 1, ["Vd", PT], [Po])
                                            for jj in jjs:
                                                mm(ps_o[sl][hp:hp + 64, 1, :], ones[:, 0:64], pT[sl][:, h * 2 + jj, :],
                                                   jj == jjs[0], jj == 1, ["ones", PT], [Po])
                                        av = acc[:, :, tsl[j]]
                                        if di == 0:
                                            vcopy(av, ps_o[sl][:, 0:2, :], [Po], ["acc"])
                                        else:
                                            vtt(av, ps_o[sl][:, 0:2, :], av, ALU.add, [Po, "acc"], ["acc"])
                                    if pend[0] is not None:
                                        pend[0]()
                                    pend[0] = pv_dil
                                if pend[0] is not None:
                                    pend[0]()
                                    pend[0] = None
                            finalize_pair(acc, o16[p], "o16_%d" % p, ssqacc, p == 0, scr)
                        group_norm_store(b, o16, min(3, npair), 384, ssqacc, 0, ybuf, scr)
                        S.barrier()

                    with ExitStack() as gst:
                      if "moba" in stages:
                        qa = sb(gst, "qa", [128, SEQ], BF16)
                        ka = sb(gst, "ka", [128, SEQ], BF16)
                        km32 = sb(gst, "km32", [64, 16], F32)
                        kmT = sb(gst, "kmT", [64, 16], BF16)
                        gm = sb(gst, "gm", [128, 16], F32)
                        mx8 = sb(gst, "mx8", [128, 8], F32)
                        pen = sb(gst, "pen", [128, 16], F32)
                        penb = sb(gst, "penb", [128, 16], BF16)
                        dma(ka[64:86, :], cd["c_moba_k"], [], ["ka_c"])
                        tsl = [slice(j * 128, (j + 1) * 128) for j in range(NT)]
                        for p in range(min(3, npair)):
                            load_w(st, wstage, wq, "wq", w_in_d, 1152 + p * 128, 128)
                            load_w(st, wstage, wk, "wk", w_in_d, 1536 + p * 128, 128)
                            load_w(st, wstage, wv, "wv", w_in_d, 1920 + p * 128, 128)
                            proj_v(wv, "wv", 0, hnT, "hnT", tsl, Vd, "Vd")
                            for h in range(2):
                                H = 2 * p + h
                                hp = 64 * h
                                dma(qa[80:86, :], cd["c_moba_q"][H], [], ["qa_c"])
                                proj_norm(wq, "wq", hp, 64, hnT, "hnT", SEQ, gcols[0:64, 2:3], ones, "ones",
                                          lambda tc, N: qa[0:64, tc * N:(tc + 1) * N], "qa", scr)
                                proj_norm(wk, "wk", hp, 64, hnT, "hnT", SEQ, gcols[0:64, 3:4], ones, "ones",
                                          lambda tc, N: ka[0:64, tc * N:(tc + 1) * N], "ka", scr)
                                S.op("dve", lambda e: e.tensor_reduce(out=km32[:, :], in_=ka[0:64, :].rearrange("p (a b) -> p a b", a=16),
                                                                      axis=AX.X, op=ALU.add), ["ka"], ["km32"])
                                vts(kmT[:, :], km32[:, :], 1.0 / 256, None, ALU.mult, None, ["km32"], ["kmT"])
                                for t in range(NT):
                                    mm(ps_ss[:, 0:16], qa[0:64, tsl[t]], kmT[:, :], True, True, ["qa", "kmT"], ["ps_ss"])
                                    vtt(gm[:, :], ps_ss[:, 0:16], pm2[:, t // 2, :], ALU.add, ["ps_ss", "pm2"], ["gm"])
                                    S.op("dve", lambda e: e.max(out=mx8[:, :], in_=gm[:, :]), ["gm"], ["mx8"])
                                    vts(pen[:, :], gm[:, :], mx8[:, 3:4], None, ALU.is_ge, None, ["gm", "mx8"], ["pen"])
                                    vts(penb[:, :], pen[:, :], -NEGM, NEGM, ALU.mult, ALU.add, ["pen"], ["penb"])
                                    mm(ps_ss[64:80, 128:256], penb[:, :], ident[:], True, True, ["penb", "ident"], ["ps_ss"])
                                    acopy(qa[64:80, tsl[t]], ps_ss[64:80, 128:256], ["ps_ss"], ["qa_p"])
                                cnt = 0
                                for t in range(NT):
                                    osl = t % 2
                                    Po = "ps_o%d" % osl
                                    for b0 in range(0, t + 1, 4):
                                        nb = min(4, t + 1 - b0)
                                        sl = cnt % 2
                                        cnt += 1
                                        qk = []
                                        for i in range(nb):
                                            kt = b0 + i
                                            ex = [(ident[:], tri[:], ["ident", "tri"])] if kt == t else []
                                            qk.append((i, ka[0:86, tsl[kt]], qa[0:86, tsl[t]], ["ka", "ka_c", "qa", "qa_c", "qa_p"], ex))
                                        PT = attn_tile(sl, qk, None, None, None, exp_bias=bcol[:, H:H + 1])

                                        def pv_moba(t=t, b0=b0, nb=nb, sl=sl, osl=osl, Po=Po, PT=PT, hp=hp, tsl=tsl):
                                            for i in range(nb):
                                                kt = b0 + i
                                                mm(ps_o[osl][hp:hp + 64, 0, :], Vd[:, kt, hp:hp + 64], pT[sl][:, i, :], kt == 0, kt == t,
                                                   ["Vd", PT], [Po], skip=True)
                                                mm(ps_o[osl][hp:hp + 64, 1, :], ones[:, 0:64], pT[sl][:, i, :], False, kt == t,
                                                   ["ones", PT], [Po], skip=True)
                                            if b0 + nb == t + 1:
                                                acopy(acc[hp:hp + 64, :, tsl[t]], ps_o[osl][hp:hp + 64, 0:2, :], [Po], ["acc"])
                                        if pend[0] is not None:
                                            pend[0]()
                                        pend[0] = pv_moba
                                if pend[0] is not None:
                                    pend[0]()
                                    pend[0] = None
                            finalize_pair(acc, o16[p], "o16_%d" % p, ssqacc, p == 0, scr)
                        group_norm_store(b, o16, min(3, npair), 384, ssqacc, 3, ybuf, scr)
                        S.barrier()

                    with ExitStack() as gst:
                      if "mem" in stages:
                        memT = sb(gst, "memT", [128, 8, 256], BF16)
                        import os
                        MEMCUT = int(os.environ.get("MEMCUT", "9"))
                        rms_rows_to_T(gst, lambda t: mem_d[b, t * 128:(t + 1) * 128, :], 2, 2, memT, "memT", "M")
                        qT = sb(gst, "qTm", [128, SEQ], BF16)
                        kmem = sb(gst, "kmem", [128, 256], BF16)
                        Vm = sb(gst, "Vm", [128, 2, 128], BF16)
                        tsl = [slice(j * 128, (j + 1) * 128) for j in range(NT)]
                        for p in range(min(2, npair) if MEMCUT > 1 else 0):
                            load_w(st, wstage, wq, "wq", w_in_d, 2304 + p * 128, 128)
                            load_w(st, wstage, wk, "wk", w_mkv_d, 0 + p * 128, 128)
                            load_w(st, wstage, wv, "wv", w_mkv_d, 256 + p * 128, 128)
                            proj_norm(wq, "wq", 0, 128, hnT, "hnT", SEQ, gcols[:, 4:5], blockones, "blockones",
                                      lambda tc, N: qT[:, tc * N:(tc + 1) * N], "qTm", scr)
                            if MEMCUT <= 2:
                                continue
                            proj_norm(wk, "wk", 0, 128, memT, "memT", 256, gcols[:, 5:6], blockones, "blockones",
                                      lambda tc, N: kmem[:, tc * N:(tc + 1) * N], "kmem", scr)
                            if MEMCUT <= 3:
                                continue
                            if os.environ.get("PVA"):
                                proj_v(wv, "wv", 0, memT, "memT", tsl[0:2], Vd, "Vd")
                            elif os.environ.get("PVB"):
                                proj_v(wv, "wv", 0, hnT, "hnT", tsl[0:2], Vm, "Vm")
                            else:
                                proj_v(wv, "wv", 0, memT, "memT", tsl[0:2], Vm, "Vm")
                            if MEMCUT <= 4:
                                continue
                            for t in range(NT):
                                sl = t % 2
                                Po = "ps_o%d" % sl
                                qk = []
                                for h in range(2):
                                    hp = 64 * h
                                    for jj in range(2):
                                        qk.append((h * 2 + jj, kmem[hp:hp + 64, tsl[jj]], qT[hp:hp + 64, tsl[t]], ["kmem", "qTm"], []))
                                PT = attn_tile(sl, qk, None, None, None)

                                def pv_mem(t=t, sl=sl, Po=Po, PT=PT, tsl=tsl):
                                    for h in range(2):
                                        hp = 64 * h
                                        for jj in range(2):
                                            mm(ps_o[sl][hp:hp + 64, 0, :], Vm[:, jj, hp:hp + 64], pT[sl][:, h * 2 + jj, :], jj == 0, jj == 1,
                                               ["Vm", PT], [Po])
                                        for jj in range(2):
                                            mm(ps_o[sl][hp:hp + 64, 1, :], ones[:, 0:64], pT[sl][:, h * 2 + jj, :], jj == 0, jj == 1,
                                               ["ones", PT], [Po])
                                    vcopy(acc[:, :, tsl[t]], ps_o[sl][:, 0:2, :], [Po], ["acc"])
                                if pend[0] is not None:
                                    pend[0]()
                                pend[0] = pv_mem
                            if pend[0] is not None:
                                pend[0]()
                                pend[0] = None
                            finalize_pair(acc, o16[p], "o16_%d" % p, ssqacc, p == 0, scr)
                        group_norm_store(b, o16, min(2, npair), 256, ssqacc, 6, ybuf, scr)
                        S.barrier()
            S.barrier()
            if do_peer:
                peer(b)
        S.barrier()
        S.emit()
    return nc, cst


def _host_inputs(inputs, cst, nseq, core, do_peer=True):
    f = lambda a: np.ascontiguousarray(np.asarray(a, dtype=np.float32))
    m = {}
    m["x"] = f(inputs["x"][core * nseq:(core + 1) * nseq])
    m["mem"] = f(inputs["mem"][core * nseq:(core + 1) * nseq])
    m["w_in"] = f(inputs["w_in"][0])
    m["w_mem_kv"] = f(inputs["w_mem_kv"][0])
    m["w_out"] = f(inputs["w_out"][0])
    m["w_peer_q"] = f(inputs["w_peer_q"][0])
    s1 = np.asarray(inputs["peer_subkeys_1"][0]); s2 = np.asarray(inputs["peer_subkeys_2"][0])
    m["subkeys"] = f(np.stack([s1, s2], axis=1).reshape(16, 128, 128))
    m["peer_u"] = f(inputs["peer_u"][0] if do_peer else inputs["peer_u"][0][:128])
    m["peer_v"] = f(inputs["peer_v"][0] if do_peer else inputs["peer_v"][0][:128])
    gb = np.stack([np.broadcast_to(np.asarray(inputs[k][0]), (128, DM)) for k in ("g_mix", "g_ffn", "g_memtok")])
    m["g_bc"] = f(gb)
    cols = []
    for k in ("qg_dil", "kg_dil", "qg_moba", "kg_moba", "qg_mem", "kg_mem"):
        cols.append(np.tile(np.asarray(inputs[k][0]), 2))
    og = np.concatenate([np.asarray(inputs["og_dil"][0]), np.asarray(inputs["og_moba"][0]), np.asarray(inputs["og_mem"][0])])
    for c in range(8):
        cols.append(og[c * 128:(c + 1) * 128])
    m["g_cols"] = f(np.stack(cols, axis=1))
    for k, v in cst.items():
        m[k] = v
    return m


_CACHE = {}


def kernel(**inputs):
    nseq = 2
    if "prog" not in _CACHE:
        _CACHE["prog"] = build_program(nseq=nseq)
    nc, cst = _CACHE["prog"]
    in_maps = [_host_inputs(inputs, cst, nseq, c) for c in range(NCORES)]
    res = run_bass_kernel_spmd(nc, in_maps, core_ids=list(range(NCORES)))
    out = np.concatenate([np.asarray(r["out"]) for r in res.results], axis=0)
    return out.astype(np.float32)
```

```python
import math
from contextlib import ExitStack

import numpy as np
import ml_dtypes

import concourse.bass as bass
import concourse.mybir as mybir
from concourse.bass_utils import run_bass_kernel_spmd

F32 = mybir.dt.float32
BF16 = mybir.dt.bfloat16
U32 = mybir.dt.uint32
I32 = mybir.dt.int32
AF = mybir.ActivationFunctionType
ALU = mybir.AluOpType
AX = mybir.AxisListType

NCORES = 8
SEQ = 4096
DM = 1024
NT = SEQ // 128
SCALE = 0.125
EPS = 1e-6
NEGM = -30000.0
SEM_LIMIT = 32000
DIL = (1, 4, 16)


class Sched:
    ENGS = ("pe", "act", "dve", "pool", "sp")

    def __init__(self, nc, stack):
        self.nc = nc
        self.stack = stack
        self.ops = {e: [] for e in self.ENGS}
        self.cnt = {}
        self.sems = {}
        self.last_w = {}
        self.readers = {}
        self.seen = {e: {} for e in self.ENGS}
        self.nops = 0

    def _sem(self, key, epoch):
        k = (key, epoch)
        if k not in self.sems:
            self.sems[k] = self.stack.enter_context(self.nc.semaphore("s%d" % len(self.sems)))
        return self.sems[k]

    def _bump(self, key, amt):
        ep, v = self.cnt.get(key, (0, 0))
        if v + amt > SEM_LIMIT:
            ep, v = ep + 1, 0
        v += amt
        self.cnt[key] = (ep, v)
        return (key, ep, v)

    def op(self, engine, fn, reads=(), writes=(), dma=False, semkey=None, pe_self=False):
        deps = set()
        for r in reads:
            if r in self.last_w:
                deps.add(self.last_w[r])
            if r.startswith("ps_"):
                for ev in self.readers.get(r, ()):
                    deps.add(ev)
        for w in writes:
            if w in self.last_w:
                deps.add(self.last_w[w])
            for ev in self.readers.get(w, ()):
                deps.add(ev)
        if dma:
            key = semkey if semkey is not None else ("dma", (writes[0] if writes else reads[0]))
            ev = self._bump(key, 16)
            amt = 16
        else:
            key = engine
            ev = self._bump(key, 1)
            amt = 1
        seen = self.seen[engine]
        best = {}
        for (k, ep, v) in deps:
            if k == "pe" and engine == "pe" and not dma and not pe_self:
                continue
            if best.get(k, (-1, -1)) < (ep, v):
                best[k] = (ep, v)
        waits = []
        for k, (ep, v) in best.items():
            if seen.get(k, (-1, -1)) >= (ep, v):
                continue
            seen[k] = (ep, v)
            waits.append((self._sem(k, ep), v))
        self.ops[engine].append((waits, fn, self._sem(ev[0], ev[1]), amt))
        for w in writes:
            self.last_w[w] = ev
            self.readers[w] = []
        for r in reads:
            if r not in writes:
                self.readers.setdefault(r, []).append(ev)
        self.nops += 1
        return ev

    def barrier(self, engines=None):
        for e in (engines or self.ENGS):
            waits = []
            seen = self.seen[e]
            for key, (ep, v) in self.cnt.items():
                if seen.get(key, (-1, -1)) >= (ep, v):
                    continue
                seen[key] = (ep, v)
                waits.append((self._sem(key, ep), v))
            if waits:
                self.ops[e].append((waits, None, None, 0))

    def emit(self):
        nc = self.nc
        with nc.Block() as block:
            def run(engname):
                def body(eng):
                    for waits, fn, sem, amt in self.ops[engname]:
                        for s, v in waits:
                            eng.wait_ge(s, v)
                        if fn is not None:
                            fn(eng).then_inc(sem, amt)
                return body
            block.tensor(run("pe"))
            block.scalar(run("act"))
            block.vector(run("dve"))
            block.gpsimd(run("pool"))
            block.sync(run("sp"))


def _bf(a):
    return np.asarray(a, dtype=np.float32).astype(ml_dtypes.bfloat16)


def _split3(a):
    a = np.asarray(a, dtype=np.float64)
    h = _bf(a)
    r = a - h.astype(np.float64)
    l = _bf(r)
    r2 = r - l.astype(np.float64)
    l2 = _bf(r2)
    return h, l, l2


def _constants():
    c = {}
    slopes = 2.0 ** (-8.0 * np.arange(1, 13, dtype=np.float64) / 12.0)
    sl_dil, sl_moba = slopes[0::2], slopes[1::2]
    c["c_ident"] = _bf(np.eye(128))
    bo = np.zeros((128, 128)); bo[:64, :64] = 1; bo[64:, 64:] = 1
    c["c_blockones"] = _bf(bo)
    c["c_ones"] = _bf(np.ones((128, 128)))
    kl = np.arange(128)[:, None]
    ql = np.arange(128)[None, :]
    tab = np.zeros((3, 128, 2, 3, 2, 2, 128), dtype=ml_dtypes.bfloat16)
    for p in range(3):
        for h in range(2):
            for di, d in enumerate(DIL):
                for jj in range(2):
                    delta = ql - kl + (128 if jj == 0 else 0)
                    valid = (delta >= 0) & (delta <= 128)
                    b = np.where(valid, -sl_dil[2 * p + h] * d * delta / SCALE, NEGM)
                    hi = _bf(b)
                    lo = _bf(b - hi.astype(np.float64))
                    tab[p, :, h, di, 0, jj, :] = hi
                    tab[p, :, h, di, 1, jj, :] = lo
    c["c_dilbias"] = tab.reshape(3, 128, 24 * 128)
    c["c_tri"] = _bf(np.where(kl <= ql, 0.0, NEGM))
    pm = np.zeros((16, 16))
    for npast in range(16):
        for n in range(16):
            pm[npast, n] = 0.0 if n < npast else (1e30 if n == npast else -1e30)
    c["c_pm2"] = np.broadcast_to(pm.reshape(1, 256), (128, 256)).astype(np.float32).copy()
    tok = np.arange(SEQ)
    mq = np.zeros((6, 6, SEQ), dtype=ml_dtypes.bfloat16)
    for h in range(6):
        cc = 1024.0 * sl_moba[h]
        a, b_, c_ = _split3(np.full(SEQ, cc))
        mq[h, 0], mq[h, 1], mq[h, 2] = a, b_, c_
        a, b_, c_ = _split3(-cc * (tok // 128))
        mq[h, 3], mq[h, 4], mq[h, 5] = a, b_, c_
    c["c_moba_q"] = mq
    mk = np.zeros((22, SEQ))
    for n in range(16):
        mk[n, n * 256:(n + 1) * 256] = 1.0
    mk[16:19] = (tok // 128)[None, :]
    mk[19:22] = 1.0
    c["c_moba_k"] = _bf(mk)
    c["c_moba_bcol"] = (sl_moba[None, :] * (np.arange(128)[:, None] - 64.0)).astype(np.float32)
    c["c_iota16"] = np.broadcast_to(np.arange(16, dtype=np.float32)[None, :], (128, 16)).copy()
    return c


def build_program(nseq=2, debug=False, do_peer=True, stages=('dil', 'moba', 'mem'), npair=3, peer_tiles=NT):
    nc = bass.Bass("TRN2", target_bir_lowering=False)

    def din(name, shape, dt):
        return nc.dram_tensor(name, list(shape), dt, kind="ExternalInput").ap()

    x_d = din("x", [nseq, SEQ, DM], F32)
    mem_d = din("mem", [nseq, 256, DM], F32)
    w_in_d = din("w_in", [DM, 2560], F32)
    w_mkv_d = din("w_mem_kv", [DM, 512], F32)
    w_out_d = din("w_out", [DM, DM], F32)
    w_pq_d = din("w_peer_q", [DM, 2048], F32)
    sub_d = din("subkeys", [16, 128, 128], F32)
    pu_d = din("peer_u", [16384 if do_peer else 128, DM], F32)
    pv_d = din("peer_v", [16384 if do_peer else 128, DM], F32)
    gbc_d = din("g_bc", [3, 128, DM], F32)
    gcol_d = din("g_cols", [128, 14], F32)
    cst = _constants()
    cd = {}
    for k, v in cst.items():
        dt = BF16 if v.dtype == ml_dtypes.bfloat16 else F32
        cd[k] = din(k, v.shape, dt)
    out_d = nc.dram_tensor("out", [nseq, SEQ, DM], F32, kind="ExternalOutput").ap()
    yscr_d = nc.dram_tensor("yscr", [nseq, 8, 128, SEQ], BF16,
                            kind=("ExternalOutput" if debug else "Internal")).ap()
    if debug:
        dbg_hnT = nc.dram_tensor("dbg_hnT", [128, 8, SEQ], BF16, kind="ExternalOutput").ap()
    NEXP = 16384 if do_peer else 128
    uv16_d = nc.dram_tensor("uv16", [NEXP, 2 * DM], BF16, kind="Internal").ap()

    with ExitStack() as top:
        S = Sched(nc, top)

        uid = [0]

        def sb(st, name, shape, dt):
            uid[0] += 1
            return st.enter_context(nc.sbuf_tensor("%s_%d" % (name, uid[0]), list(shape), dt))

        def ps(st, name, shape, dt):
            return st.enter_context(nc.psum_tensor(name, list(shape), dt))

        def mm(out, lhsT, rhs, start, stop, reads, writes, skip=False, pe_self=False):
            S.op("pe", lambda e: e.matmul(out, lhsT=lhsT, rhs=rhs, start=start, stop=stop, skip_group_check=skip), reads, writes,
                 pe_self=pe_self)

        def tr(out, in_, ident, reads, writes):
            S.op("pe", lambda e: e.transpose(out=out, in_=in_, identity=ident), reads, writes)

        def act(out, in_, func, reads, writes, bias=None, scale=None, accum_out=None):
            kw = {}
            if bias is not None:
                kw["bias"] = bias
            if scale is not None:
                kw["scale"] = scale
            if accum_out is not None:
                kw["accum_out"] = accum_out
            S.op("act", lambda e: e.activation(out=out, in_=in_, func=func, **kw), reads, writes)

        def acopy(out, in_, reads, writes):
            S.op("act", lambda e: e.copy(out=out, in_=in_), reads, writes)

        def vcopy(out, in_, reads, writes, eng="dve"):
            S.op(eng, lambda e: e.tensor_copy(out=out, in_=in_), reads, writes)

        def vtt(out, in0, in1, op, reads, writes, eng="dve"):
            S.op(eng, lambda e: e.tensor_tensor(out=out, in0=in0, in1=in1, op=op), reads, writes)

        def vts(out, in0, s1, s2, op0, op1, reads, writes, eng="dve"):
            if op1 is None:
                S.op(eng, lambda e: e.tensor_scalar(out=out, in0=in0, scalar1=s1, scalar2=None, op0=op0), reads, writes)
            else:
                S.op(eng, lambda e: e.tensor_scalar(out=out, in0=in0, scalar1=s1, scalar2=s2, op0=op0, op1=op1), reads, writes)

        def vstt(out, in0, scalar, in1, op0, op1, reads, writes):
            S.op("dve", lambda e: e.scalar_tensor_tensor(out=out, in0=in0, scalar=scalar, in1=in1, op0=op0, op1=op1),
                 reads, writes)

        def vrecip(out, in_, reads, writes):
            S.op("dve", lambda e: e.reciprocal(out=out, in_=in_), reads, writes)

        def dma(out, in_, reads, writes, eng="sp", semkey=None):
            S.op(eng, lambda e: e.dma_start(out=out, in_=in_), reads, writes, dma=True, semkey=semkey)

        ident = sb(top, "ident", [128, 128], BF16)
        blockones = sb(top, "blockones", [128, 128], BF16)
        ones = sb(top, "ones", [128, 128], BF16)
        gcols = sb(top, "gcols", [128, 14], F32)
        epsc = sb(top, "epsc", [128, 1], F32)
        tri = sb(top, "tri", [128, 128], BF16)
        pm2 = sb(top, "pm2", [128, 16, 16], F32)
        bcol = sb(top, "bcol", [128, 6], F32)
        iota16 = sb(top, "iota16", [128, 16], F32)
        dma(ident[:], cd["c_ident"], [], ["ident"])
        dma(blockones[:], cd["c_blockones"], [], ["blockones"])
        dma(ones[:], cd["c_ones"], [], ["ones"])
        dma(gcols[:], gcol_d, [], ["gcols"])
        dma(tri[:], cd["c_tri"], [], ["tri"])
        dma(pm2[:], cd["c_pm2"].rearrange("p (a b) -> p a b", a=16), [], ["pm2"])
        dma(bcol[:], cd["c_moba_bcol"], [], ["bcol"])
        dma(iota16[:], cd["c_iota16"], [], ["iota16"])
        S.op("dve", lambda e: e.memset(epsc[:], EPS), [], ["epsc"])

        ps_tr = ps(top, "ps_tr", [128, 8, 128], BF16)
        ps_pj = [ps(top, "ps_pj%d" % i, [128, 512], F32) for i in range(2)]
        ps_ss = ps(top, "ps_ss", [128, 512], F32)
        ps_s = [ps(top, "ps_s%d" % i, [128, 4, 128], F32) for i in range(2)]
        ps_o = [ps(top, "ps_o%d" % i, [128, 4, 128], F32) for i in range(2)]

        def rms_rows_to_T(st, src_dram_tile_fn, ntiles, gslot, dstT, dst_name, tag):
            gbc = sb(st, "gbc" + tag, [128, DM], F32)
            dma(gbc[:], gbc_d[gslot], [], ["gbc" + tag])
            xt = [sb(st, "xt%s%d" % (tag, i), [128, DM], F32) for i in range(2)]
            hn = [sb(st, "hn%s%d" % (tag, i), [128, DM], BF16) for i in range(2)]
            junk = sb(st, "junk" + tag, [128, DM], BF16)
            ssq = sb(st, "ssq" + tag, [128, 2], F32)
            rt = sb(st, "rt" + tag, [128, 2], F32)
            rs = sb(st, "rs" + tag, [128, 2], F32)
            for t in range(ntiles):
                sl = t % 2
                X, H = "xt%s%d" % (tag, sl), "hn%s%d" % (tag, sl)
                dma(xt[sl][:], src_dram_tile_fn(t), [], [X])
                act(junk[:], xt[sl][:], AF.Square, [X], ["junk" + tag, "ssq%s%d" % (tag, sl)], accum_out=ssq[:, sl:sl + 1])
                act(rt[:, sl:sl + 1], ssq[:, sl:sl + 1], AF.Sqrt, ["ssq%s%d" % (tag, sl), "epsc"], ["rt%s%d" % (tag, sl)],
                    bias=epsc[:], scale=1.0 / DM)
                vrecip(rs[:, sl:sl + 1], rt[:, sl:sl + 1], ["rt%s%d" % (tag, sl)], ["rs%s%d" % (tag, sl)])
                vstt(hn[sl][:], xt[sl][:], rs[:, sl:sl + 1], gbc[:], ALU.mult, ALU.mult,
                     [X, "rs%s%d" % (tag, sl), "gbc" + tag], [H])
                for kc in range(8):
                    tr(ps_tr[:, kc, :], hn[sl][:, kc * 128:(kc + 1) * 128], ident[:], [H, "ident"], ["ps_tr"])
                acopy(dstT[:, :, t * 128:(t + 1) * 128], ps_tr[:], ["ps_tr"], [dst_name])

        wctr = [0]
        pend = [None]

        def load_w(st_w, wstage, wdst, wname, w_dram, c0, ncols):
            for cc in range(0, ncols, 128):
                sl = wctr[0] % 2
                wctr[0] += 1
                dma(wstage[sl][:], w_dram[:, c0 + cc:c0 + cc + 128].rearrange("(kc p) n -> p kc n", p=128),
                    [], ["wstage%d" % sl])
                eng = "pool" if (wctr[0] % 2) else "dve"
                vcopy(wdst[:, :, cc:cc + 128], wstage[sl][:], ["wstage%d" % sl], [wname], eng=eng)

        pjctr = [0]

        def proj_norm(wbf, wname, m0, M, src, srcname, ntok, gcol, blk, blkname, dst_fn, dstname, scr):
            sqb, rtb, rsb = scr
            N = min(512, ntok)
            for tc in range(ntok // N):
                sl = pjctr[0] % 2
                pjctr[0] += 1
                P = "ps_pj%d" % sl
                for kc in range(8):
                    mm(ps_pj[sl][0:M, 0:N], wbf[:, kc, m0:m0 + M], src[:, kc, tc * N:(tc + 1) * N], kc == 0, kc == 7,
                       [wname, srcname], [P])
                act(sqb[0:M, 0:N], ps_pj[sl][0:M, 0:N], AF.Square, [P], ["sqb"])
                mm(ps_ss[0:M, 0:N], blk[0:M, 0:M], sqb[0:M, 0:N], True, True, ["sqb", blkname], ["ps_ss"])
                act(rtb[0:M, 0:N], ps_ss[0:M, 0:N], AF.Sqrt, ["ps_ss", "epsc"], ["rtb"], bias=epsc[0:M, :], scale=1.0 / 64)
                vrecip(rsb[0:M, 0:N], rtb[0:M, 0:N], ["rtb"], ["rsb"])
                vstt(dst_fn(tc, N), ps_pj[sl][0:M, 0:N], gcol, rsb[0:M, 0:N], ALU.mult, ALU.mult,
                     [P, "rsb", "gcols"], [dstname])

        def proj_v(wbf, wname, c0, src, srcname, tok_slices, dst, dstname):
            nt = len(tok_slices)
            for jb in range(0, nt, 4):
                sl = pjctr[0] % 2
                pjctr[0] += 1
                P = "ps_pj%d" % sl
                nb = min(4, nt - jb)
                for i in range(nb):
                    for kc in range(8):
                        mm(ps_pj[sl][:, i * 128:(i + 1) * 128], src[:, kc, tok_slices[jb + i]], wbf[:, kc, c0:c0 + 128],
                           kc == 0, kc == 7, [wname, srcname], [P])
                o = dst[:, jb:jb + nb, :]
                i_ = ps_pj[sl][:, 0:nb * 128].rearrange("p (a b) -> p a b", a=nb)
                import os
                if (jb // 4) % 2 == 0 and not os.environ.get("PVDVE"):
                    acopy(o, i_, [P], [dstname])
                else:
                    vcopy(o, i_, [P], [dstname])

        def finalize_pair(acc, o16p, o16name, ssqacc, first, scr):
            sqb, rtb, rsb = scr
            for tc in range(8):
                c = slice(tc * 512, (tc + 1) * 512)
                vrecip(rsb[:, :], acc[:, 1, c], ["acc"], ["rsb"])
                vtt(rtb[:, :], acc[:, 0, c], rsb[:, :], ALU.mult, ["acc", "rsb"], ["rtb"])
                act(sqb[:, :], rtb[:, :], AF.Square, ["rtb"], ["sqb"])
                vcopy(o16p[:, c], rtb[:, :], ["rtb"], [o16name], eng="pool")
                mm(ps_ss[:, :], ones[:], sqb[:, :], True, True, ["sqb", "ones"], ["ps_ss"])
                if first:
                    vcopy(ssqacc[:, c], ps_ss[:, :], ["ps_ss"], ["ssqacc"])
                else:
                    vtt(ssqacc[:, c], ps_ss[:, :], ssqacc[:, c], ALU.add, ["ps_ss", "ssqacc"], ["ssqacc"])

        def group_norm_store(b, o16, npairs, nfeat, ssqacc, chunk0, ybuf, scr):
            sqb, rtb, rsb = scr
            for tc in range(8):
                c = slice(tc * 512, (tc + 1) * 512)
                act(rtb[:, :], ssqacc[:, c], AF.Sqrt, ["ssqacc", "epsc"], ["rtb"], bias=epsc[:], scale=1.0 / nfeat)
                vrecip(ssqacc[:, c], rtb[:, :], ["rtb"], ["ssqacc"])
            for p in range(npairs):
                Y = "o16_%d" % p
                for tc in range(8):
                    c = slice(tc * 512, (tc + 1) * 512)
                    vstt(o16[p][:, c], o16[p][:, c], gcols[:, 6 + chunk0 + p:7 + chunk0 + p], ssqacc[:, c],
                         ALU.mult, ALU.mult, [Y, "ssqacc", "gcols"], [Y])
                dma(yscr_d[b, chunk0 + p], o16[p][:], [Y], ["yscr%d_%d" % (b, chunk0 + p)])

        def attn_tile(sl, qk_list, v_list, acc_view, acc_first, exp_bias=None, evac_eng="dve", acc_part=None):
            Ps = "ps_s%d" % sl
            nslots = 0
            prev_base = None
            for (si, kT_ap, qT_ap, knames, extras) in qk_list:
                nslots = max(nslots, si + 1)
                base = kT_ap.base_partition()
                mm(ps_s[sl][:, si, :], kT_ap, qT_ap, True, len(extras) == 0, knames, [Ps],
                   pe_self=(prev_base is not None and base != prev_base))
                prev_base = base
                for ei, (l_, r_, nm) in enumerate(extras):
                    mm(ps_s[sl][:, si, :], l_, r_, False, ei == len(extras) - 1, nm, [Ps])
            PT = "pT%d" % sl
            used = sorted(q[0] for q in qk_list)
            if used == list(range(nslots)):
                ssel = slice(0, nslots)
            else:
                step = used[1] - used[0] if len(used) > 1 else 1
                assert used == list(range(used[0], used[-1] + 1, step))
                ssel = slice(used[0], used[-1] + 1, step)
            if exp_bias is None:
                act(pT[sl][:, ssel, :], ps_s[sl][:, ssel, :], AF.Exp, [Ps], [PT], scale=SCALE)
            else:
                act(pT[sl][:, ssel, :], ps_s[sl][:, ssel, :], AF.Exp, [Ps, "bcol"], [PT], scale=SCALE, bias=exp_bias)
            return PT


        NU = 13

        def peer(b):
            with ExitStack() as st:
                wout = sb(st, "wout", [128, 8, 1024], BF16)
                wpq = sb(st, "wpq", [128, 8, 2048], BF16)
                subT = sb(st, "subT", [128, 16, 128], BF16)
                wstage = [sb(st, "wstageP%d" % i, [128, 8, 128], F32) for i in range(2)]
                gffn = sb(st, "gffn", [128, DM], F32)
                dma(gffn[:], gbc_d[1], [], ["gffn"])
                wctr[0] = 0
                for cc in range(0, 1024, 128):
                    sl = (cc // 128) % 2
                    dma(wstage[sl][:], w_out_d[:, cc:cc + 128].rearrange("(kc p) n -> p kc n", p=128), [], ["wstageP%d" % sl])
                    vcopy(wout[:, :, cc:cc + 128], wstage[sl][:], ["wstageP%d" % sl], ["wout"], eng=("pool" if sl else "dve"))
                for cc in range(0, 2048, 128):
                    sl = (cc // 128) % 2
                    dma(wstage[sl][:], w_pq_d[:, cc:cc + 128].rearrange("(kc p) n -> p kc n", p=128), [], ["wstageP%d" % sl])
                    vcopy(wpq[:, :, cc:cc + 128], wstage[sl][:], ["wstageP%d" % sl], ["wpq"], eng=("pool" if sl else "dve"))

                ytc = [sb(st, "ytc%d" % i, [128, 8, 512], BF16) for i in range(2)]
                xt = [sb(st, "xtP%d" % i, [128, DM], F32) for i in range(2)]
                x1 = [sb(st, "x1_%d" % i, [128, DM], F32) for i in range(2)]
                hn2b = sb(st, "hn2b", [128, DM], BF16)
                junkb = sb(st, "junkP", [128, DM], BF16)
                hn2T = sb(st, "hn2T", [128, 8, 128], BF16)
                qryT = sb(st, "qryT", [128, 16, 128], BF16)
                sc = sb(st, "sc", [128, 16, 128], F32)
                dma(sc[:], sub_d.rearrange("c k d -> k c d"), [], ["sc"])
                vcopy(qryT[:], sc[:], ["sc"], ["qryT"])
                for c8 in range(2):
                    for i in range(8):
                        tr(ps_tr[:, i, :], qryT[:, c8 * 8 + i, :], ident[:], ["qryT", "ident"], ["ps_tr"])
                    acopy(subT[:, c8 * 8:(c8 + 1) * 8, :], ps_tr[:], ["ps_tr"], ["subT"])
                wk1 = sb(st, "wk1", [128, 128], F32)
                v12 = sb(st, "v12", [128, 8, 2, 16], F32)
                i12 = sb(st, "i12", [128, 8, 2, 16], U32)
                i12f = sb(st, "i12f", [128, 8, 2, 16], F32)
                cand = sb(st, "cand", [128, 8, 256], F32)
                wk2 = sb(st, "wk2", [128, 256], F32)
                ts = sb(st, "ts", [128, 8, 16], F32)
                pos = sb(st, "pos", [128, 8, 16], U32)
                pab = sb(st, "pab", [128, 2, 8, 16], U32)
                pabf = sb(st, "pabf", [128, 2, 8, 16], F32)
                oh = sb(st, "oh", [128, 8, 16, 16], F32)
                e12 = sb(st, "e12", [128, 2, 8, 16], F32)
                ef = sb(st, "ef", [128, 128], F32)
                idx = sb(st, "idx", [128, 128], U32)
                ex = sb(st, "ex", [128, 8, 16], F32)
                gs = sb(st, "gs", [128, 8], F32)
                gate = sb(st, "gate", [128, 128], F32)
                aact = sb(st, "aact", [128, 128], F32)
                wgt = sb(st, "wgt", [128, 128], F32)
                ssq = sb(st, "ssqP", [128, 1], F32)
                rt = sb(st, "rtP", [128, 1], F32)
                rs = sb(st, "rsP", [128, 1], F32)
                uvb = [sb(st, "uvb%d" % i, [128, 2 * DM], BF16) for i in range(NU)]
                ga = sb(st, "ga", [128, 128], F32)
                dg = [sb(st, "dg%d" % i, [128, 128], BF16) for i in range(6)]
                yres = ["yscr%d_%d" % (b, c) for c in range(8)]

                hn2bs = [hn2b, sb(st, "hn2b_b", [128, DM], BF16)]
                idxs = [idx, sb(st, "idx_b", [128, 128], U32)]
                gates = [gate, sb(st, "gate_b", [128, 128], F32)]

                def stage1(t):
                    sl = t % 2
                    X, X1 = "xtP%d" % sl, "x1_%d" % sl
                    HB, IDX, GT = "hn2b%d" % sl, "idx%d" % sl, "gate%d" % sl
                    hn2b_, idx_, gate_ = hn2bs[sl], idxs[sl], gates[sl]
                    ysl = (t // 4) % 2
                    YT = "ytc%d" % ysl
                    if t % 4 == 0:
                        dma(ytc[ysl][:], yscr_d[b, :, :, t * 128:t * 128 + 512].rearrange("c p n -> p c n"), yres, [YT])
                    dma(xt[sl][:], x_d[b, t * 128:(t + 1) * 128, :], [], [X])
                    yield
                    tcol = slice((t % 4) * 128, (t % 4 + 1) * 128)
                    for nh in range(2):
                        for kc in range(8):
                            mm(ps_o[nh][:].rearrange("p a b -> p (a b)"), ytc[ysl][:, kc, tcol], wout[:, kc, nh * 512:(nh + 1) * 512],
                               kc == 0, kc == 7, [YT, "wout"], ["ps_o%d" % nh])
                        yield
                    for nh in range(2):
                        c = slice(nh * 512, (nh + 1) * 512)
                        vtt(x1[sl][:, c], ps_o[nh][:].rearrange("p a b -> p (a b)"), xt[sl][:, c], ALU.add, ["ps_o%d" % nh, X], [X1])
                        yield
                    act(junkb[:], x1[sl][:], AF.Square, [X1], ["junkP", "ssqP"], accum_out=ssq[:, 0:1])
                    act(rt[:], ssq[:], AF.Sqrt, ["ssqP", "epsc"], ["rtP"], bias=epsc[:], scale=1.0 / DM)
                    vrecip(rs[:], rt[:], ["rtP"], ["rsP"])
                    yield
                    vstt(hn2b_[:], x1[sl][:], rs[:, 0:1], gffn[:], ALU.mult, ALU.mult, [X1, "rsP", "gffn"], [HB])
                    yield
                    for kc in range(8):
                        tr(ps_tr[:, kc, :], hn2b_[:, kc * 128:(kc + 1) * 128], ident[:], [HB, "ident"], ["ps_tr"])
                    acopy(hn2T[:], ps_tr[:], ["ps_tr"], ["hn2T"])
                    yield
                    for c4 in range(4):
                        q_ = c4 % 2
                        for i in range(4):
                            c = c4 * 4 + i
                            for kc in range(8):
                                mm(ps_s[q_][:, i, :], wpq[:, kc, c * 128:(c + 1) * 128], hn2T[:, kc, :], kc == 0, kc == 7,
                                   ["wpq", "hn2T"], ["ps_s%d" % q_])
                            yield
                        acopy(qryT[:, c4 * 4:(c4 + 1) * 4, :], ps_s[q_][:], ["ps_s%d" % q_], ["qryT"])
                    for c4 in range(4):
                        q_ = c4 % 2
                        for i in range(4):
                            c = c4 * 4 + i
                            mm(ps_s[q_][:, i, :], qryT[:, c, :], subT[:, c, :], True, True, ["qryT", "subT"], ["ps_s%d" % q_])
                        acopy(sc[:, c4 * 4:(c4 + 1) * 4, :], ps_s[q_][:], ["ps_s%d" % q_], ["sc"])
                        yield
                    for h in range(8):
                        for hf in range(2):
                            a_ = sc[:, 2 * h + hf, :]
                            S.op("dve", lambda e, a_=a_, h=h, hf=hf: e.max(out=v12[:, h, hf, 0:8], in_=a_), ["sc"], ["v12"])
                            yield
                            S.op("dve", lambda e, a_=a_, h=h, hf=hf: e.match_replace(out=wk1[:], in_to_replace=v12[:, h, hf, 0:8],
                                                                                   in_values=a_, imm_value=-1e30), ["sc", "v12"], ["wk1"])
                            yield
                            S.op("dve", lambda e, h=h, hf=hf: e.max(out=v12[:, h, hf, 8:16], in_=wk1[:]), ["wk1"], ["v12"])
                            yield
                            S.op("dve", lambda e, a_=a_, h=h, hf=hf: e.max_index(out=i12[:, h, hf, 0:8], in_max=v12[:, h, hf, 0:8],
                                                                               in_values=a_), ["sc", "v12"], ["i12"])
                            yield
                            S.op("dve", lambda e, a_=a_, h=h, hf=hf: e.max_index(out=i12[:, h, hf, 8:16], in_max=v12[:, h, hf, 8:16],
                                                                               in_values=a_), ["sc", "v12"], ["i12"])
                            yield
                        cv = cand[:, h, :].rearrange("p (a b) -> p a b", a=16)
                        vtt(cv, v12[:, h, 0, :].unsqueeze(2).to_broadcast([128, 16, 16]),
                            v12[:, h, 1, :].unsqueeze(1).to_broadcast([128, 16, 16]), ALU.add, ["v12"], ["cand"])
                        yield
                        ch = cand[:, h, :]
                        S.op("dve", lambda e, ch=ch, h=h: e.max(out=ts[:, h, 0:8], in_=ch), ["cand"], ["ts"])
                        yield
                        S.op("dve", lambda e, ch=ch, h=h: e.match_replace(out=wk2[:], in_to_replace=ts[:, h, 0:8], in_values=ch,
                                                                        imm_value=-1e30), ["cand", "ts"], ["wk2"])
                        yield
                        S.op("dve", lambda e, h=h: e.max(out=ts[:, h, 8:16], in_=wk2[:]), ["wk2"], ["ts"])
                        yield
                        S.op("dve", lambda e, ch=ch, h=h: e.max_index(out=pos[:, h, 0:8], in_max=ts[:, h, 0:8], in_values=ch),
                             ["cand", "ts"], ["pos"])
                        yield
                        S.op("dve", lambda e, ch=ch, h=h: e.max_index(out=pos[:, h, 8:16], in_max=ts[:, h, 8:16], in_values=ch),
                             ["cand", "ts"], ["pos"])
                        yield
                    vts(pab[:, 0], pos[:], 4, None, ALU.logical_shift_right, None, ["pos"], ["pab"])
                    vts(pab[:, 1], pos[:], 15, None, ALU.bitwise_and, None, ["pos"], ["pab"])
                    yield
                    vcopy(pabf[:], pab[:], ["pab"], ["pabf"])
                    vcopy(i12f[:], i12[:], ["i12"], ["i12f"])
                    yield
                    for w_ in range(2):
                        vtt(oh[:], pabf[:, w_].unsqueeze(3).to_broadcast([128, 8, 16, 16]),
                            iota16[:].unsqueeze(1).unsqueeze(1).to_broadcast([128, 8, 16, 16]), ALU.is_equal,
                            ["pabf", "iota16"], ["oh"])
                        yield
                        vtt(oh[:], oh[:], i12f[:, :, w_, :].unsqueeze(2).to_broadcast([128, 8, 16, 16]), ALU.mult,
                            ["oh", "i12f"], ["oh"])
                        yield
                        S.op("dve", lambda e, w_=w_: e.tensor_reduce(out=e12[:, w_], in_=oh[:], axis=AX.X, op=ALU.add), ["oh"], ["e12"])
                        yield
                    vstt(ef[:], e12[:, 0].rearrange("p a b -> p (a b)"), 128.0, e12[:, 1].rearrange("p a b -> p (a b)"),
                         ALU.mult, ALU.add, ["e12"], ["ef"])
                    vcopy(idx_[:], ef[:], ["ef"], [IDX])
                    yield
                    vtt(ex[:], ts[:], ts[:, :, 0:1].to_broadcast([128, 8, 16]), ALU.subtract, ["ts"], ["ex"])
                    act(ex[:], ex[:], AF.Exp, ["ex"], ["ex"])
                    yield
                    S.op("dve", lambda e: e.tensor_reduce(out=gs[:], in_=ex[:], axis=AX.X, op=ALU.add), ["ex"], ["gs"])
                    vrecip(gs[:], gs[:], ["gs"], ["gs"])
                    vtt(gate_[:].rearrange("p (a b) -> p a b", a=8), ex[:], gs[:].unsqueeze(2).to_broadcast([128, 8, 16]), ALU.mult,
                        ["ex", "gs"], [GT])
                    yield

                def advance(g, n=1):
                    if g is None:
                        return None
                    try:
                        for _ in range(n):
                            next(g)
                    except StopIteration:
                        return None
                    return g

                g = stage1(0)
                while g is not None:
                    g = advance(g, 8)
                for t in range(peer_tiles):
                    sl = t % 2
                    X1 = "x1_%d" % sl
                    HB, IDX, GT = "hn2b%d" % sl, "idx%d" % sl, "gate%d" % sl
                    hn2b_, idx_, gate_ = hn2bs[sl], idxs[sl], gates[sl]
                    g = stage1(t + 1) if t + 1 < peer_tiles else None
                    LAG = 4
                    for j in range(128 + LAG):
                        if j < 128:
                            u_ = j % NU
                            A_, G_ = "aact%d" % (j % 8), "ga%d" % (j % 8)
                            S.op("pool", lambda e, u_=u_, j=j, idx_=idx_: e.indirect_dma_start(
                                out=uvb[u_][:], out_offset=None, in_=uv16_d,
                                in_offset=bass.IndirectOffsetOnAxis(ap=idx_[:, j:j + 1], axis=0)),
                                [IDX, "uv16"], ["uvb%d" % u_], dma=True)
                            S.op("dve", lambda e, u_=u_, j=j, hn2b_=hn2b_: e.scalar_tensor_tensor(
                                out=junkb[:], in0=uvb[u_][:, 0:DM], scalar=1.0, in1=hn2b_[:], op0=ALU.mult, op1=ALU.mult,
                                accum_out=aact[:, j:j + 1]), ["uvb%d" % u_, HB], ["junkP", A_])
                            act(ga[:, j:j + 1], aact[:, j:j + 1], AF.Gelu, [A_], [G_])
                        jj = j - LAG
                        if jj >= 0:
                            u2 = jj % NU
                            d_ = jj % 6
                            vts(dg[d_][:], ident[:], ga[:, jj:jj + 1], gate_[:, jj:jj + 1], ALU.mult, ALU.mult,
                                ["ident", "ga%d" % (jj % 8), GT], ["dg%d" % d_])
                            for nh in range(2):
                                mm(ps_pj[nh][:, :], dg[d_][:], uvb[u2][:, DM + nh * 512:DM + (nh + 1) * 512], jj == 0, jj == 127,
                                   ["dg%d" % d_, "uvb%d" % u2], ["ps_pj%d" % nh], skip=True)
                        g = advance(g, 2 if (j % 2) else 1)
                    while g is not None:
                        g = advance(g, 8)
                    for nh in range(2):
                        c = slice(nh * 512, (nh + 1) * 512)
                        vtt(x1[sl][:, c], ps_pj[nh][:, :], x1[sl][:, c], ALU.add, ["ps_pj%d" % nh, X1], [X1])
                    dma(out_d[b, t * 128:(t + 1) * 128, :], x1[sl][:], [X1], ["out%d" % sl], semkey=("dma", "out%d" % sl))
                S.barrier()

        if do_peer:
            with ExitStack() as cst_:
                stg = [sb(cst_, "cstg%d" % i, [128, 2, DM], F32) for i in range(3)]
                cbf = [sb(cst_, "cbf%d" % i, [128, 2, DM], BF16) for i in range(3)]
                k_ = 0
                for (src_, dst_, nm_) in ((pu_d, uv16_d[:, 0:DM], "uv16"), (pv_d, uv16_d[:, DM:2 * DM], "uv16")):
                    for ch in range(NEXP // 256):
                        s_ = k_ % 3
                        k_ += 1
                        rows = slice(ch * 256, (ch + 1) * 256)
                        dma(stg[s_][:], src_[rows, :].rearrange("(p r) d -> p r d", r=2), [], ["cstg%d" % s_])
                        if s_ == 0:
                            vcopy(cbf[s_][:], stg[s_][:], ["cstg%d" % s_], ["cbf%d" % s_])
                        elif s_ == 1:
                            acopy(cbf[s_][:], stg[s_][:], ["cstg%d" % s_], ["cbf%d" % s_])
                        else:
                            vcopy(cbf[s_][:], stg[s_][:], ["cstg%d" % s_], ["cbf%d" % s_], eng="pool")
                        dma(dst_[rows, :].rearrange("(p r) d -> p r d", r=2), cbf[s_][:], ["cbf%d" % s_], [nm_],
                            semkey=("dma", "cbfo%d" % s_))
                S.barrier()

        for b in range(nseq):
            with ExitStack() as seqst:
                hnT = sb(seqst, "hnT", [128, 8, SEQ], BF16)
                with ExitStack() as st:
                    rms_rows_to_T(st, lambda t: x_d[b, t * 128:(t + 1) * 128, :], NT, 0, hnT, "hnT", "A")
                    if debug:
                        dma(dbg_hnT, hnT[:], ["hnT"], ["dbg_hnT"])
                    S.barrier()
                with ExitStack() as st:
                    wstage = [sb(st, "wstage%d" % i, [128, 8, 128], F32) for i in range(2)]
                    wq = sb(st, "wq", [128, 8, 128], BF16)
                    wk = sb(st, "wk", [128, 8, 128], BF16)
                    wv = sb(st, "wv", [128, 8, 128], BF16)
                    sqb = sb(st, "sqb", [128, 512], BF16)
                    rtb = sb(st, "rtb", [128, 512], F32)
                    rsb = sb(st, "rsb", [128, 512], F32)
                    scr = (sqb, rtb, rsb)
                    acc = sb(st, "acc", [128, 2, SEQ], F32)
                    ssqacc = sb(st, "ssqacc", [128, SEQ], F32)
                    o16 = [sb(st, "o16_%d" % i, [128, SEQ], BF16) for i in range(3)]
                    pT = [sb(st, "pT%d" % i, [128, 4, 128], BF16) for i in range(2)]
                    ybuf = o16
                    Vd = sb(st, "Vd", [128, NT, 128], BF16)

                    with ExitStack() as gst:
                      if "dil" in stages:
                        qT = sb(gst, "qT", [128, SEQ], BF16)
                        kT = sb(gst, "kT", [128, SEQ], BF16)
                        dbias = sb(gst, "dbias", [128, 24, 128], BF16)
                        for p in range(min(3, npair)):
                            dma(dbias[:], cd["c_dilbias"][p].rearrange("k (a q) -> k a q", a=24), [], ["dbias"])
                            load_w(st, wstage, wq, "wq", w_in_d, 0 + p * 128, 128)
                            load_w(st, wstage, wk, "wk", w_in_d, 384 + p * 128, 128)
                            load_w(st, wstage, wv, "wv", w_in_d, 768 + p * 128, 128)
                            proj_norm(wq, "wq", 0, 128, hnT, "hnT", SEQ, gcols[:, 0:1], blockones, "blockones",
                                      lambda tc, N: qT[:, tc * N:(tc + 1) * N], "qT", scr)
                            proj_norm(wk, "wk", 0, 128, hnT, "hnT", SEQ, gcols[:, 1:2], blockones, "blockones",
                                      lambda tc, N: kT[:, tc * N:(tc + 1) * N], "kT", scr)
                            for di, d in enumerate(DIL):
                                nblk = NT // d
                                tsl = []
                                for j in range(NT):
                                    r, n = divmod(j, nblk)
                                    base = d * 128 * n + r
                                    tsl.append(slice(base, base + 127 * d + 1, d))
                                proj_v(wv, "wv", 0, hnT, "hnT", tsl, Vd, "Vd")
                                for j in range(NT):
                                    r, n = divmod(j, nblk)
                                    sl = j % 2
                                    have_prev = n > 0
                                    qk = []
                                    for h in range(2):
                                        hp = 64 * h
                                        for jj in range(2):
                                            if jj == 0 and not have_prev:
                                                continue
                                            tk = tsl[j - 1] if jj == 0 else tsl[j]
                                            bi = ((h * 3 + di) * 2 + 0) * 2 + jj
                                            bl = ((h * 3 + di) * 2 + 1) * 2 + jj
                                            qk.append((h * 2 + jj, kT[hp:hp + 64, tk], qT[hp:hp + 64, tsl[j]], ["kT", "qT"],
                                                       [(ident[:], dbias[:, bi, :], ["ident", "dbias"]),
                                                        (ident[:], dbias[:, bl, :], ["ident", "dbias"])]))
                                    PT = attn_tile(sl, qk, None, None, None)

                                    def pv_dil(j=j, sl=sl, have_prev=have_prev, PT=PT, di=di, tsl=tsl):
                                        Po = "ps_o%d" % sl
                                        for h in range(2):
                                            hp = 64 * h
                                            jjs = [1] if not have_prev else [0, 1]
                                            for jj in jjs:
                                                jk = j - 1 if jj == 0 else j
                                                mm(ps_o[sl][hp:hp + 64, 0, :], Vd[:, jk, hp:hp + 64], pT[sl][:, h * 2 + jj, :],
                                                   jj == jjs[0], jj == 1, ["Vd", PT], [Po])
                                            for jj in jjs:
                                                mm(ps_o[sl][hp:hp + 64, 1, :], ones[:, 0:64], pT[sl][:, h * 2 + jj, :],
                                                   jj == jjs[0], jj == 1, ["ones", PT], [Po])
                                        av = acc[:, :, tsl[j]]
                                        if di == 0:
                                            vcopy(av, ps_o[sl][:, 0:2, :], [Po], ["acc"])
                                        else:
                                            vtt(av, ps_o[sl][:, 0:2, :], av, ALU.add, [Po, "acc"], ["acc"])
                                    if pend[0] is not None:
                                        pend[0]()
                                    pend[0] = pv_dil
                                if pend[0] is not None:
                                    pend[0]()
                                    pend[0] = None
                            finalize_pair(acc, o16[p], "o16_%d" % p, ssqacc, p == 0, scr)
                        group_norm_store(b, o16, min(3, npair), 384, ssqacc, 0, ybuf, scr)
                        S.barrier()

                    with ExitStack() as gst:
                      if "moba" in stages:
                        qa = sb(gst, "qa", [128, SEQ], BF16)
                        ka = sb(gst, "ka", [128, SEQ], BF16)
                        km32 = sb(gst, "km32", [64, 16], F32)
                        kmT = sb(gst, "kmT", [64, 16], BF16)
                        gm = sb(gst, "gm", [128, 16], F32)
                        mx8 = sb(gst, "mx8", [128, 8], F32)
                        pen = sb(gst, "pen", [128, 16], F32)
                        penb = sb(gst, "penb", [128, 16], BF16)
                        dma(ka[64:86, :], cd["c_moba_k"], [], ["ka_c"])
                        tsl = [slice(j * 128, (j + 1) * 128) for j in range(NT)]
                        for p in range(min(3, npair)):
                            load_w(st, wstage, wq, "wq", w_in_d, 1152 + p * 128, 128)
                            load_w(st, wstage, wk, "wk", w_in_d, 1536 + p * 128, 128)
                            load_w(st, wstage, wv, "wv", w_in_d, 1920 + p * 128, 128)
                            proj_v(wv, "wv", 0, hnT, "hnT", tsl, Vd, "Vd")
                            for h in range(2):
                                H = 2 * p + h
                                hp = 64 * h
                                dma(qa[80:86, :], cd["c_moba_q"][H], [], ["qa_c"])
                                proj_norm(wq, "wq", hp, 64, hnT, "hnT", SEQ, gcols[0:64, 2:3], ones, "ones",
                                          lambda tc, N: qa[0:64, tc * N:(tc + 1) * N], "qa", scr)
                                proj_norm(wk, "wk", hp, 64, hnT, "hnT", SEQ, gcols[0:64, 3:4], ones, "ones",
                                          lambda tc, N: ka[0:64, tc * N:(tc + 1) * N], "ka", scr)
                                S.op("dve", lambda e: e.tensor_reduce(out=km32[:, :], in_=ka[0:64, :].rearrange("p (a b) -> p a b", a=16),
                                                                      axis=AX.X, op=ALU.add), ["ka"], ["km32"])
                                vts(kmT[:, :], km32[:, :], 1.0 / 256, None, ALU.mult, None, ["km32"], ["kmT"])
                                for t in range(NT):
                                    mm(ps_ss[:, 0:16], qa[0:64, tsl[t]], kmT[:, :], True, True, ["qa", "kmT"], ["ps_ss"])
                                    vtt(gm[:, :], ps_ss[:, 0:16], pm2[:, t // 2, :], ALU.add, ["ps_ss", "pm2"], ["gm"])
                                    S.op("dve", lambda e: e.max(out=mx8[:, :], in_=gm[:, :]), ["gm"], ["mx8"])
                                    vts(pen[:, :], gm[:, :], mx8[:, 3:4], None, ALU.is_ge, None, ["gm", "mx8"], ["pen"])
                                    vts(penb[:, :], pen[:, :], -NEGM, NEGM, ALU.mult, ALU.add, ["pen"], ["penb"])
                                    mm(ps_ss[64:80, 128:256], penb[:, :], ident[:], True, True, ["penb", "ident"], ["ps_ss"])
                                    acopy(qa[64:80, tsl[t]], ps_ss[64:80, 128:256], ["ps_ss"], ["qa_p"])
                                cnt = 0
                                for t in range(NT):
                                    osl = t % 2
                                    Po = "ps_o%d" % osl
                                    for b0 in range(0, t + 1, 4):
                                        nb = min(4, t + 1 - b0)
                                        sl = cnt % 2
                                        cnt += 1
                                        qk = []
                                        for i in range(nb):
                                            kt = b0 + i
                                            ex = [(ident[:], tri[:], ["ident", "tri"])] if kt == t else []
                                            qk.append((i, ka[0:86, tsl[kt]], qa[0:86, tsl[t]], ["ka", "ka_c", "qa", "qa_c", "qa_p"], ex))
                                        PT = attn_tile(sl, qk, None, None, None, exp_bias=bcol[:, H:H + 1])

                                        def pv_moba(t=t, b0=b0, nb=nb, sl=sl, osl=osl, Po=Po, PT=PT, hp=hp, tsl=tsl):
                                            for i in range(nb):
                                                kt = b0 + i
                                                mm(ps_o[osl][hp:hp + 64, 0, :], Vd[:, kt, hp:hp + 64], pT[sl][:, i, :], kt == 0, kt == t,
                                                   ["Vd", PT], [Po], skip=True)
                                                mm(ps_o[osl][hp:hp + 64, 1, :], ones[:, 0:64], pT[sl][:, i, :], False, kt == t,
                                                   ["ones", PT], [Po], skip=True)
                                            if b0 + nb == t + 1:
                                                acopy(acc[hp:hp + 64, :, tsl[t]], ps_o[osl][hp:hp + 64, 0:2, :], [Po], ["acc"])
                                        if pend[0] is not None:
                                            pend[0]()
                                        pend[0] = pv_moba
                                if pend[0] is not None:
                                    pend[0]()
                                    pend[0] = None
                            finalize_pair(acc, o16[p], "o16_%d" % p, ssqacc, p == 0, scr)
                        group_norm_store(b, o16, min(3, npair), 384, ssqacc, 3, ybuf, scr)
                        S.barrier()

                    with ExitStack() as gst:
                      if "mem" in stages:
                        memT = sb(gst, "memT", [128, 8, 256], BF16)
                        import os
                        MEMCUT = int(os.environ.get("MEMCUT", "9"))
                        rms_rows_to_T(gst, lambda t: mem_d[b, t * 128:(t + 1) * 128, :], 2, 2, memT, "memT", "M")
                        qT = sb(gst, "qTm", [128, SEQ], BF16)
                        kmem = sb(gst, "kmem", [128, 256], BF16)
                        Vm = sb(gst, "Vm", [128, 2, 128], BF16)
                        tsl = [slice(j * 128, (j + 1) * 128) for j in range(NT)]
                        for p in range(min(2, npair) if MEMCUT > 1 else 0):
                            load_w(st, wstage, wq, "wq", w_in_d, 2304 + p * 128, 128)
                            load_w(st, wstage, wk, "wk", w_mkv_d, 0 + p * 128, 128)
                            load_w(st, wstage, wv, "wv", w_mkv_d, 256 + p * 128, 128)
                            proj_norm(wq, "wq", 0, 128, hnT, "hnT", SEQ, gcols[:, 4:5], blockones, "blockones",
                                      lambda tc, N: qT[:, tc * N:(tc + 1) * N], "qTm", scr)
                            if MEMCUT <= 2:
                                continue
                            proj_norm(wk, "wk", 0, 128, memT, "memT", 256, gcols[:, 5:6], blockones, "blockones",
                                      lambda tc, N: kmem[:, tc * N:(tc + 1) * N], "kmem", scr)
                            if MEMCUT <= 3:
                                continue
                            if os.environ.get("PVA"):
                                proj_v(wv, "wv", 0, memT, "memT", tsl[0:2], Vd, "Vd")
                            elif os.environ.get("PVB"):
                                proj_v(wv, "wv", 0, hnT, "hnT", tsl[0:2], Vm, "Vm")
                            else:
                                proj_v(wv, "wv", 0, memT, "memT", tsl[0:2], Vm, "Vm")
                            if MEMCUT <= 4:
                                continue
                            for t in range(NT):
                                sl = t % 2
                                Po = "ps_o%d" % sl
                                qk = []
                                for h in range(2):
                                    hp = 64 * h
                                    for jj in range(2):
                                        qk.append((h * 2 + jj, kmem[hp:hp + 64, tsl[jj]], qT[hp:hp + 64, tsl[t]], ["kmem", "qTm"], []))
                                PT = attn_tile(sl, qk, None, None, None)

                                def pv_mem(t=t, sl=sl, Po=Po, PT=PT, tsl=tsl):
                                    for h in range(2):
                                        hp = 64 * h
                                        for jj in range(2):
                                            mm(ps_o[sl][hp:hp + 64, 0, :], Vm[:, jj, hp:hp + 64], pT[sl][:, h * 2 + jj, :], jj == 0, jj == 1,
                                               ["Vm", PT], [Po])
                                        for jj in range(2):
                                            mm(ps_o[sl][hp:hp + 64, 1, :], ones[:, 0:64], pT[sl][:, h * 2 + jj, :], jj == 0, jj == 1,
                                               ["ones", PT], [Po])
                                    vcopy(acc[:, :, tsl[t]], ps_o[sl][:, 0:2, :], [Po], ["acc"])
                                if pend[0] is not None:
                                    pend[0]()
                                pend[0] = pv_mem
                            if pend[0] is not None:
                                pend[0]()
                                pend[0] = None
                            finalize_pair(acc, o16[p], "o16_%d" % p, ssqacc, p == 0, scr)
                        group_norm_store(b, o16, min(2, npair), 256, ssqacc, 6, ybuf, scr)
                        S.barrier()
            S.barrier()
            if do_peer:
                peer(b)
        S.barrier()
        S.emit()
    return nc, cst


def _host_inputs(inputs, cst, nseq, core, do_peer=True):
    f = lambda a: np.ascontiguousarray(np.asarray(a, dtype=np.float32))
    m = {}
    m["x"] = f(inputs["x"][core * nseq:(core + 1) * nseq])
    m["mem"] = f(inputs["mem"][core * nseq:(core + 1) * nseq])
    m["w_in"] = f(inputs["w_in"][0])
    m["w_mem_kv"] = f(inputs["w_mem_kv"][0])
    m["w_out"] = f(inputs["w_out"][0])
    m["w_peer_q"] = f(inputs["w_peer_q"][0])
    s1 = np.asarray(inputs["peer_subkeys_1"][0]); s2 = np.asarray(inputs["peer_subkeys_2"][0])
    m["subkeys"] = f(np.stack([s1, s2], axis=1).reshape(16, 128, 128))
    m["peer_u"] = f(inputs["peer_u"][0] if do_peer else inputs["peer_u"][0][:128])
    m["peer_v"] = f(inputs["peer_v"][0] if do_peer else inputs["peer_v"][0][:128])
    gb = np.stack([np.broadcast_to(np.asarray(inputs[k][0]), (128, DM)) for k in ("g_mix", "g_ffn", "g_memtok")])
    m["g_bc"] = f(gb)
    cols = []
    for k in ("qg_dil", "kg_dil", "qg_moba", "kg_moba", "qg_mem", "kg_mem"):
        cols.append(np.tile(np.asarray(inputs[k][0]), 2))
    og = np.concatenate([np.asarray(inputs["og_dil"][0]), np.asarray(inputs["og_moba"][0]), np.asarray(inputs["og_mem"][0])])
    for c in range(8):
        cols.append(og[c * 128:(c + 1) * 128])
    m["g_cols"] = f(np.stack(cols, axis=1))
    for k, v in cst.items():
        m[k] = v
    return m


_CACHE = {}


def kernel(**inputs):
    nseq = 2
    if "prog" not in _CACHE:
        _CACHE["prog"] = build_program(nseq=nseq)
    nc, cst = _CACHE["prog"]
    in_maps = [_host_inputs(inputs, cst, nseq, c) for c in range(NCORES)]
    res = run_bass_kernel_spmd(nc, in_maps, core_ids=list(range(NCORES)))
    out = np.concatenate([np.asarray(r["out"]) for r in res.results], axis=0)
    return out.astype(np.float32)
```
